# Optimizing a Trainium2 kernel written in Bass

```python
import math
import jax, jax.numpy as jnp
from jax import lax
import numpy as np

D_MODEL = 2048
BATCH = 8
SEQ = 2048
DEPTH = 1

RWKV_HEAD = 64
RWKV_WIDTH = D_MODEL // 2
RWKV_HEADS = RWKV_WIDTH // RWKV_HEAD
DECAY_LORA = max(32, int(round(RWKV_WIDTH ** 0.5 * 1.8 / 32)) * 32)
AAA_LORA = max(32, int(round(RWKV_WIDTH ** 0.5 * 1.8 / 32)) * 32)
GATE_LORA = max(32, int(round(RWKV_WIDTH ** 0.8 * 0.6 / 32)) * 32)
RWKV_COLS = 3 * RWKV_WIDTH + DECAY_LORA + AAA_LORA + GATE_LORA

DIFF_HEAD = 64
DIFF_VDIM = 2 * DIFF_HEAD
DIFF_WIDTH = D_MODEL - RWKV_WIDTH
DIFF_HEADS = DIFF_WIDTH // DIFF_VDIM
DIFF_QK_COLS = DIFF_HEADS * 2 * DIFF_HEAD
DIFF_COLS = 2 * DIFF_QK_COLS + DIFF_WIDTH
IN_COLS = RWKV_COLS + DIFF_COLS
Q_BLOCK = 128

N_EXPERTS = 32
TOP_K = 4
D_FF = D_MODEL
SWIGLU_LIMIT = 7.0
SWIGLU_ALPHA = 1.702
MOE_BLOCK = 128

LN_EPS = 1e-5
GN_EPS = RWKV_HEAD * 1e-5
RMS_EPS = 1e-5
NEG_BIG = -1e30
DEEPNORM_ALPHA = (2.0 * DEPTH) ** 0.25
DEEPNORM_BETA = (8.0 * DEPTH) ** -0.25

kernel_name = "hybrid_rwkv7_diffattn_moe_deepnorm"


def _split_cols(p, sizes):
    offs = [int(o) for o in np.cumsum(sizes)[:-1]]
    return jnp.split(p, offs, axis=-1)


def layer_norm(x, g, b):
    xf = x.astype(jnp.float32)
    mu = jnp.mean(xf, -1, keepdims=True)
    var = jnp.mean(jnp.square(xf - mu), -1, keepdims=True)
    y = (xf - mu) * lax.rsqrt(var + LN_EPS) * g.astype(jnp.float32) + b.astype(jnp.float32)
    return y.astype(x.dtype)


def token_shift(p, mu):
    prev = jnp.pad(p, ((0, 0), (1, 0), (0, 0)))[:, :-1]
    return p + (prev - p) * mu


def rwkv7_mix(p, mu, w0, w_up, a0, a_up, g_up, k_k, k_a, r_k, gn_g, gn_b):
    B, S, _ = p.shape
    H, N = RWKV_HEADS, RWKV_HEAD
    f32 = jnp.float32
    p = token_shift(p, mu)
    r, k, v, dw, da, dg = _split_cols(p, [RWKV_WIDTH] * 3 + [DECAY_LORA, AAA_LORA, GATE_LORA])
    w = (w0 + jnp.tanh(dw) @ w_up).astype(f32)
    w = -jax.nn.softplus(-w) - 0.5
    decay = jnp.exp(-jnp.exp(w))
    a = jax.nn.sigmoid((a0 + da @ a_up).astype(f32))
    g = (jax.nn.sigmoid(dg) @ g_up).astype(f32)
    hs = lambda t: t.astype(f32).reshape(B, S, H, N)
    r, k, v, a, decay = hs(r), hs(k), hs(v), hs(a), hs(decay)
    kk = k * k_k.astype(f32).reshape(H, N)
    kk = kk / jnp.maximum(jnp.sqrt(jnp.sum(kk * kk, -1, keepdims=True)), 1e-12)
    k = k * (1.0 + (a - 1.0) * k_a.astype(f32).reshape(H, N))

    def step(state, inp):
        r_t, w_t, k_t, v_t, a_t, b_t = inp
        sa = jnp.einsum('bhvk,bhk->bhv', state, a_t)
        state = (state * w_t[:, :, None, :] + sa[..., None] * b_t[:, :, None, :]
                 + v_t[..., None] * k_t[:, :, None, :])
        y_t = jnp.einsum('bhvk,bhk->bhv', state, r_t)
        return state, y_t

    xs = tuple(jnp.swapaxes(t, 0, 1) for t in (r, decay, k, v, -kk, kk * a))
    s0 = jnp.zeros((B, H, N, N), f32)
    _, y = lax.scan(step, s0, xs)
    y = jnp.swapaxes(y, 0, 1)
    mu_y = jnp.mean(y, -1, keepdims=True)
    var_y = jnp.mean(jnp.square(y - mu_y), -1, keepdims=True)
    y = ((y - mu_y) * lax.rsqrt(var_y + GN_EPS)).reshape(B, S, RWKV_WIDTH)
    y = y * gn_g.astype(f32) + gn_b.astype(f32)
    bonus = jnp.sum(r * k * r_k.astype(f32), -1, keepdims=True) * v
    out = (y + bonus.reshape(B, S, RWKV_WIDTH)) * g
    return out.astype(p.dtype)


def diff_attention(p, lq1, lk1, lq2, lk2, subln_g, lambda_init):
    B, S, _ = p.shape
    H, Dh, Vd = DIFF_HEADS, DIFF_HEAD, DIFF_VDIM
    f32 = jnp.float32
    q, k, v = _split_cols(p, [DIFF_QK_COLS, DIFF_QK_COLS, DIFF_WIDTH])
    q = q.reshape(B, S, H, 2, Dh)
    k = k.reshape(B, S, H, 2, Dh)
    v = v.reshape(B, S, H, Vd).astype(f32)
    lam = (jnp.exp(jnp.sum(lq1.astype(f32) * lk1.astype(f32)))
           - jnp.exp(jnp.sum(lq2.astype(f32) * lk2.astype(f32))) + lambda_init)
    nb = S // Q_BLOCK
    qb = jnp.moveaxis(q.reshape(B, nb, Q_BLOCK, H, 2, Dh), 1, 0)
    kpos = jnp.arange(S)
    scale = Dh ** -0.5

    def block(args):
        q_blk, start = args
        qpos = start + jnp.arange(Q_BLOCK)
        mask = kpos[None, :] <= qpos[:, None]
        s = jnp.einsum('bqhmd,bkhmd->bmhqk', q_blk, k).astype(f32) * scale
        s = jnp.where(mask, s, NEG_BIG)
        pr = jax.nn.softmax(s, axis=-1)
        attn = pr[:, 0] - lam * pr[:, 1]
        return jnp.einsum('bhqk,bkhd->bqhd', attn, v)

    o = lax.map(block, (qb, jnp.arange(nb) * Q_BLOCK))
    o = jnp.moveaxis(o, 0, 1).reshape(B, S, H, Vd)
    o = o * lax.rsqrt(jnp.mean(o * o, -1, keepdims=True) + RMS_EPS) * subln_g.astype(f32)
    o = o * (1.0 - lambda_init)
    return o.reshape(B, S, DIFF_WIDTH).astype(p.dtype)


def moe_ffn(h, w_router, b_router, w_gu, b_gu, w_dn, b_dn):
    B, S, D = h.shape
    n_tok = B * S
    nk = n_tok * TOP_K
    hf = h.reshape(n_tok, D)
    logits = (hf @ w_router + b_router).astype(jnp.float32)
    top_vals, top_idx = lax.top_k(logits, TOP_K)
    gates = jax.nn.softmax(top_vals, axis=-1)
    flat_e = top_idx.reshape(-1)
    flat_g = gates.reshape(-1)
    flat_t = jnp.arange(nk, dtype=jnp.int32) // TOP_K
    order = jnp.argsort(flat_e)
    se, st, sg = flat_e[order], flat_t[order], flat_g[order]
    counts = jnp.bincount(flat_e, length=N_EXPERTS)
    starts = jnp.cumsum(counts) - counts
    padded = ((counts + MOE_BLOCK - 1) // MOE_BLOCK) * MOE_BLOCK
    pends = jnp.cumsum(padded)
    pstarts = pends - padded
    dest = pstarts[se] + (jnp.arange(nk) - starts[se])
    n_blocks = (nk + MOE_BLOCK - 1) // MOE_BLOCK + N_EXPERTS
    n_rows = n_blocks * MOE_BLOCK
    row_tok = jnp.zeros((n_rows,), jnp.int32).at[dest].set(st)
    row_gate = jnp.zeros((n_rows,), jnp.float32).at[dest].set(sg)
    block_e = jnp.minimum(jnp.searchsorted(pends, jnp.arange(n_blocks) * MOE_BLOCK, side='right'),
                          N_EXPERTS - 1)
    xs = hf[row_tok].reshape(n_blocks, MOE_BLOCK, D)

    def expert_block(args):
        xb, e = args
        gu = xb @ w_gu[e] + b_gu[e]
        gate, up = gu[:, :D_FF], gu[:, D_FF:]
        gate = jnp.minimum(gate, SWIGLU_LIMIT)
        up = jnp.clip(up, -SWIGLU_LIMIT, SWIGLU_LIMIT)
        act = (up + 1.0) * (gate * jax.nn.sigmoid(SWIGLU_ALPHA * gate))
        return act @ w_dn[e] + b_dn[e]

    ys = lax.map(expert_block, (xs, block_e)).reshape(n_rows, D)
    y = jnp.zeros((n_tok, D), h.dtype).at[row_tok].add(ys * row_gate[:, None].astype(h.dtype))
    return y.reshape(B, S, D)


def setup_inputs(seed: int = 0) -> dict:
    key = jax.random.key(seed)
    ks = iter(jax.random.split(key, 40))
    f32 = jnp.float32

    def nrm(shape, scale):
        return jax.random.normal(next(ks), shape, f32) * scale

    L, D, C = DEPTH, D_MODEL, RWKV_WIDTH
    x = nrm((BATCH, SEQ, D), 1.0)
    col_scale = jnp.ones((IN_COLS,), f32)
    col_scale = col_scale.at[2 * C:3 * C].set(DEEPNORM_BETA)
    col_scale = col_scale.at[RWKV_COLS + 2 * DIFF_QK_COLS:].set(DEEPNORM_BETA)
    w_in = nrm((L, D, IN_COLS), D ** -0.5) * col_scale
    shift_mu = jax.random.uniform(next(ks), (L, RWKV_COLS), f32)
    w0 = jnp.linspace(-6.5, -1.5, C, dtype=f32)[None, :] + nrm((L, C), 0.1)
    w_up = nrm((L, DECAY_LORA, C), 0.1 * DECAY_LORA ** -0.5)
    a0 = nrm((L, C), 0.1)
    a_up = nrm((L, AAA_LORA, C), AAA_LORA ** -0.5)
    g_up = nrm((L, GATE_LORA, C), GATE_LORA ** -0.5)
    k_k = 0.85 + nrm((L, C), 0.02)
    k_a = 1.0 + nrm((L, C), 0.02)
    r_k = nrm((L, RWKV_HEADS, RWKV_HEAD), 0.1)
    gn_g = 1.0 + nrm((L, C), 0.02)
    gn_b = nrm((L, C), 0.01)
    lq1 = nrm((L, DIFF_HEAD), 0.1)
    lk1 = nrm((L, DIFF_HEAD), 0.1)
    lq2 = nrm((L, DIFF_HEAD), 0.1)
    lk2 = nrm((L, DIFF_HEAD), 0.1)
    subln_g = 1.0 + nrm((L, DIFF_VDIM), 0.02)
    w_out = nrm((L, D, D), D ** -0.5 * DEEPNORM_BETA)
    ln1_g = 1.0 + nrm((L, D), 0.02)
    ln1_b = nrm((L, D), 0.01)
    w_router = nrm((L, D, N_EXPERTS), D ** -0.5)
    b_router = nrm((L, N_EXPERTS), 0.01)
    w_gu = nrm((L, N_EXPERTS, D, 2 * D_FF), D ** -0.5 * DEEPNORM_BETA)
    b_gu = nrm((L, N_EXPERTS, 2 * D_FF), 0.01)
    w_dn = nrm((L, N_EXPERTS, D_FF, D), D_FF ** -0.5 * DEEPNORM_BETA)
    b_dn = nrm((L, N_EXPERTS, D), 0.01)
    ln2_g = 1.0 + nrm((L, D), 0.02)
    ln2_b = nrm((L, D), 0.01)
    return {"x": x, "w_in": w_in, "shift_mu": shift_mu, "w0": w0, "w_up": w_up,
            "a0": a0, "a_up": a_up, "g_up": g_up, "k_k": k_k, "k_a": k_a, "r_k": r_k,
            "gn_g": gn_g, "gn_b": gn_b, "lq1": lq1, "lk1": lk1, "lq2": lq2, "lk2": lk2,
            "subln_g": subln_g, "w_out": w_out, "ln1_g": ln1_g, "ln1_b": ln1_b,
            "w_router": w_router, "b_router": b_router, "w_gu": w_gu, "b_gu": b_gu,
            "w_dn": w_dn, "b_dn": b_dn, "ln2_g": ln2_g, "ln2_b": ln2_b}


def reference(x, w_in, shift_mu, w0, w_up, a0, a_up, g_up, k_k, k_a, r_k, gn_g, gn_b,
              lq1, lk1, lq2, lk2, subln_g, w_out, ln1_g, ln1_b,
              w_router, b_router, w_gu, b_gu, w_dn, b_dn, ln2_g, ln2_b):
    for l in range(DEPTH):
        lambda_init = 0.8 - 0.6 * math.exp(-0.3 * l)
        p = x @ w_in[l]
        p_rwkv, p_diff = p[..., :RWKV_COLS], p[..., RWKV_COLS:]
        h_rwkv = rwkv7_mix(p_rwkv, shift_mu[l], w0[l], w_up[l], a0[l], a_up[l], g_up[l],
                           k_k[l], k_a[l], r_k[l], gn_g[l], gn_b[l])
        h_diff = diff_attention(p_diff, lq1[l], lk1[l], lq2[l], lk2[l], subln_g[l], lambda_init)
        mix = jnp.concatenate([h_rwkv, h_diff], axis=-1) @ w_out[l]
        x = layer_norm(DEEPNORM_ALPHA * x + mix, ln1_g[l], ln1_b[l])
        ffn = moe_ffn(x, w_router[l], b_router[l], w_gu[l], b_gu[l], w_dn[l], b_dn[l])
        x = layer_norm(DEEPNORM_ALPHA * x + ffn, ln2_g[l], ln2_b[l])
    return x
```

```python
import math
from contextlib import ExitStack
import numpy as np
import ml_dtypes
import concourse.bass as bass
import concourse.mybir as mybir
from concourse.bass_utils import run_bass_kernel_spmd

F32 = mybir.dt.float32
BF16 = mybir.dt.bfloat16
I32 = mybir.dt.int32
U32 = mybir.dt.uint32
AF = mybir.ActivationFunctionType
ALU = mybir.AluOpType
AX = mybir.AxisListType

SEQ = 2048
DM = 2048
NE = 32
CAP = 384
ALPHA = 2.0 ** 0.25
LAM_INIT = 0.8 - 0.6
SCALE = 64 ** -0.5
EXPM05 = math.exp(-0.5)
N_FM = 43


class Sched:
    def __init__(self, nc, stack):
        self.nc = nc
        self.stack = stack
        self.engs = {"pe": nc.tensor, "act": nc.scalar, "dve": nc.vector,
                     "pool": nc.gpsimd, "sp": nc.sync}
        self.sems = {}
        self.cnt = {}
        self.waited = {e: {} for e in self.engs}
        self.last_write = {}
        self.readers = {}
        for e in ("pe", "act", "dve", "pool"):
            self._sem("E_" + e)
        self.n_ins = 0
        self.last_rg = None
        self.last_pe_inc = True

    def _sem(self, name):
        if name not in self.sems:
            self.sems[name] = self.stack.enter_context(self.nc.semaphore(name))
            self.cnt[name] = 0
        return self.sems[name]

    def _emit_waits(self, eng, reads, writes):
        need = {}

        def add(tok, same_ok):
            if tok is None:
                return
            s, v = tok
            if same_ok and s == "E_" + eng and eng == "pe":
                return
            if need.get(s, 0) < v:
                need[s] = v
        for k in reads:
            add(self.last_write.get(k), False)
        for k in writes:
            add(self.last_write.get(k), True)
            for t in self.readers.get(k, ()):
                add(t, True)
        e = self.engs[eng]
        for s, v in need.items():
            if self.waited[eng].get(s, 0) >= v:
                continue
            e.wait_ge(self.sems[s], v)
            self.waited[eng][s] = v
            self.n_ins += 1

    def _record(self, tok, reads, writes):
        for k in reads:
            self.readers.setdefault(k, []).append(tok)
        for k in writes:
            self.last_write[k] = tok
            self.readers[k] = []

    def op(self, eng, fn, reads=(), writes=(), inc=True, rg=None):
        if eng == "pe":
            if rg is not None and self.last_rg is not None and rg != self.last_rg:
                assert self.last_pe_inc
                self.engs["pe"].wait_ge(self.sems["E_pe"], self.cnt["E_pe"])
                self.n_ins += 1
            self.last_rg = rg
            self.last_pe_inc = inc
        self._emit_waits(eng, reads, writes)
        ins = fn(self.engs[eng])
        self.n_ins += 1
        s = "E_" + eng
        if inc:
            self.cnt[s] += 1
            ins.then_inc(self.sems[s], 1)
            tok = (s, self.cnt[s])
        else:
            tok = (s, self.cnt[s] + 1)
        self._record(tok, reads, writes)
        return ins

    def dma(self, q, out, in_, semkey, reads=(), writes=(), indirect=None, **kw):
        self._emit_waits(q, reads, writes)
        s = "D_" + semkey
        self._sem(s)
        if indirect is None:
            ins = self.engs[q].dma_start(out=out, in_=in_, **kw)
        else:
            ins = self.engs[q].indirect_dma_start(out, indirect[0], in_, indirect[1], **kw)
        self.n_ins += 1
        self.cnt[s] += 16
        ins.then_inc(self.sems[s], 16)
        tok = (s, self.cnt[s])
        self._record(tok, reads, writes)
        return ins

    def barrier(self):
        for eng in self.engs:
            e = self.engs[eng]
            for s, v in self.cnt.items():
                if v == 0 or s == "E_" + eng:
                    continue
                if self.waited[eng].get(s, 0) >= v:
                    continue
                e.wait_ge(self.sems[s], v)
                self.waited[eng][s] = v
                self.n_ins += 1
        self.last_write = {}
        self.readers = {}


class K:
    pass


def build(cfg=None):
    cfg = cfg or {}
    phases = cfg.get("phases", "ABCD")
    dbg = cfg.get("dbg", ())
    nc = bass.Bass("TRN2", target_bir_lowering=False)
    k = K()
    k.nc, k.cfg, k.dbg = nc, cfg, dbg
    D = {}
    k.D = D

    def din(name, shape, dt=F32):
        D[name] = nc.dram_tensor(name, list(shape), dt, kind="ExternalInput").ap()

    def dout(name, shape, dt=F32):
        D[name] = nc.dram_tensor(name, list(shape), dt, kind="ExternalOutput").ap()

    def dscr(name, shape, dt):
        if name in cfg.get("ext_in", ()):
            D[name] = nc.dram_tensor(name, list(shape), dt, kind="ExternalInput").ap()
        elif name in cfg.get("ext", ()):
            D[name] = nc.dram_tensor(name, list(shape), dt, kind="ExternalOutput").ap()
        else:
            D[name] = nc.dram_tensor(name, list(shape), dt).ap()
    k.dout = dout

    din("xT", [DM, SEQ]); din("x", [SEQ, DM])
    din("w_fm", [N_FM, 128, 2048]); din("w_v", [2, 128, 16 * 512])
    din("mu", [128, 27]); din("chv", [128, 56])
    din("w_up", [64, 1024]); din("a_up", [64, 1024]); din("g_up", [160, 1024])
    din("lqk", [1, 256]); din("subln", [128, 1])
    din("w_out", [DM, DM]); din("ln1", [2, DM]); din("ln2", [2, DM])
    din("w_router", [DM, NE]); din("b_router", [1, NE])
    din("w_gu", [cfg.get("ne_decl", NE), DM, 2 * DM]); din("b_gu_r", [128, NE * 32])
    din("w_dn", [cfg.get("ne_decl", NE), DM, DM]); din("b_dn", [NE, DM])
    din("zeros", [CAP, DM], BF16)
    dout("out", [SEQ, DM])
    dscr("hT_d", [16, 128, SEQ], BF16)
    dscr("ra_d", [8, 128, 4096], BF16); dscr("bt_d", [8, 128, SEQ], BF16); dscr("kt_d", [8, 128, SEQ], BF16)
    dscr("vb_d", [8, 128, SEQ], BF16); dscr("bo_d", [8, 128, SEQ], F32); dscr("bg_d", [8, 128, SEQ], F32)
    dscr("gl_d", [8, 128, 16], F32)
    dscr("x1f_d", [SEQ, DM], F32)
    dscr("xdisp_d", [NE * CAP, DM], BF16)
    dscr("y_d", [NE * CAP, DM], F32)

    with ExitStack() as st:
        k.st = st
        k.S = Sched(nc, st)

        def sb(name, shape, dt=F32, stack=None):
            return (stack or st).enter_context(nc.sbuf_tensor(name, list(shape), dt))

        def ps(name, shape, dt=F32, stack=None):
            return (stack or st).enter_context(nc.psum_tensor(name, list(shape), dt))
        k.sb, k.ps = sb, ps
        setup_consts(k)
        k.dest_all = sb("dest_all", [128, 16, 4], I32)
        k.gates_all = sb("gates_all", [128, 16, 4], F32)
        k.S.op("dve", lambda e: e.memset(k.dest_all[:], 0), writes=["dest_all"])
        k.S.op("dve", lambda e: e.memset(k.gates_all[:], 0.0), writes=["gates_all"])
        if "A" in phases:
            with ExitStack() as pst:
                k.pst = pst
                if cfg.get("pro", True):
                    phase_a_setup(k)
                if cfg.get("rwkv", True) and cfg.get("pro", True):
                    with ExitStack() as st2:
                        phase_rwkv_pro(k, st2)
                    k.S.barrier()
                if cfg.get("diff", True):
                    with ExitStack() as st2:
                        phase_diff(k, st2)
            k.S.barrier()
            if cfg.get("rwkv", True) and cfg.get("scan", True):
                with ExitStack() as st2:
                    phase_rwkv_scan(k, st2)
                k.S.barrier()
        if "B" in phases:
            with ExitStack() as pst:
                k.pst = pst
                phase_b(k)
            k.S.barrier()
        if "route" in dbg:
            dout("dbg_dest", [128, 64], I32); dout("dbg_gates", [128, 64], F32)
            k.S.dma("sp", D["dbg_dest"], k.dest_all[:].rearrange("p c k -> p (c k)"), "dbg_dest", reads=["dest_all"])
            k.S.dma("sp", D["dbg_gates"], k.gates_all[:].rearrange("p c k -> p (c k)"), "dbg_gates", reads=["gates_all"])
            k.S.barrier()
        if "C" in phases:
            with ExitStack() as pst:
                k.pst = pst
                phase_c(k)
            k.S.barrier()
        if "D" in phases:
            with ExitStack() as pst:
                k.pst = pst
                phase_d(k)
        k.S.barrier()
    k.n_ins = k.S.n_ins
    return nc, k


def setup_consts(k):
    nc, S, sb = k.nc, k.S, k.sb
    k.iota_jp = sb("iota_jp", [128, 512], F32)
    S.op("pool", lambda e: e.iota(k.iota_jp[:], [[1, 512]], base=0, channel_multiplier=-1,
                                  allow_small_or_imprecise_dtypes=True), writes=["iota_jp"])
    k.ident_bf = sb("ident_bf", [128, 128], BF16)
    k.ident_f = sb("ident_f", [128, 128], F32)
    k.m_incl = sb("m_incl", [128, 128], F32)
    k.m_strict = sb("m_strict", [128, 128], F32)
    k.m_incl_bf = sb("m_incl_bf", [128, 128], BF16)
    k.m_strict_bf = sb("m_strict_bf", [128, 128], BF16)
    k.ones_bf = sb("ones_bf", [128, 128], BF16)
    k.ones_f = sb("ones_f", [128, 128], F32)
    k.blk_bf = sb("blk_bf", [128, 128], BF16)
    k.blk_f = sb("blk_f", [128, 128], F32)
    k.iota_e = sb("iota_e", [128, NE], F32)
    ij = k.iota_jp[:, 0:128]
    for t, op, key in ((k.ident_bf, ALU.is_equal, "ident_bf"), (k.ident_f, ALU.is_equal, "ident_f"),
                       (k.m_incl, ALU.is_ge, "m_incl"), (k.m_strict, ALU.is_gt, "m_strict"),
                       (k.m_incl_bf, ALU.is_ge, "m_incl_bf"), (k.m_strict_bf, ALU.is_gt, "m_strict_bf")):
        S.op("dve", lambda e, t=t, op=op: e.tensor_single_scalar(t[:], ij, 0.0, op),
             reads=["iota_jp"], writes=[key])
    S.op("dve", lambda e: e.memset(k.ones_bf[:], 1.0), writes=["ones_bf"])
    S.op("dve", lambda e: e.memset(k.ones_f[:], 1.0), writes=["ones_f"])
    for t, key in ((k.blk_bf, "blk_bf"), (k.blk_f, "blk_f")):
        S.op("dve", lambda e, t=t: e.memset(t[:], 0.0), writes=[key])
        S.op("dve", lambda e, t=t: e.memset(t[0:64, 0:64], 1.0), writes=[key])
        S.op("dve", lambda e, t=t: e.memset(t[64:128, 64:128], 1.0), writes=[key])
    S.op("pool", lambda e: e.iota(k.iota_e[:], [[1, NE]], base=0, channel_multiplier=0,
                                  allow_small_or_imprecise_dtypes=True), writes=["iota_e"])
    k.iota4 = sb("iota4", [128, 4, 128], F32)
    S.op("pool", lambda e: e.iota(k.iota4[:], [[0, 4], [1, 128]], base=0, channel_multiplier=-1,
                                  allow_small_or_imprecise_dtypes=True), writes=["iota4"])
    k.ms4 = sb("ms4", [128, 4, 128], BF16)
    k.ml4 = sb("ml4", [128, 4, 128], BF16)
    k.mi4 = sb("mi4", [128, 4, 128], BF16)
    k.I4 = sb("I4", [128, 4, 128], F32)
    for t, op, key in ((k.ms4, ALU.is_gt, "ms4"), (k.ml4, ALU.is_lt, "ml4"), (k.mi4, ALU.is_ge, "mi4"), (k.I4, ALU.is_equal, "I4")):
        S.op("dve", lambda e, t=t, op=op: e.tensor_single_scalar(t[:], k.iota4[:], 0.0, op),
             reads=["iota4"], writes=[key])
    k.eps_gn = sb("eps_gn", [128, 1], F32)
    S.op("dve", lambda e: e.memset(k.eps_gn[:], 64e-5), writes=["eps_gn"])
    k.eps_rms = sb("eps_rms", [128, 1], F32)
    S.op("dve", lambda e: e.memset(k.eps_rms[:], 1e-5), writes=["eps"])


def phase_a_setup(k):
    nc, S, D = k.nc, k.S, k.D
    sb = lambda n, s, d=F32: k.sb(n, s, d, k.pst)
    k.xT = sb("xT_sb", [128, 16, SEQ], BF16)
    xv = D["xT"].rearrange("(c p) t -> p c t", p=128)
    for i in range(4):
        S.dma("pool", k.xT[:, 4 * i:4 * i + 4, :], xv[:, 4 * i:4 * i + 4, :], "xT%d" % i, writes=[("xT", i)])
    k.xT_keys = [("xT", i) for i in range(4)]
    k.wfm = [sb("wfm%d" % i, [128, 16, 128], BF16) for i in range(2)]
    k.wfm_n = 0
    k.mu = sb("mu_sb", [128, 27]); k.omm = sb("omm_sb", [128, 27])
    S.dma("sp", k.mu[:], D["mu"], "mu", writes=["mu"])
    S.op("dve", lambda e: e.tensor_scalar(k.omm[:], k.mu[:], -1.0, 1.0, ALU.mult, ALU.add),
         reads=["mu"], writes=["omm"])


def load_wfm(k, tile):
    S, D = k.S, k.D
    i = k.wfm_n % 2
    k.wfm_n += 1
    S.dma("pool", k.wfm[i][:], D["w_fm"][tile].rearrange("p (c n) -> p c n", n=128), "wfm%d" % i,
          writes=[("wfm", i)])
    return k.wfm[i], ("wfm", i)


def inproj_fm(k, tile, ps_pair, evac, ncols=128):
    S = k.S
    wt, wkey = load_wfm(k, tile)
    for g in range(4):
        pst, pkey = ps_pair[g % 2]
        for dc in range(16):
            S.op("pe", lambda e: e.matmul(pst[0:ncols, :], wt[:, dc, 0:ncols], k.xT[:, dc, g * 512:(g + 1) * 512],
                                          start=(dc == 0), stop=(dc == 15)),
                 reads=[wkey, ("xT", dc // 4)], writes=[pkey], inc=(dc == 15))
        evac(g, pst[0:ncols, :], pkey)


def phase_diff(k, st2):
    nc, S, D = k.nc, k.S, k.D
    sb = lambda n, s, d=F32: k.sb(n, s, d, st2)
    ps = lambda n, s, d=F32: k.ps(n, s, d, st2)
    ps_s = [(ps("dps_s%d" % i, [128, 512]), ("dps_s", i)) for i in range(3)]
    ps_ms = (ps("dps_ms", [128, 512]), ("dps_ms", 0))
    ps_in = ps_s[0:2]
    ps_o = [(ps("dps_o%d" % i, [128, 512]), ("dps_o", i)) for i in range(2)]
    ps_l = [(ps("dps_l%d" % i, [128, 512]), ("dps_l", i)) for i in range(2)]
    qk = [sb("qk%d" % i, [128, 2, SEQ], BF16) for i in range(2)]
    v4 = sb("v4", [128, 16, 512], BF16)
    wv = sb("wv", [128, 16, 512], BF16)
    pT = [sb("pT%d" % i, [128, 512], BF16) for i in range(3)]
    rl = sb("rl", [128, 512]); o0 = sb("o0", [128, 512]); o1 = sb("o1", [128, 512]); oo = sb("oo", [128, 512])
    sq = sb("sq", [128, 512], BF16); rstd = sb("rstd", [128, 512])
    hst = [sb("hst%d" % i, [128, 512], BF16) for i in range(2)]
    lqk = sb("lqk_sb", [128, 256]); prod = sb("lprod", [128, 128]); s12 = sb("ls12", [128, 2]); e12 = sb("le12", [128, 2])
    nlam = sb("nlam", [128, 1]); sgs = sb("sgs", [128, 1]); sgin = sb("sgin", [128, 1])
    S.dma("sp", lqk[:], D["lqk"][0].partition_broadcast(128), "lqk", writes=["lqk"])
    S.dma("sp", sgin[:], D["subln"], "sgin", writes=["sgin"])
    S.op("dve", lambda e: e.tensor_tensor(prod[:, 0:64], lqk[:, 0:64], lqk[:, 64:128], ALU.mult), reads=["lqk"], writes=["lprod"])
    S.op("dve", lambda e: e.tensor_tensor(prod[:, 64:128], lqk[:, 128:192], lqk[:, 192:256], ALU.mult), reads=["lqk"], writes=["lprod"])
    S.op("dve", lambda e: e.reduce_sum(s12[:], prod[:].rearrange("p (a n) -> p a n", a=2), AX.X), reads=["lprod"], writes=["ls12"])
    S.op("act", lambda e: e.activation(e12[:], s12[:], AF.Exp), reads=["ls12"], writes=["le12"])
    S.op("dve", lambda e: e.tensor_tensor(nlam[:], e12[:, 1:2], e12[:, 0:1], ALU.subtract), reads=["le12"], writes=["nlam"])
    S.op("dve", lambda e: e.tensor_scalar_add(nlam[:], nlam[:], -LAM_INIT), reads=["nlam"], writes=["nlam"])
    S.op("dve", lambda e: e.tensor_scalar_mul(sgs[:], sgin[:], 1.0 - LAM_INIT), reads=["sgin"], writes=["sgs"])

    if k.cfg.get("zfill", True):
        for e_ in range(NE):
            S.dma("sp", D["xdisp_d"][e_ * CAP:(e_ + 1) * CAP, :], D["zeros"], "zfill", writes=["xdisp"])
    for h in range(k.cfg.get("diff_heads", 8)):
        hh = h % 4
        if hh == 0:
            S.dma("pool", wv[:], D["w_v"][h // 4].rearrange("p (c n) -> p c n", n=512), "wv", writes=["wv"])
            for tc in range(16):
                pst, pkey = ps_in[tc % 2]
                for dc in range(16):
                    S.op("pe", lambda e: e.matmul(pst[:], k.xT[:, dc, tc * 128:(tc + 1) * 128], wv[:, dc, :],
                                                  start=(dc == 0), stop=(dc == 15)),
                         reads=["wv", ("xT", dc // 4)], writes=[pkey], inc=(dc == 15))
                S.op("act", lambda e: e.copy(v4[:, tc, :], pst[:]), reads=[pkey], writes=[("v4", tc)])
        qkb = qk[h % 2]
        qkey = ("qk", h % 2)
        for which, tile in ((0, 27 + h), (1, 35 + h)):
            def evac(g, pap, pkey, which=which):
                S.op("act", lambda e: e.copy(qkb[:, which, g * 512:(g + 1) * 512], pap), reads=[pkey], writes=[qkey])
            inproj_fm(k, tile, ps_in, evac)
        tiles = [(m, g, j) for g in range(4) for m in range(2) for j in range(4 * g + 4)]

        def emit_qk(n):
            m, g, j = tiles[n]
            i = j - 4 * g
            q0 = 128 * i if i > 0 else 0
            pst, pkey = ps_s[n % 3]
            S.op("pe", lambda e: e.matmul(pst[:, q0:512], qkb[64 * m:64 * m + 64, 1, j * 128:(j + 1) * 128],
                                          qkb[64 * m:64 * m + 64, 0, g * 512 + q0:(g + 1) * 512], start=True, stop=True),
                 reads=[qkey], writes=[pkey], rg=m)
        emit_qk(0)
        emit_qk(1)
        for n, (m, g, j) in enumerate(tiles):
            if n + 2 < len(tiles):
                emit_qk(n + 2)
            i = j - 4 * g
            q0 = 128 * i if i > 0 else 0
            pst, pkey = ps_s[n % 3]
            pt = pT[n % 3]
            ptk = ("pT", n % 3)
            acc = (2 * g + m) % 2
            S.op("act", lambda e: e.activation(pt[:, q0:512], pst[:, q0:512], AF.Exp, scale=SCALE), reads=[pkey], writes=[ptk])
            if i >= 0:
                S.op("pool", lambda e: e.tensor_tensor(pt[:, q0:q0 + 128], pt[:, q0:q0 + 128], k.m_incl_bf[:], ALU.mult),
                     reads=[ptk, "m_incl_bf"], writes=[ptk])
            last = (j == 4 * g + 3)
            S.op("pe", lambda e: e.matmul(ps_o[acc][0][:, q0:512], v4[:, j, hh * 128:(hh + 1) * 128], pt[:, q0:512],
                                          start=(j == 0), stop=last),
                 reads=[ptk, ("v4", j)], writes=[ps_o[acc][1]], inc=False)
            S.op("pe", lambda e: e.matmul(ps_l[acc][0][:, q0:512], k.ones_bf[:], pt[:, q0:512], start=(j == 0), stop=last),
                 reads=[ptk, "ones_bf"], writes=[ps_l[acc][1]], inc=True)
            if last:
                S.op("dve", lambda e: e.reciprocal(rl[:], ps_l[acc][0][:]), reads=[ps_l[acc][1]], writes=["rl"])
                if m == 0:
                    S.op("dve", lambda e: e.tensor_tensor(o0[:], ps_o[acc][0][:], rl[:], ALU.mult),
                         reads=[ps_o[acc][1], "rl"], writes=["o0"])
                else:
                    S.op("dve", lambda e: e.tensor_tensor(o1[:], ps_o[acc][0][:], rl[:], ALU.mult),
                         reads=[ps_o[acc][1], "rl"], writes=["o1"])
                    S.op("dve", lambda e: e.scalar_tensor_tensor(oo[:], o1[:], nlam[:], o0[:], ALU.mult, ALU.add),
                         reads=["o0", "o1", "nlam"], writes=["oo"])
                    S.op("pool", lambda e: e.tensor_tensor(sq[:], oo[:], oo[:], ALU.mult), reads=["oo"], writes=["sq"])
                    mp, mkey = ps_ms
                    S.op("pe", lambda e: e.matmul(mp[:], k.ones_bf[:], sq[:], start=True, stop=True),
                         reads=["sq", "ones_bf"], writes=[mkey])
                    S.op("act", lambda e: e.activation(rstd[:], mp[:], AF.Sqrt, bias=k.eps_rms[:], scale=1.0 / 128),
                         reads=[mkey, "eps"], writes=["rstd"])
                    S.op("dve", lambda e: e.reciprocal(rstd[:], rstd[:]), reads=["rstd"], writes=["rstd"])
                    hs = hst[g % 2]
                    S.op("dve", lambda e: e.scalar_tensor_tensor(hs[:], oo[:], sgs[:], rstd[:], ALU.mult, ALU.mult),
                         reads=["oo", "sgs", "rstd"], writes=[("hst", g % 2)])
                    S.dma("sp", D["hT_d"][8 + h, :, g * 512:(g + 1) * 512], hs[:], "hst%d" % (g % 2), reads=[("hst", g % 2)])


def phase_rwkv_pro(k, st2):
    nc, S, D = k.nc, k.S, k.D
    sb = lambda n, s, d=F32: k.sb(n, s, d, st2)
    ps = lambda n, s, d=F32: k.ps(n, s, d, st2)
    npairs = k.cfg.get("rwkv_pairs", 8)
    ps_in = [(ps("rps_in%d" % i, [128, 512]), ("rps_in", i)) for i in range(2)]
    NB = {}
    for n in ("Br", "Bk", "B1", "B4", "B5", "B6", "B8"):
        NB[n] = sb("rw_" + n, [128, SEQ])
    raw = sb("rw_raw", [128, SEQ + 4])
    NB["B2"] = raw[:, 1:SEQ + 1]
    NB["Bo"] = NB["B6"]; NB["Bg"] = NB["B5"]
    E2 = sb("rw_E2", [128, SEQ])
    RA = sb("rw_RA", [128, 16, 256], BF16)
    BT = sb("rw_BT", [128, SEQ], BF16); KT = sb("rw_KT", [128, SEQ], BF16); VB = sb("rw_VB", [128, SEQ], BF16)
    tb = sb("rw_tb", [128, SEQ], BF16); sqb = tb
    LW = sb("rw_LW", [128, SEQ], BF16); SG1 = sb("rw_SG1", [128, SEQ], BF16); SG2 = sb("rw_SG2", [32, SEQ], BF16)
    wup = sb("rw_wup", [64, 1024], BF16); aup = sb("rw_aup", [128, 1024], BF16)
    gup1 = sb("rw_gup1", [128, 1024], BF16); gup2 = sb("rw_gup2", [32, 1024], BF16)
    chv = sb("rw_chv", [128, 56]); omka = sb("rw_omka", [128, 8]); glt = sb("rw_glt", [128, 16])
    S.dma("sp", chv[:], D["chv"], "chv", writes=["chv"])
    S.dma("pool", wup[:], D["w_up"], "wup", writes=["wup"])
    S.dma("pool", aup[64:128, :], D["a_up"], "aup", writes=["aup"])
    S.dma("pool", gup1[:], D["g_up"][0:128, :], "gup1", writes=["gup1"])
    S.dma("pool", gup2[:], D["g_up"][128:160, :], "gup2", writes=["gup2"])
    S.op("dve", lambda e: e.tensor_scalar(omka[:], chv[:, 24:32], -1.0, 1.0, ALU.mult, ALU.add), reads=["chv"], writes=["omka"])
    S.op("dve", lambda e: e.memset(raw[:, 0:1], 0.0), writes=["B2"])
    CW0, CA0, CKK, CKA, CGG, CGB, CRK = [8 * i for i in range(7)]

    def proj(tile, dst, dkey, ncols=128):
        def evac(g, pap, pkey):
            S.op("act", lambda e: e.copy(raw[0:ncols, 1 + g * 512:1 + (g + 1) * 512], pap), reads=[pkey], writes=["B2"])
        inproj_fm(k, tile, ps_in, evac, ncols)
        S.op("dve", lambda e: e.tensor_scalar_mul(dst[0:ncols, :], raw[0:ncols, 0:SEQ], k.mu[0:ncols, tile:tile + 1]),
             reads=["B2", "mu"], writes=[dkey])
        S.op("dve", lambda e: e.scalar_tensor_tensor(dst[0:ncols, :], raw[0:ncols, 1:SEQ + 1], k.omm[0:ncols, tile:tile + 1],
                                                     dst[0:ncols, :], ALU.mult, ALU.add),
             reads=["B2", "omm", dkey], writes=[dkey])

    B1 = NB["B1"]
    proj(24, B1, "B1")
    S.op("act", lambda e: e.activation(LW[0:64, :], B1[0:64, :], AF.Tanh), reads=["B1"], writes=["LW"])
    S.op("act", lambda e: e.copy(LW[64:128, :], B1[64:128, :]), reads=["B1"], writes=["LW"])
    proj(25, B1, "B1")
    S.op("act", lambda e: e.activation(SG1[:], B1[:], AF.Sigmoid), reads=["B1"], writes=["SG1"])
    proj(26, B1, "B1", ncols=32)
    S.op("act", lambda e: e.activation(SG2[:], B1[0:32, :], AF.Sigmoid), reads=["B1"], writes=["SG2"])

    def gs(g):
        return slice(g * 512, (g + 1) * 512)

    for u in range(npairs):
        cs = slice(u * 128, (u + 1) * 128)
        Br, Bk, B2, B4, B5, B6, B8, Bo, Bg = (NB[n] for n in ("Br", "Bk", "B2", "B4", "B5", "B6", "B8", "Bo", "Bg"))
        proj(u, Br, "Br"); proj(8 + u, Bk, "Bk"); proj(16 + u, B8, "B8")
        S.op("act", lambda e: e.copy(VB[:], B8[:]), reads=["B8"], writes=["VB"])
        for g in range(4):
            pst, pkey = ps_in[g % 2]
            S.op("pe", lambda e: e.matmul(pst[:], wup[0:64, cs], LW[0:64, gs(g)], start=True, stop=True),
                 reads=["wup", "LW"], writes=[pkey])
            S.op("act", lambda e: e.activation(B1[:, gs(g)], pst[:], AF.Sigmoid, bias=chv[:, CW0 + u:CW0 + u + 1]),
                 reads=[pkey, "chv"], writes=["B1"])
        S.op("dve", lambda e: e.tensor_scalar_mul(B1[:], B1[:], -EXPM05), reads=["B1"], writes=["B1"])
        for n in range(16):
            S.op("dve", lambda e: e.tensor_tensor_scan(B2[:, n * 128:(n + 1) * 128], k.ones_f[:], B1[:, n * 128:(n + 1) * 128],
                                                       0.0, ALU.mult, ALU.add), reads=["B1", "ones_f"], writes=["B2"])
        for g in range(4):
            pst, pkey = ps_in[g % 2]
            S.op("pe", lambda e: e.matmul(pst[:], aup[64:128, cs], LW[64:128, gs(g)], start=True, stop=True),
                 reads=["aup", "LW"], writes=[pkey])
            S.op("act", lambda e: e.activation(B4[:, gs(g)], pst[:], AF.Sigmoid, bias=chv[:, CA0 + u:CA0 + u + 1]),
                 reads=[pkey, "chv"], writes=["B4"])
        S.op("dve", lambda e: e.tensor_scalar_mul(B5[:], Bk[:], chv[:, CKK + u:CKK + u + 1]), reads=["Bk", "chv"], writes=["B5"])
        S.op("pool", lambda e: e.tensor_tensor(sqb[:], B5[:], B5[:], ALU.mult), reads=["B5"], writes=["tb"])
        for g in range(4):
            pst, pkey = ps_in[g % 2]
            S.op("pe", lambda e: e.matmul(pst[:], k.blk_bf[:], sqb[:, gs(g)], start=True, stop=True),
                 reads=["blk_bf", "tb"], writes=[pkey])
            S.op("act", lambda e: e.activation(B6[:, gs(g)], pst[:], AF.Sqrt), reads=[pkey], writes=["B6"])
        S.op("dve", lambda e: e.tensor_scalar_max(B6[:], B6[:], 1e-12), reads=["B6"], writes=["B6"])
        S.op("dve", lambda e: e.reciprocal(B6[:], B6[:]), reads=["B6"], writes=["B6"])
        S.op("dve", lambda e: e.tensor_tensor(B5[:], B5[:], B6[:], ALU.mult), reads=["B5", "B6"], writes=["B5"])
        S.op("dve", lambda e: e.tensor_scalar(B6[:], B4[:], chv[:, CKA + u:CKA + u + 1], omka[:, u:u + 1], ALU.mult, ALU.add),
             reads=["B4", "chv", "omka"], writes=["B6"])
        S.op("pool", lambda e: e.tensor_tensor(Bk[:], Bk[:], B6[:], ALU.mult), reads=["Bk", "B6"], writes=["Bk"])
        S.op("act", lambda e: e.activation(B6[:], B2[:], AF.Exp), reads=["B2"], writes=["B6"])
        S.op("act", lambda e: e.activation(E2[:], B2[:], AF.Exp, scale=-1.0), reads=["B2"], writes=["E2"])
        S.op("dve", lambda e: e.tensor_tensor(B8[:], B2[:], B1[:], ALU.subtract), reads=["B2", "B1"], writes=["B8"])
        S.op("act", lambda e: e.activation(B8[:], B8[:], AF.Exp), reads=["B8"], writes=["B8"])
        S.op("dve", lambda e: e.tensor_copy(glt[:], B6[:].rearrange("p (c l) -> p c l", l=128)[:, :, 127]),
             reads=["B6"], writes=["glt"])
        S.op("pool", lambda e: e.tensor_tensor(RA[:, :, 0:128], Br[:].rearrange("p (c l) -> p c l", l=128),
                                               B6[:].rearrange("p (c l) -> p c l", l=128), ALU.mult),
             reads=["Br", "B6"], writes=["RA"])
        S.op("dve", lambda e: e.scalar_tensor_tensor(RA[:, :, 128:256], B5[:].rearrange("p (c l) -> p c l", l=128), -1.0,
                                                     B8[:].rearrange("p (c l) -> p c l", l=128), ALU.mult, ALU.mult),
             reads=["B5", "B8"], writes=["RA"])
        S.op("dve", lambda e: e.tensor_tensor(B5[:], B5[:], B4[:], ALU.mult), reads=["B5", "B4"], writes=["B5"])
        S.op("pool", lambda e: e.tensor_tensor(BT[:], B5[:], E2[:], ALU.mult), reads=["B5", "E2"], writes=["BT"])
        S.op("dve", lambda e: e.tensor_tensor(KT[:], Bk[:], E2[:], ALU.mult), reads=["Bk", "E2"], writes=["KT"])
        S.op("dve", lambda e: e.scalar_tensor_tensor(tb[:], Br[:], chv[:, CRK + u:CRK + u + 1], Bk[:], ALU.mult, ALU.mult),
             reads=["Br", "Bk", "chv"], writes=["tb"])
        for g in range(4):
            pst, pkey = ps_in[g % 2]
            S.op("pe", lambda e: e.matmul(pst[:], k.blk_bf[:], tb[:, gs(g)], start=True, stop=True),
                 reads=["blk_bf", "tb"], writes=[pkey])
            S.op("dve", lambda e: e.tensor_tensor(Bo[:, gs(g)], pst[:], VB[:, gs(g)], ALU.mult), reads=[pkey, "VB"], writes=["B6"])
        for g in range(4):
            pst, pkey = ps_in[g % 2]
            S.op("pe", lambda e: e.matmul(pst[:], gup1[:, cs], SG1[:, gs(g)], start=True, stop=False),
                 reads=["gup1", "SG1"], writes=[pkey], inc=False)
            S.op("pe", lambda e: e.matmul(pst[:], gup2[:, cs], SG2[:, gs(g)], start=False, stop=True),
                 reads=["gup2", "SG2"], writes=[pkey])
            S.op("act", lambda e: e.copy(Bg[:, gs(g)], pst[:]), reads=[pkey], writes=["B5"])
        S.dma("sp", D["ra_d"][u], RA[:].rearrange("p c n -> p (c n)"), "st_ra", reads=["RA"])
        S.dma("sp", D["bt_d"][u], BT[:], "st_bt", reads=["BT"])
        S.dma("sp", D["kt_d"][u], KT[:], "st_kt", reads=["KT"])
        S.dma("sp", D["vb_d"][u], VB[:], "st_vb", reads=["VB"])
        S.dma("sp", D["bo_d"][u], Bo[:], "st_bo", reads=["B6"])
        S.dma("sp", D["bg_d"][u], Bg[:], "st_bg", reads=["B5"])
        S.dma("sp", D["gl_d"][u], glt[:], "st_gl", reads=["glt"])


def phase_rwkv_scan(k, st2):
    nc, S, D = k.nc, k.S, k.D
    sb = lambda n, s, d=F32: k.sb(n, s, d, st2)
    ps = lambda n, s, d=F32: k.ps(n, s, d, st2)
    npairs = k.cfg.get("rwkv_pairs", 8)
    scm = k.cfg.get("sc_mode", 3)
    P1 = ps("sc_P1", [128, 4, 128]); P2 = ps("sc_P2", [128, 4, 128])
    PA = [ps("sc_PA%d" % i, [128, 4, 128]) for i in range(3)]
    PC = ps("sc_PC", [128, 4, 128])
    PM = [ps("sc_PM%d" % i, [128, 512]) for i in range(2)]
    chv = sb("sc_chv", [128, 56])
    S.dma("sp", chv[:], D["chv"], "chv2", writes=["chv2"])
    CGG, CGB = 32, 40
    RA = [sb("sc_RA%d" % i, [128, 16, 256], BF16) for i in range(2)]
    BT = [sb("sc_BT%d" % i, [128, SEQ], BF16) for i in range(2)]
    KT = [sb("sc_KT%d" % i, [128, SEQ], BF16) for i in range(2)]
    VB = [sb("sc_VB%d" % i, [128, SEQ], BF16) for i in range(2)]
    GL = [sb("sc_GL%d" % i, [128, 16]) for i in range(2)]
    TT = [sb("sc_TT%d" % i, [128, 16, 2, 128], BF16) for i in range(2)]
    ACH = [sb("sc_ACH%d" % i, [128, 16, 2, 3, 128], BF16) for i in range(2)]
    TOK = [sb("sc_TOK%d" % i, [128, 16, 3, 128], BF16) for i in range(2)]
    VPAD = [sb("sc_VPAD%d" % i, [128, 16, 2, 128], BF16) for i in range(2)]
    for i in range(2):
        S.op("pool", lambda e: e.memset(VPAD[i][:], 0.0), writes=[("VPAD", i)])
    Xb = [sb("sc_X%d" % i, [128, 4, 128], BF16) for i in range(2)]
    XTb = [sb("sc_XT%d" % i, [128, 4, 128], BF16) for i in range(2)]
    Pf = sb("sc_Pf", [128, 4, 128]); Pbf = sb("sc_Pbf", [128, 4, 128], BF16)
    Wsb = sb("sc_W", [128, 128], BF16); Ut = sb("sc_Ut", [128, 128], BF16)
    UPAD = [sb("sc_UPAD%d" % i, [128, 2, 128], BF16) for i in range(2)]
    for i in range(2):
        S.op("pool", lambda e: e.memset(UPAD[i][:], 0.0), writes=[("UPAD", i)])
    Sf = sb("sc_Sf", [128, 128]); t1 = sb("sc_t1", [128, 128])
    Sbf = [sb("sc_Sbf%d" % i, [128, 128], BF16) for i in range(2)]
    Yf = sb("sc_Yf", [128, SEQ]); Bo = sb("sc_Bo", [128, SEQ]); Bg = sb("sc_Bg", [128, SEQ])
    yb = sb("sc_yb", [128, SEQ], BF16); yc = sb("sc_yc", [128, SEQ]); rs = Yf
    hb = sb("sc_hb", [128, SEQ], BF16)

    def load(u):
        pp = u % 2
        S.dma("sp", RA[pp][:].rearrange("p c n -> p (c n)"), D["ra_d"][u], "ld_ra%d" % pp, writes=[("RA", pp)])
        S.dma("sp", BT[pp][:], D["bt_d"][u], "ld_bt%d" % pp, writes=[("BT", pp)])
        S.dma("sp", KT[pp][:], D["kt_d"][u], "ld_kt%d" % pp, writes=[("KT", pp)])
        S.dma("sp", VB[pp][:], D["vb_d"][u], "ld_vb%d" % pp, writes=[("VB", pp)])
        S.dma("sp", GL[pp][:], D["gl_d"][u], "ld_gl%d" % pp, writes=[("GL", pp)])

    def stage1(u):
        pp = u % 2
        ra, bt, kt, vb = RA[pp], BT[pp], KT[pp], VB[pp]
        rkeys = [("RA", pp), ("BT", pp), ("KT", pp), ("VB", pp)]
        for cp in range(8):
            sysl = [(2 * ci + hd, ci, hd) for hd in range(2) for ci in range(2)]
            for q, ci, hd in sysl:
                n = 2 * cp + ci
                ph = slice(64 * hd, 64 * hd + 64)
                cn = slice(n * 128, (n + 1) * 128)
                S.op("pe", lambda e: e.matmul(P1[:, q, :], bt[ph, cn], ra[ph, n, 128:256], start=True, stop=True),
                     reads=rkeys, writes=["P1"], inc=(ci == 1), rg=hd)
            for q, ci, hd in sysl:
                n = 2 * cp + ci
                ph = slice(64 * hd, 64 * hd + 64)
                cn = slice(n * 128, (n + 1) * 128)
                S.op("pe", lambda e: e.matmul(P2[:, q, :], ra[ph, n, 128:256], bt[ph, cn], start=True, stop=True),
                     reads=rkeys, writes=["P2"], inc=(ci == 1), rg=hd)
            for kind, (lt, rsl) in enumerate(((kt, slice(128, 256)), (bt, slice(0, 128)), (kt, slice(0, 128)))):
                for q, ci, hd in sysl:
                    n = 2 * cp + ci
                    ph = slice(64 * hd, 64 * hd + 64)
                    cn = slice(n * 128, (n + 1) * 128)
                    S.op("pe", lambda e: e.matmul(PA[kind][:, q, :], lt[ph, cn], ra[ph, n, rsl], start=True, stop=True),
                         reads=rkeys, writes=[("PA", kind)], inc=(ci == 1), rg=hd)
            S.op("dve", lambda e: e.tensor_tensor(Xb[0][:], P1[:], k.ms4[:], ALU.mult), reads=["P1", "ms4"], writes=[("X", 0)])
            S.op("dve", lambda e: e.tensor_tensor(XTb[0][:], P2[:], k.ml4[:], ALU.mult), reads=["P2", "ml4"], writes=[("XT", 0)])
            ach = ACH[pp][:, 2 * cp:2 * cp + 2, :, :, :].rearrange("p c h k t -> p (c h) k t")
            for kind, mk, mkey in ((0, k.ms4, "ms4"), (1, k.mi4, "mi4"), (2, k.mi4, "mi4")):
                S.op("dve" if kind != 1 else "dve", lambda e: e.tensor_tensor(ach[:, :, kind, :], PA[kind][:], mk[:], ALU.mult),
                     reads=[("PA", kind), mkey], writes=[("ACH", pp, cp)])
            S.op("dve", lambda e: e.tensor_tensor(Pf[:], Xb[0][:], k.I4[:], ALU.add), reads=[("X", 0), "I4"], writes=["Pf"])
            S.op("act", lambda e: e.copy(Pbf[:], Pf[:]), reads=["Pf"], writes=["Pbf"])
            if scm < 1:
                yield
                continue
            pT = PM[0][:].bitcast(BF16)
            for ci in range(2):
                n = 2 * cp + ci
                cn = slice(n * 128, (n + 1) * 128)
                for j, src in enumerate((bt, kt, vb)):
                    S.op("pe", lambda e: e.transpose(pT[:, (ci * 3 + j) * 128:(ci * 3 + j + 1) * 128], src[:, cn], k.ident_bf[:]),
                         reads=rkeys + ["ident_bf"], writes=[("PM", 0)], inc=(ci == 1 and j == 2))
            S.op("act", lambda e: e.copy(TOK[pp][:, 2 * cp:2 * cp + 2, :, :].rearrange("p c j t -> p (c j t)"), pT[:, 0:768]),
                 reads=[("PM", 0)], writes=[("TOK", pp, cp)])
            pT3 = pT[:, 0:768].rearrange("p (c j t) -> p c j t", c=2, j=3)
            for hd in range(2):
                S.op("act", lambda e: e.copy(VPAD[pp][:, 2 * cp:2 * cp + 2, hd, 64 * hd:64 * hd + 64], pT3[:, :, 2, 64 * hd:64 * hd + 64]),
                     reads=[("PM", 0)], writes=[("VPAD", pp)])
            yield
            for lv in range(1, 7 if scm >= 2 else 1):
                xi, xo = (lv - 1) % 2, lv % 2
                if lv < 6:
                    for q in range(4):
                        S.op("pe", lambda e: e.matmul(PA[0][:, q, :], XTb[xi][:, q, :], Xb[xi][:, q, :], start=True, stop=True),
                             reads=[("X", xi), ("XT", xi)], writes=[("PA", 0)], inc=(q == 3))
                for q in range(4):
                    S.op("pe", lambda e: e.matmul(PA[1][:, q, :], Xb[xi][:, q, :], XTb[xi][:, q, :], start=True, stop=True),
                         reads=[("X", xi), ("XT", xi)], writes=[("PA", 1)], inc=(q == 3))
                if lv < 6:
                    S.op("act", lambda e: e.copy(Xb[xo][:], PA[0][:]), reads=[("PA", 0)], writes=[("X", xo)])
                S.op("dve", lambda e: e.tensor_copy(XTb[xo][:], PA[1][:]), reads=[("PA", 1)], writes=[("XT", xo)])
                yield
                for q in range(4):
                    S.op("pe", lambda e: e.matmul(PA[2][:, q, :], XTb[xo][:, q, :], Pbf[:, q, :], start=True, stop=True),
                         reads=[("XT", xo), "Pbf"], writes=[("PA", 2)], inc=(q == 3))
                S.op("dve", lambda e: e.tensor_tensor(Pf[:], Pf[:], PA[2][:], ALU.add), reads=["Pf", ("PA", 2)], writes=["Pf"])
                if lv < 6:
                    S.op("act", lambda e: e.copy(Pbf[:], Pf[:]), reads=["Pf"], writes=["Pbf"])
                else:
                    S.op("act", lambda e: e.copy(TT[pp][:, 2 * cp:2 * cp + 2, :, :].rearrange("p c h t -> p (c h) t"), Pf[:]),
                         reads=["Pf"], writes=[("TT", pp, cp)])
                yield

    def chain(u):
        pp = u % 2
        ra = RA[pp]
        if scm < 3:
            return
        S.op("dve", lambda e: e.memset(Sf[:], 0.0), writes=["Sf"])
        S.op("dve", lambda e: e.memset(Sbf[0][:], 0.0), writes=[("Sbf", 0)])
        for n in range(16):
            cp, ci = n // 2, n % 2
            si, so = n % 2, (n + 1) % 2
            cn = slice(n * 128, (n + 1) * 128)
            akey, tkey, ttkey = ("ACH", pp, cp), ("TOK", pp, cp), ("TT", pp, cp)
            S.op("pe", lambda e: e.matmul(PC[:, 0, :], ra[:, n, 128:256], Sbf[si][:], start=True, stop=False),
                 reads=[("RA", pp), ("Sbf", si)], writes=["PC"], inc=False)
            for hd in range(2):
                hs = slice(64 * hd, 64 * hd + 64)
                S.op("pe", lambda e: e.matmul(PC[:, 0, hs], ACH[pp][:, n, hd, 0, :], TOK[pp][:, n, 2, hs], start=False, stop=(hd == 1)),
                     reads=[akey, tkey], writes=["PC"], inc=(hd == 1))
            S.op("act", lambda e: e.copy(Wsb[:], PC[:, 0, :]), reads=["PC"], writes=["Wsb"])
            yield
            import os
            sub = int(os.environ.get("SCSUB", "9"))
            if sub <= 1:
                continue
            for hd in range(2):
                hs = slice(64 * hd, 64 * hd + 64)
                S.op("pe", lambda e: e.matmul(PC[:, 1, hs], TT[pp][:, n, hd, :], Wsb[:, hs], start=True, stop=True),
                     reads=[ttkey, "Wsb"], writes=["PC"], inc=(hd == 1))
            S.op("dve", lambda e: e.tensor_copy(Ut[:], PC[:, 1, :]), reads=["PC"], writes=["Ut"])
            for hd in range(2 if os.environ.get("NOUPAD") is None else 0):
                hs = slice(64 * hd, 64 * hd + 64)
                S.op("dve", lambda e: e.tensor_copy(UPAD[ci][:, hd, hs], PC[:, 1, hs]), reads=["PC"], writes=[("UPAD", ci)])
            yield
            if sub <= 2:
                continue
            PY = PM[1][:, 0:128]
            S.op("pe", lambda e: e.matmul(PY, Sbf[si][:], ra[:, n, 0:128], start=True, stop=False),
                 reads=[("RA", pp), ("Sbf", si)], writes=[("PM", 1)], inc=False)
            for hd in range(2):
                S.op("pe", lambda e: e.matmul(PY, UPAD[ci][:, hd, :], ACH[pp][:, n, hd, 1, :], start=False, stop=False),
                     reads=[("UPAD", ci), akey], writes=[("PM", 1)], inc=False)
                S.op("pe", lambda e: e.matmul(PY, VPAD[pp][:, n, hd, :], ACH[pp][:, n, hd, 2, :], start=False, stop=(hd == 1)),
                     reads=[("VPAD", pp), akey], writes=[("PM", 1)], inc=(hd == 1))
            if sub <= 3:
                S.op("act", lambda e: e.copy(Yf[:, cn], PY), reads=[("PM", 1)], writes=["Yf"])
                continue
            S.op("pe", lambda e: e.matmul(PC[:, 3, :], TOK[pp][:, n, 0, :], Ut[:], start=True, stop=False),
                 reads=[tkey, "Ut"], writes=["PC"], inc=False)
            S.op("pe", lambda e: e.matmul(PC[:, 3, :], TOK[pp][:, n, 1, :], TOK[pp][:, n, 2, :], start=False, stop=True),
                 reads=[tkey], writes=["PC"])
            S.op("act", lambda e: e.copy(Yf[:, cn], PY), reads=[("PM", 1)], writes=["Yf"])
            S.op("dve", lambda e: e.scalar_tensor_tensor(t1[:], PC[:, 3, :], GL[pp][:, n:n + 1], k.blk_f[:], ALU.mult, ALU.mult),
                 reads=["PC", ("GL", pp), "blk_f"], writes=["t1"])
            S.op("dve", lambda e: e.scalar_tensor_tensor(Sf[:], Sf[:], GL[pp][:, n:n + 1], t1[:], ALU.mult, ALU.add),
                 reads=["Sf", "t1", ("GL", pp)], writes=["Sf"])
            S.op("act", lambda e: e.copy(Sbf[so][:], Sf[:]), reads=["Sf"], writes=[("Sbf", so)])
            yield
        if sub <= 4:
            return
        S.dma("sp", Bo[:], D["bo_d"][u], "ld_bo", writes=["Bo"])
        S.dma("sp", Bg[:], D["bg_d"][u], "ld_bg", writes=["Bg"])
        S.op("act", lambda e: e.copy(yb[:], Yf[:]), reads=["Yf"], writes=["yb"])
        for g in range(4):
            gsl = slice(g * 512, (g + 1) * 512)
            S.op("pe", lambda e: e.matmul(PM[1][:], k.blk_bf[:], yb[:, gsl], start=True, stop=True),
                 reads=["blk_bf", "yb"], writes=[("PM", 1)])
            S.op("dve", lambda e: e.scalar_tensor_tensor(yc[:, gsl], PM[1][:], -1.0 / 64, Yf[:, gsl], ALU.mult, ALU.add),
                 reads=[("PM", 1), "Yf"], writes=["yc"])
        S.op("pool", lambda e: e.tensor_tensor(yb[:], yc[:], yc[:], ALU.mult), reads=["yc"], writes=["yb"])
        for g in range(4):
            gsl = slice(g * 512, (g + 1) * 512)
            S.op("pe", lambda e: e.matmul(PM[1][:], k.blk_bf[:], yb[:, gsl], start=True, stop=True),
                 reads=["blk_bf", "yb"], writes=[("PM", 1)])
            S.op("act", lambda e: e.activation(rs[:, gsl], PM[1][:], AF.Sqrt, bias=k.eps_gn[:], scale=1.0 / 64),
                 reads=[("PM", 1), "eps_gn"], writes=["Yf"])
        S.op("dve", lambda e: e.reciprocal(rs[:], rs[:]), reads=["Yf"], writes=["Yf"])
        S.op("dve", lambda e: e.tensor_tensor(yc[:], yc[:], rs[:], ALU.mult), reads=["yc", "Yf"], writes=["yc"])
        S.op("dve", lambda e: e.tensor_scalar(yc[:], yc[:], chv[:, CGG + u:CGG + u + 1], chv[:, CGB + u:CGB + u + 1], ALU.mult, ALU.add),
             reads=["yc", "chv2"], writes=["yc"])
        S.op("pool", lambda e: e.tensor_tensor(yc[:], yc[:], Bo[:], ALU.add), reads=["yc", "Bo"], writes=["yc"])
        S.op("dve", lambda e: e.tensor_tensor(hb[:], yc[:], Bg[:], ALU.mult), reads=["yc", "Bg"], writes=["hb"])
        S.dma("sp", D["hT_d"][u], hb[:], "st_hb", reads=["hb"])
        yield

    def drive(gens):
        gens = [g for g in gens if g is not None]
        while gens:
            for g in list(gens):
                try:
                    next(g)
                except StopIteration:
                    gens.remove(g)

    load(0)
    if scm < 0:
        return
    drive([stage1(0)])
    for u in range(npairs):
        if u + 1 < npairs:
            load(u + 1)
        drive([chain(u), stage1(u + 1) if u + 1 < npairs else None])


def layer_norm_rows(k, z, zkey, gbc, bbc, gbkey, out, okey, tmp):
    S, nc = k.S, k.nc
    st6, mv, rstd, nmr = tmp
    for i in range(4):
        S.op("dve", lambda e: e.bn_stats(st6[:, i, :], z[:, i * 512:(i + 1) * 512]), reads=[zkey], writes=["ln_st6"])
    S.op("dve", lambda e: e.bn_aggr(mv[:], st6[:].rearrange("p a b -> p (a b)")), reads=["ln_st6"], writes=["ln_mv"])
    S.op("act", lambda e: e.activation(rstd[:], mv[:, 1:2], AF.Sqrt, bias=k.eps_ln[:], scale=1.0), reads=["ln_mv", "eps_ln"], writes=["ln_rstd"])
    S.op("dve", lambda e: e.reciprocal(rstd[:], rstd[:]), reads=["ln_rstd"], writes=["ln_rstd"])
    S.op("dve", lambda e: e.scalar_tensor_tensor(nmr[:], mv[:, 0:1], -1.0, rstd[:], ALU.mult, ALU.mult),
         reads=["ln_mv", "ln_rstd"], writes=["ln_nmr"])
    S.op("act", lambda e: e.activation(z[:], z[:], AF.Identity, bias=nmr[:], scale=rstd[:]),
         reads=[zkey, "ln_nmr", "ln_rstd"], writes=[zkey])
    S.op("dve", lambda e: e.tensor_tensor(z[:], z[:], gbc[:], ALU.mult), reads=[zkey, gbkey], writes=[zkey])
    S.op("pool", lambda e: e.tensor_tensor(out[:], z[:], bbc[:], ALU.add), reads=[zkey, gbkey], writes=[okey])


def phase_b(k):
    nc, S, D = k.nc, k.S, k.D
    sb = lambda n, s, d=F32: k.sb(n, s, d, k.pst)
    ps = lambda n, s, d=F32: k.ps(n, s, d, k.pst)
    nchunks = k.cfg.get("b_chunks", 16)
    hTs = sb("b_hT", [128, 16, SEQ], BF16)
    wout = sb("b_wout", [128, 16, DM], BF16)
    for i in range(4):
        S.dma("sp", hTs[:, 4 * i:4 * i + 4, :], D["hT_d"][4 * i:4 * i + 4].rearrange("c p t -> p c t"), "b_hT%d" % i, writes=[("hT", i)])
    wov = D["w_out"].rearrange("(c p) n -> p c n", p=128)
    for i in range(4):
        S.dma("pool", wout[:, 4 * i:4 * i + 4, :], wov[:, 4 * i:4 * i + 4, :], "b_wout%d" % i, writes=[("wout", i)])
    gbc = sb("b_gbc", [128, DM]); bbc = sb("b_bbc", [128, DM])
    S.dma("sp", gbc[:], D["ln1"][0].partition_broadcast(128), "b_gbc", writes=["gb1"])
    S.dma("sp", bbc[:], D["ln1"][1].partition_broadcast(128), "b_bbc", writes=["gb1"])
    wr = sb("b_wr", [128, 16, NE]); brt = sb("b_brt", [128, NE])
    S.dma("sp", wr[:], D["w_router"].rearrange("(c p) e -> p c e", p=128), "b_wr", writes=["wr"])
    S.dma("sp", brt[:], D["b_router"][0].partition_broadcast(128), "b_brt", writes=["brt"])
    k.eps_ln = sb("eps_ln", [128, 1])
    S.op("dve", lambda e: e.memset(k.eps_ln[:], 1e-5), writes=["eps_ln"])
    ecap = sb("b_ecap", [128, NE])
    S.op("dve", lambda e: e.tensor_scalar_mul(ecap[:], k.iota_e[:], float(CAP)), reads=["iota_e"], writes=["ecap"])
    carry = sb("b_carry", [128, NE])
    S.op("dve", lambda e: e.memset(carry[:], 0.0), writes=["carry"])
    xc = [sb("b_xc%d" % i, [128, DM]) for i in range(2)]
    z = sb("b_z", [128, DM]); x1 = sb("b_x1", [128, DM]); x1T = sb("b_x1T", [128, 16, 128]); x1b = sb("b_x1b", [128, DM], BF16)
    lntmp = (sb("ln_st6", [128, 4, 6]), sb("ln_mv", [128, 2]), sb("ln_rstd", [128, 1]), sb("ln_nmr", [128, 1]))
    lg = sb("b_lg", [128, NE]); t8 = sb("b_t8", [128, 8]); oh = sb("b_oh", [128, 4, NE]); ma = sb("b_ma", [128, NE])
    mab = sb("b_mab", [128, NE], BF16); negm = sb("b_negm", [128, 1]); ev = sb("b_ev", [128, 4]); esum = sb("b_esum", [128, 1])
    pe_ = sb("b_pe", [128, NE]); ohp = sb("b_ohp", [128, 4, NE]); destf = sb("b_destf", [128, 4]); posk = sb("b_posk", [128, 4])
    ovf = sb("b_ovf", [128, 4])
    psM = [ps("b_psM%d" % i, [128, 512]) for i in range(4)]
    psT = [ps("b_psT%d" % i, [128, 4, 128]) for i in range(2)]
    psL = ps("b_psL", [128, 512])
    for c in range(nchunks):
        tc_ = slice(c * 128, (c + 1) * 128)
        xcb = xc[c % 2]
        S.dma("sp", xcb[:], D["x"][tc_, :], "b_xc%d" % (c % 2), writes=[("xc", c % 2)])
        for nb in range(4):
            for fc in range(16):
                S.op("pe", lambda e: e.matmul(psM[nb][:], hTs[:, fc, tc_], wout[:, fc, nb * 512:(nb + 1) * 512],
                                              start=(fc == 0), stop=(fc == 15)),
                     reads=[("hT", fc // 4), ("wout", fc // 4)], writes=[("psM", nb)], inc=(fc == 15))
            S.op("dve", lambda e: e.scalar_tensor_tensor(z[:, nb * 512:(nb + 1) * 512], xcb[:, nb * 512:(nb + 1) * 512], ALPHA,
                                                         psM[nb][:], ALU.mult, ALU.add),
                 reads=[("psM", nb), ("xc", c % 2)], writes=["z"])
        layer_norm_rows(k, z, "z", gbc, bbc, "gb1", x1, "x1", lntmp)
        S.dma("sp", D["x1f_d"][tc_, :], x1[:], "b_x1st", reads=["x1"])
        S.op("act", lambda e: e.copy(x1b[:], x1[:]), reads=["x1"], writes=["x1b"])
        for r in range(4):
            pt = psT[r % 2]
            for i in range(4):
                dc = 4 * r + i
                S.op("pe", lambda e: e.transpose(pt[:, i, :], x1[:, dc * 128:(dc + 1) * 128], k.ident_f[:]),
                     reads=["x1", "ident_f"], writes=[("psT", r % 2)], inc=(i == 3))
            S.op("act", lambda e: e.copy(x1T[:, 4 * r:4 * r + 4, :], pt[:]), reads=[("psT", r % 2)], writes=["x1T"])
        for dc in range(16):
            S.op("pe", lambda e: e.matmul(psL[:, 0:NE], x1T[:, dc, :], wr[:, dc, :], start=(dc == 0), stop=(dc == 15)),
                 reads=["x1T", "wr"], writes=["psL"], inc=(dc == 15))
        S.op("dve", lambda e: e.tensor_tensor(lg[:], psL[:, 0:NE], brt[:], ALU.add), reads=["psL", "brt"], writes=["lg"])
        S.op("dve", lambda e: e.max(t8[:], lg[:]), reads=["lg"], writes=["t8"])
        for kk in range(4):
            S.op("dve", lambda e: e.tensor_scalar(oh[:, kk, :], lg[:], t8[:, kk:kk + 1], None, ALU.is_equal),
                 reads=["lg", "t8"], writes=["oh"])
        S.op("dve", lambda e: e.reduce_sum(ma[:], oh[:].rearrange("p k e -> p e k"), AX.X), reads=["oh"], writes=["ma"])
        S.op("dve", lambda e: e.tensor_copy(mab[:], ma[:]), reads=["ma"], writes=["mab"])
        S.op("dve", lambda e: e.tensor_scalar_mul(negm[:], t8[:, 0:1], -1.0), reads=["t8"], writes=["negm"])
        S.op("act", lambda e: e.activation(ev[:], t8[:, 0:4], AF.Exp, bias=negm[:], scale=1.0), reads=["t8", "negm"], writes=["ev"])
        S.op("dve", lambda e: e.reduce_sum(esum[:], ev[:], AX.X), reads=["ev"], writes=["esum"])
        S.op("dve", lambda e: e.reciprocal(esum[:], esum[:]), reads=["esum"], writes=["esum"])
        S.op("dve", lambda e: e.tensor_scalar_mul(k.gates_all[:, c, :], ev[:], esum[:, 0:1]), reads=["ev", "esum"], writes=["gates_all"])
        S.op("pe", lambda e: e.matmul(psL[:, 32:64], k.m_strict_bf[:], mab[:], start=True, stop=True),
             reads=["m_strict_bf", "mab"], writes=["psL"], inc=False)
        S.op("pe", lambda e: e.matmul(psL[:, 64:96], k.ones_bf[:], mab[:], start=True, stop=True),
             reads=["ones_bf", "mab"], writes=["psL"])
        S.op("dve", lambda e: e.tensor_tensor(pe_[:], psL[:, 32:64], carry[:], ALU.add), reads=["psL", "carry"], writes=["pe"])
        S.op("dve", lambda e: e.tensor_tensor(carry[:], psL[:, 64:96], carry[:], ALU.add), reads=["psL", "carry"], writes=["carry"])
        for kk in range(4):
            S.op("dve", lambda e: e.tensor_tensor(ohp[:, kk, :], oh[:, kk, :], pe_[:], ALU.mult), reads=["oh", "pe"], writes=["ohp"])
        S.op("dve", lambda e: e.reduce_sum(posk[:], ohp[:], AX.X), reads=["ohp"], writes=["posk"])
        for kk in range(4):
            S.op("dve", lambda e: e.tensor_tensor(ohp[:, kk, :], oh[:, kk, :], ecap[:], ALU.mult), reads=["oh", "ecap"], writes=["ohp"])
        S.op("dve", lambda e: e.reduce_sum(destf[:], ohp[:], AX.X), reads=["ohp"], writes=["destf"])
        S.op("dve", lambda e: e.tensor_scalar_min(posk[:], posk[:], float(CAP - 1)), reads=["posk"], writes=["posk"])
        S.op("dve", lambda e: e.tensor_tensor(destf[:], destf[:], posk[:], ALU.add), reads=["destf", "posk"], writes=["destf"])
        S.op("dve", lambda e: e.tensor_copy(k.dest_all[:, c, :], destf[:]), reads=["destf"], writes=["dest_all"])
        for kk in range(4):
            S.dma("pool", D["xdisp_d"][:, :], x1b[:], "b_disp", reads=["x1b", "dest_all", "xdisp"],
                  indirect=(bass.IndirectOffsetOnAxis(ap=k.dest_all[:, c, kk:kk + 1], axis=0), None))


def phase_c(k):
    nc, S, D = k.nc, k.S, k.D
    sb = lambda n, s, d=F32: k.sb(n, s, d, k.pst)
    ps = lambda n, s, d=F32: k.ps(n, s, d, k.pst)
    nexp = k.cfg.get("c_experts", NE)
    NJ = CAP // 128
    xtok = sb("c_xtok", [128, NJ, DM], BF16)
    xeT = [sb("c_xeT%d" % i, [128, 16, CAP], BF16) for i in range(2)]
    wg = [sb("c_wg%d" % i, [128, 16, 512], BF16) for i in range(3)]
    NWG = 3
    wd = [sb("c_wd%d" % i, [128, 16, 512], BF16) for i in range(2)]
    gg = sb("c_gg", [128, 4, CAP]); gsb = sb("c_gsb", [128, CAP]); sg = sb("c_sg", [128, CAP]); usb = sb("c_usb", [128, CAP])
    actT = sb("c_actT", [128, 16, CAP], BF16)
    ysb = [sb("c_ysb%d" % i, [128, 512]) for i in range(4)]
    nys = [0]
    bdn = sb("c_bdn", [128, DM])
    bgu = sb("c_bgu", [128, NE, 32])
    S.dma("sp", bgu[:].rearrange("p e f -> p (e f)"), D["b_gu_r"], "c_bgu", writes=["bgu"])
    psT = [ps("c_psT%d" % i, [128, 512]) for i in range(2)]
    psG = [ps("c_psG%d" % i, [128, 512]) for i in range(3)]
    psD = [ps("c_psD%d" % i, [128, 512]) for i in range(3)]
    nwg = [0]; nwd = [0]; npg = [0]; npd = [0]

    def load_expert_inputs(e):
        S.dma("sp", xtok[:], D["xdisp_d"][e * CAP:(e + 1) * CAP, :].rearrange("(j p) d -> p j d", p=128), "c_xtok",
              reads=["xdisp"], writes=["xtok"])

    def transposes(e):
        xe = xeT[e % 2]
        xkey = ("xeT", e % 2)
        tn = 0
        for j in range(NJ):
            for r in range(2):
                pt = psT[tn % 2]
                ptb = pt[:].bitcast(BF16)
                for i in range(8):
                    dc = 8 * r + i
                    S.op("pe", lambda e_: e_.transpose(ptb[:, i * 128:(i + 1) * 128], xtok[:, j, dc * 128:(dc + 1) * 128], k.ident_bf[:]),
                         reads=["xtok", "ident_bf"], writes=[("c_psT", tn % 2)], inc=(i == 7))
                S.op("act", lambda e_: e_.copy(xe[:, 8 * r:8 * r + 8, j * 128:(j + 1) * 128],
                                               ptb[:].rearrange("p (i s) -> p i s", s=128)),
                     reads=[("c_psT", tn % 2)], writes=[xkey])
                tn += 1

    load_expert_inputs(0)
    transposes(0)
    for e in range(nexp):
        S.dma("sp", bdn[:], D["b_dn"][e].partition_broadcast(128), "c_bdn", writes=["bdn"])
        if e + 1 < nexp:
            load_expert_inputs(e + 1)
        xe = xeT[e % 2]
        xkey = ("xeT", e % 2)
        for t in range(4):
            for half in range(2):
                w = wg[nwg[0] % 3]; wkey = ("wg", nwg[0] % 3); nwg[0] += 1
                c0 = half * DM + t * 512
                S.dma("pool", w[:], D["w_gu"][e, :, c0:c0 + 512].rearrange("(c p) n -> p c n", p=128), "c_wg%d" % ((nwg[0] - 1) % 3),
                      writes=[wkey])
                for fbi in range(4):
                    fb = t * 4 + fbi
                    pg = psG[npg[0] % 3]; pgkey = ("psG", npg[0] % 3); npg[0] += 1
                    for dc in range(16):
                        S.op("pe", lambda e_: e_.matmul(pg[:, 0:CAP], w[:, dc, fbi * 128:(fbi + 1) * 128], xe[:, dc, :],
                                                        start=(dc == 0), stop=(dc == 15)),
                             reads=[wkey, xkey], writes=[pgkey], inc=(dc == 15))
                    bcol = bgu[:, e, half * 16 + fb:half * 16 + fb + 1]
                    if half == 0:
                        S.op("dve", lambda e_: e_.tensor_scalar(gsb[:], pg[:, 0:CAP], bcol, 7.0, ALU.add, ALU.min),
                             reads=[pgkey, "bgu"], writes=["gsb"])
                        S.op("act", lambda e_: e_.activation(sg[:], gsb[:], AF.Sigmoid, scale=1.702), reads=["gsb"], writes=["sg"])
                        S.op("dve", lambda e_: e_.tensor_tensor(gg[:, fbi, :], gsb[:], sg[:], ALU.mult), reads=["gsb", "sg"], writes=[("gg", fbi)])
                    else:
                        S.op("dve", lambda e_: e_.tensor_scalar(usb[:], pg[:, 0:CAP], bcol, 7.0, ALU.add, ALU.min),
                             reads=[pgkey, "bgu"], writes=["usb"])
                        S.op("dve", lambda e_: e_.tensor_scalar(usb[:], usb[:], -7.0, 1.0, ALU.max, ALU.add), reads=["usb"], writes=["usb"])
                        S.op("dve", lambda e_: e_.tensor_tensor(actT[:, fb, :], usb[:], gg[:, fbi, :], ALU.mult),
                             reads=["usb", ("gg", fbi)], writes=["actT"])
        if e + 1 < nexp:
            transposes(e + 1)
        for db in range(4):
            w = wd[nwd[0] % 2]; wkey = ("wd", nwd[0] % 2); nwd[0] += 1
            S.dma("pool", w[:], D["w_dn"][e, :, db * 512:(db + 1) * 512].rearrange("(c p) n -> p c n", p=128), "c_wd%d" % ((nwd[0] - 1) % 2),
                  writes=[wkey])
            for j in range(NJ):
                pd = psD[npd[0] % 3]; pdkey = ("psD", npd[0] % 3); npd[0] += 1
                for fc in range(16):
                    S.op("pe", lambda e_: e_.matmul(pd[:], actT[:, fc, j * 128:(j + 1) * 128], w[:, fc, :], start=(fc == 0), stop=(fc == 15)),
                         reads=["actT", wkey], writes=[pdkey], inc=(fc == 15))
                yi = nys[0] % 4; nys[0] += 1
                S.op("dve", lambda e_: e_.tensor_tensor(ysb[yi][:], pd[:], bdn[:, db * 512:(db + 1) * 512], ALU.add),
                     reads=[pdkey, "bdn"], writes=[("ysb", yi)])
                S.dma("sp", D["y_d"][e * CAP + j * 128:e * CAP + (j + 1) * 128, db * 512:(db + 1) * 512], ysb[yi][:], "c_yst%d" % yi,
                      reads=[("ysb", yi)])


def phase_d(k):
    nc, S, D = k.nc, k.S, k.D
    sb = lambda n, s, d=F32: k.sb(n, s, d, k.pst)
    nchunks = k.cfg.get("b_chunks", 16)
    gbc = sb("d_gbc", [128, DM]); bbc = sb("d_bbc", [128, DM])
    S.dma("sp", gbc[:], D["ln2"][0].partition_broadcast(128), "d_gbc", writes=["gb2"])
    S.dma("sp", bbc[:], D["ln2"][1].partition_broadcast(128), "d_bbc", writes=["gb2"])
    if not hasattr(k, "eps_ln"):
        k.eps_ln = sb("eps_ln", [128, 1])
    else:
        k.eps_ln = sb("eps_ln2", [128, 1])
    S.op("dve", lambda e: e.memset(k.eps_ln[:], 1e-5), writes=["eps_ln"])
    lntmp = (sb("ln2_st6", [128, 4, 6]), sb("ln2_mv", [128, 2]), sb("ln2_rstd", [128, 1]), sb("ln2_nmr", [128, 1]))
    x1 = [sb("d_x1_%d" % i, [128, DM]) for i in range(2)]
    yk = [[sb("d_y%d_%d" % (i, kk), [128, DM]) for kk in range(4)] for i in range(2)]
    acc = sb("d_acc", [128, DM]); ob = [sb("d_ob%d" % i, [128, DM]) for i in range(2)]
    for c in range(nchunks):
        tc_ = slice(c * 128, (c + 1) * 128)
        b = c % 2
        S.dma("sp", x1[b][:], D["x1f_d"][tc_, :], "d_x1_%d" % b, writes=[("dx1", b)])
        for kk in range(4):
            S.dma("pool", yk[b][kk][:], D["y_d"][:, :], "d_y%d_%d" % (b, kk), reads=["dest_all"], writes=[("dy", b, kk)],
                  indirect=(None, bass.IndirectOffsetOnAxis(ap=k.dest_all[:, c, kk:kk + 1], axis=0)))
        S.op("dve", lambda e: e.tensor_scalar_mul(acc[:], x1[b][:], ALPHA), reads=[("dx1", b)], writes=["acc"])
        for kk in range(4):
            S.op("dve", lambda e: e.scalar_tensor_tensor(acc[:], yk[b][kk][:], k.gates_all[:, c, kk:kk + 1], acc[:], ALU.mult, ALU.add),
                 reads=[("dy", b, kk), "gates_all", "acc"], writes=["acc"])
        layer_norm_rows(k, acc, "acc", gbc, bbc, "gb2", ob[b], ("ob", b), lntmp)
        S.dma("sp", D["out"][tc_, :], ob[b][:], "d_ob%d" % b, reads=[("ob", b)])


def prep_shared(inp):
    f = lambda a: np.ascontiguousarray(a, dtype=np.float32)
    w_in = inp["w_in"][0]

    def fm_tile(c0, n=128):
        t = np.zeros((2048, 128), np.float32)
        t[:, :n] = w_in[:, c0:c0 + n]
        return t.reshape(16, 128, 128).transpose(1, 0, 2).reshape(128, 2048)
    cols = [(128 * u, 128) for u in range(24)] + [(3072, 128), (3200, 128), (3328, 32)]
    cols += [(3360 + 128 * h, 128) for h in range(8)] + [(4384 + 128 * h, 128) for h in range(8)]
    w_fm = np.stack([fm_tile(c, n) for c, n in cols])
    w_v = np.stack([w_in[:, 5408 + 512 * g:5408 + 512 * (g + 1)].reshape(16, 128, 512).transpose(1, 0, 2).reshape(128, 8192)
                    for g in range(2)])
    mu = np.zeros(27 * 128, np.float32)
    mu[:3360] = inp["shift_mu"][0]
    chv = np.concatenate([inp[n][0].reshape(8, 128).T for n in ("w0", "a0", "k_k", "k_a", "gn_g", "gn_b", "r_k")], axis=1)
    sh = {
        "w_fm": f(w_fm), "w_v": f(w_v), "mu": f(mu.reshape(27, 128).T), "chv": f(chv),
        "w_up": f(inp["w_up"][0]), "a_up": f(inp["a_up"][0]), "g_up": f(inp["g_up"][0]),
        "lqk": f(np.concatenate([inp["lq1"][0], inp["lk1"][0], inp["lq2"][0], inp["lk2"][0]])[None, :]),
        "subln": f(inp["subln_g"][0][:, None]),
        "w_out": f(inp["w_out"][0]), "ln1": f(np.stack([inp["ln1_g"][0], inp["ln1_b"][0]])),
        "ln2": f(np.stack([inp["ln2_g"][0], inp["ln2_b"][0]])),
        "w_router": f(inp["w_router"][0]), "b_router": f(inp["b_router"][0][None, :]),
        "w_gu": f(inp["w_gu"][0]), "b_gu_r": f(inp["b_gu"][0].reshape(32, 32, 128).transpose(2, 0, 1).reshape(128, 1024)),
        "w_dn": f(inp["w_dn"][0]), "b_dn": f(inp["b_dn"][0]),
        "zeros": np.zeros((CAP, DM), dtype=ml_dtypes.bfloat16),
    }
    return sh


def prep_core(inp, b):
    xb = np.asarray(inp["x"][b], dtype=np.float32)
    return {"xT": np.ascontiguousarray(xb.T), "x": np.ascontiguousarray(xb)}


def kernel(**inputs):
    nc, _ = build()
    sh = prep_shared(inputs)
    in_maps = [dict(sh, **prep_core(inputs, b)) for b in range(8)]
    res = run_bass_kernel_spmd(nc, in_maps, core_ids=list(range(8)))
    return np.stack([np.asarray(r["out"], dtype=np.float32) for r in res.results])
```

```python
import math
from contextlib import ExitStack
import numpy as np
import ml_dtypes
import concourse.bass as bass
import concourse.mybir as mybir
from concourse.bass_utils import run_bass_kernel_spmd

F32 = mybir.dt.float32
BF16 = mybir.dt.bfloat16
I32 = mybir.dt.int32
U32 = mybir.dt.uint32
AF = mybir.ActivationFunctionType
ALU = mybir.AluOpType
AX = mybir.AxisListType

SEQ = 2048
DM = 2048
NE = 32
CAP = 384
ALPHA = 2.0 ** 0.25
LAM_INIT = 0.8 - 0.6
SCALE = 64 ** -0.5
EXPM05 = math.exp(-0.5)
N_FM = 43


class Sched:
    def __init__(self, nc, stack):
        self.nc = nc
        self.stack = stack
        self.engs = {"pe": nc.tensor, "act": nc.scalar, "dve": nc.vector,
                     "pool": nc.gpsimd, "sp": nc.sync}
        self.sems = {}
        self.cnt = {}
        self.waited = {e: {} for e in self.engs}
        self.last_write = {}
        self.readers = {}
        for e in ("pe", "act", "dve", "pool"):
            self._sem("E_" + e)
        self.n_ins = 0
        self.last_rg = None
        self.last_pe_inc = True

    def _sem(self, name):
        if name not in self.sems:
            self.sems[name] = self.stack.enter_context(self.nc.semaphore(name))
            self.cnt[name] = 0
        return self.sems[name]

    def _emit_waits(self, eng, reads, writes):
        need = {}

        def add(tok, same_ok):
            if tok is None:
                return
            s, v = tok
            if same_ok and s == "E_" + eng and eng == "pe":
                return
            if need.get(s, 0) < v:
                need[s] = v
        for k in reads:
            add(self.last_write.get(k), False)
        for k in writes:
            add(self.last_write.get(k), True)
            for t in self.readers.get(k, ()):
                add(t, True)
        e = self.engs[eng]
        for s, v in need.items():
            if self.waited[eng].get(s, 0) >= v:
                continue
            e.wait_ge(self.sems[s], v)
            self.waited[eng][s] = v
            self.n_ins += 1

    def _record(self, tok, reads, writes):
        for k in reads:
            self.readers.setdefault(k, []).append(tok)
        for k in writes:
            self.last_write[k] = tok
            self.readers[k] = []

    def op(self, eng, fn, reads=(), writes=(), inc=True, rg=None):
        if eng == "pe":
            if rg is not None and self.last_rg is not None and rg != self.last_rg:
                assert self.last_pe_inc
                self.engs["pe"].wait_ge(self.sems["E_pe"], self.cnt["E_pe"])
                self.n_ins += 1
            self.last_rg = rg
            self.last_pe_inc = inc
        self._emit_waits(eng, reads, writes)
        ins = fn(self.engs[eng])
        self.n_ins += 1
        s = "E_" + eng
        if inc:
            self.cnt[s] += 1
            ins.then_inc(self.sems[s], 1)
            tok = (s, self.cnt[s])
        else:
            tok = (s, self.cnt[s] + 1)
        self._record(tok, reads, writes)
        return ins

    def dma(self, q, out, in_, semkey, reads=(), writes=(), indirect=None, **kw):
        self._emit_waits(q, reads, writes)
        s = "D_" + semkey
        self._sem(s)
        if indirect is None:
            ins = self.engs[q].dma_start(out=out, in_=in_, **kw)
        else:
            ins = self.engs[q].indirect_dma_start(out, indirect[0], in_, indirect[1], **kw)
        self.n_ins += 1
        self.cnt[s] += 16
        ins.then_inc(self.sems[s], 16)
        tok = (s, self.cnt[s])
        self._record(tok, reads, writes)
        return ins

    def barrier(self):
        for eng in self.engs:
            e = self.engs[eng]
            for s, v in self.cnt.items():
                if v == 0 or s == "E_" + eng:
                    continue
                if self.waited[eng].get(s, 0) >= v:
                    continue
                e.wait_ge(self.sems[s], v)
                self.waited[eng][s] = v
                self.n_ins += 1
        self.last_write = {}
        self.readers = {}


class K:
    pass


def build(cfg=None):
    cfg = cfg or {}
    phases = cfg.get("phases", "ABCD")
    dbg = cfg.get("dbg", ())
    nc = bass.Bass("TRN2", target_bir_lowering=False)
    k = K()
    k.nc, k.cfg, k.dbg = nc, cfg, dbg
    D = {}
    k.D = D

    def din(name, shape, dt=F32):
        D[name] = nc.dram_tensor(name, list(shape), dt, kind="ExternalInput").ap()

    def dout(name, shape, dt=F32):
        D[name] = nc.dram_tensor(name, list(shape), dt, kind="ExternalOutput").ap()

    def dscr(name, shape, dt):
        if name in cfg.get("ext_in", ()):
            D[name] = nc.dram_tensor(name, list(shape), dt, kind="ExternalInput").ap()
        elif name in cfg.get("ext", ()):
            D[name] = nc.dram_tensor(name, list(shape), dt, kind="ExternalOutput").ap()
        else:
            D[name] = nc.dram_tensor(name, list(shape), dt).ap()
    k.dout = dout

    din("xT", [DM, SEQ]); din("x", [SEQ, DM])
    din("w_fm", [N_FM, 128, 2048]); din("w_v", [2, 128, 16 * 512])
    din("mu", [128, 27]); din("chv", [128, 56])
    din("w_up", [64, 1024]); din("a_up", [64, 1024]); din("g_up", [160, 1024])
    din("lqk", [1, 256]); din("subln", [128, 1])
    din("w_out", [DM, DM]); din("ln1", [2, DM]); din("ln2", [2, DM])
    din("w_router", [DM, NE]); din("b_router", [1, NE])
    din("w_gu", [cfg.get("ne_decl", NE), DM, 2 * DM]); din("b_gu_r", [128, NE * 32])
    din("w_dn", [cfg.get("ne_decl", NE), DM, DM]); din("b_dn", [NE, DM])
    din("zeros", [CAP, DM], BF16)
    dout("out", [SEQ, DM])
    dscr("hT_d", [16, 128, SEQ], BF16)
    dscr("ra_d", [8, 128, 4096], BF16); dscr("bt_d", [8, 128, SEQ], BF16); dscr("kt_d", [8, 128, SEQ], BF16)
    dscr("vb_d", [8, 128, SEQ], BF16); dscr("bo_d", [8, 128, SEQ], F32); dscr("bg_d", [8, 128, SEQ], F32)
    dscr("gl_d", [8, 128, 16], F32)
    dscr("x1f_d", [SEQ, DM], F32)
    dscr("xdisp_d", [NE * CAP, DM], BF16)
    dscr("y_d", [NE * CAP, DM], F32)

    with ExitStack() as st:
        k.st = st
        k.S = Sched(nc, st)

        def sb(name, shape, dt=F32, stack=None):
            return (stack or st).enter_context(nc.sbuf_tensor(name, list(shape), dt))

        def ps(name, shape, dt=F32, stack=None):
            return (stack or st).enter_context(nc.psum_tensor(name, list(shape), dt))
        k.sb, k.ps = sb, ps
        setup_consts(k)
        k.dest_all = sb("dest_all", [128, 16, 4], I32)
        k.gates_all = sb("gates_all", [128, 16, 4], F32)
        k.S.op("dve", lambda e: e.memset(k.dest_all[:], 0), writes=["dest_all"])
        k.S.op("dve", lambda e: e.memset(k.gates_all[:], 0.0), writes=["gates_all"])
        if "A" in phases:
            with ExitStack() as pst:
                k.pst = pst
                if cfg.get("pro", True):
                    phase_a_setup(k)
                if cfg.get("rwkv", True) and cfg.get("pro", True):
                    with ExitStack() as st2:
                        phase_rwkv_pro(k, st2)
                    k.S.barrier()
                if cfg.get("diff", True):
                    with ExitStack() as st2:
                        phase_diff(k, st2)
            k.S.barrier()
            if cfg.get("rwkv", True) and cfg.get("scan", True):
                with ExitStack() as st2:
                    phase_rwkv_scan(k, st2)
                k.S.barrier()
        if "B" in phases:
            with ExitStack() as pst:
                k.pst = pst
                phase_b(k)
            k.S.barrier()
        if "route" in dbg:
            dout("dbg_dest", [128, 64], I32); dout("dbg_gates", [128, 64], F32)
            k.S.dma("sp", D["dbg_dest"], k.dest_all[:].rearrange("p c k -> p (c k)"), "dbg_dest", reads=["dest_all"])
            k.S.dma("sp", D["dbg_gates"], k.gates_all[:].rearrange("p c k -> p (c k)"), "dbg_gates", reads=["gates_all"])
            k.S.barrier()
        if "C" in phases:
            with ExitStack() as pst:
                k.pst = pst
                phase_c(k)
            k.S.barrier()
        if "D" in phases:
            with ExitStack() as pst:
                k.pst = pst
                phase_d(k)
        k.S.barrier()
    k.n_ins = k.S.n_ins
    return nc, k


def setup_consts(k):
    nc, S, sb = k.nc, k.S, k.sb
    k.iota_jp = sb("iota_jp", [128, 512], F32)
    S.op("pool", lambda e: e.iota(k.iota_jp[:], [[1, 512]], base=0, channel_multiplier=-1,
                                  allow_small_or_imprecise_dtypes=True), writes=["iota_jp"])
    k.ident_bf = sb("ident_bf", [128, 128], BF16)
    k.ident_f = sb("ident_f", [128, 128], F32)
    k.m_incl = sb("m_incl", [128, 128], F32)
    k.m_strict = sb("m_strict", [128, 128], F32)
    k.m_incl_bf = sb("m_incl_bf", [128, 128], BF16)
    k.m_strict_bf = sb("m_strict_bf", [128, 128], BF16)
    k.ones_bf = sb("ones_bf", [128, 128], BF16)
    k.ones_f = sb("ones_f", [128, 128], F32)
    k.blk_bf = sb("blk_bf", [128, 128], BF16)
    k.blk_f = sb("blk_f", [128, 128], F32)
    k.iota_e = sb("iota_e", [128, NE], F32)
    ij = k.iota_jp[:, 0:128]
    for t, op, key in ((k.ident_bf, ALU.is_equal, "ident_bf"), (k.ident_f, ALU.is_equal, "ident_f"),
                       (k.m_incl, ALU.is_ge, "m_incl"), (k.m_strict, ALU.is_gt, "m_strict"),
                       (k.m_incl_bf, ALU.is_ge, "m_incl_bf"), (k.m_strict_bf, ALU.is_gt, "m_strict_bf")):
        S.op("dve", lambda e, t=t, op=op: e.tensor_single_scalar(t[:], ij, 0.0, op),
             reads=["iota_jp"], writes=[key])
    S.op("dve", lambda e: e.memset(k.ones_bf[:], 1.0), writes=["ones_bf"])
    S.op("dve", lambda e: e.memset(k.ones_f[:], 1.0), writes=["ones_f"])
    for t, key in ((k.blk_bf, "blk_bf"), (k.blk_f, "blk_f")):
        S.op("dve", lambda e, t=t: e.memset(t[:], 0.0), writes=[key])
        S.op("dve", lambda e, t=t: e.memset(t[0:64, 0:64], 1.0), writes=[key])
        S.op("dve", lambda e, t=t: e.memset(t[64:128, 64:128], 1.0), writes=[key])
    S.op("pool", lambda e: e.iota(k.iota_e[:], [[1, NE]], base=0, channel_multiplier=0,
                                  allow_small_or_imprecise_dtypes=True), writes=["iota_e"])
    k.iota4 = sb("iota4", [128, 4, 128], F32)
    S.op("pool", lambda e: e.iota(k.iota4[:], [[0, 4], [1, 128]], base=0, channel_multiplier=-1,
                                  allow_small_or_imprecise_dtypes=True), writes=["iota4"])
    k.ms4 = sb("ms4", [128, 4, 128], BF16)
    k.ml4 = sb("ml4", [128, 4, 128], BF16)
    k.mi4 = sb("mi4", [128, 4, 128], BF16)
    k.I4 = sb("I4", [128, 4, 128], F32)
    for t, op, key in ((k.ms4, ALU.is_gt, "ms4"), (k.ml4, ALU.is_lt, "ml4"), (k.mi4, ALU.is_ge, "mi4"), (k.I4, ALU.is_equal, "I4")):
        S.op("dve", lambda e, t=t, op=op: e.tensor_single_scalar(t[:], k.iota4[:], 0.0, op),
             reads=["iota4"], writes=[key])
    k.eps_gn = sb("eps_gn", [128, 1], F32)
    S.op("dve", lambda e: e.memset(k.eps_gn[:], 64e-5), writes=["eps_gn"])
    k.eps_rms = sb("eps_rms", [128, 1], F32)
    S.op("dve", lambda e: e.memset(k.eps_rms[:], 1e-5), writes=["eps"])


def phase_a_setup(k):
    nc, S, D = k.nc, k.S, k.D
    sb = lambda n, s, d=F32: k.sb(n, s, d, k.pst)
    k.xT = sb("xT_sb", [128, 16, SEQ], BF16)
    xv = D["xT"].rearrange("(c p) t -> p c t", p=128)
    for i in range(4):
        S.dma("pool", k.xT[:, 4 * i:4 * i + 4, :], xv[:, 4 * i:4 * i + 4, :], "xT%d" % i, writes=[("xT", i)])
    k.xT_keys = [("xT", i) for i in range(4)]
    k.wfm = [sb("wfm%d" % i, [128, 16, 128], BF16) for i in range(2)]
    k.wfm_n = 0
    k.mu = sb("mu_sb", [128, 27]); k.omm = sb("omm_sb", [128, 27])
    S.dma("sp", k.mu[:], D["mu"], "mu", writes=["mu"])
    S.op("dve", lambda e: e.tensor_scalar(k.omm[:], k.mu[:], -1.0, 1.0, ALU.mult, ALU.add),
         reads=["mu"], writes=["omm"])


def load_wfm(k, tile):
    S, D = k.S, k.D
    i = k.wfm_n % 2
    k.wfm_n += 1
    S.dma("pool", k.wfm[i][:], D["w_fm"][tile].rearrange("p (c n) -> p c n", n=128), "wfm%d" % i,
          writes=[("wfm", i)])
    return k.wfm[i], ("wfm", i)


def inproj_fm(k, tile, ps_pair, evac, ncols=128):
    S = k.S
    wt, wkey = load_wfm(k, tile)
    for g in range(4):
        pst, pkey = ps_pair[g % 2]
        for dc in range(16):
            S.op("pe", lambda e: e.matmul(pst[0:ncols, :], wt[:, dc, 0:ncols], k.xT[:, dc, g * 512:(g + 1) * 512],
                                          start=(dc == 0), stop=(dc == 15)),
                 reads=[wkey, ("xT", dc // 4)], writes=[pkey], inc=(dc == 15))
        evac(g, pst[0:ncols, :], pkey)


def phase_diff(k, st2):
    nc, S, D = k.nc, k.S, k.D
    sb = lambda n, s, d=F32: k.sb(n, s, d, st2)
    ps = lambda n, s, d=F32: k.ps(n, s, d, st2)
    ps_s = [(ps("dps_s%d" % i, [128, 512]), ("dps_s", i)) for i in range(3)]
    ps_ms = (ps("dps_ms", [128, 512]), ("dps_ms", 0))
    ps_in = ps_s[0:2]
    ps_o = [(ps("dps_o%d" % i, [128, 512]), ("dps_o", i)) for i in range(2)]
    ps_l = [(ps("dps_l%d" % i, [128, 512]), ("dps_l", i)) for i in range(2)]
    qk = [sb("qk%d" % i, [128, 2, SEQ], BF16) for i in range(2)]
    v4 = sb("v4", [128, 16, 512], BF16)
    wv = sb("wv", [128, 16, 512], BF16)
    pT = [sb("pT%d" % i, [128, 512], BF16) for i in range(3)]
    rl = sb("rl", [128, 512]); o0 = sb("o0", [128, 512]); o1 = sb("o1", [128, 512]); oo = sb("oo", [128, 512])
    sq = sb("sq", [128, 512], BF16); rstd = sb("rstd", [128, 512])
    hst = [sb("hst%d" % i, [128, 512], BF16) for i in range(2)]
    lqk = sb("lqk_sb", [128, 256]); prod = sb("lprod", [128, 128]); s12 = sb("ls12", [128, 2]); e12 = sb("le12", [128, 2])
    nlam = sb("nlam", [128, 1]); sgs = sb("sgs", [128, 1]); sgin = sb("sgin", [128, 1])
    S.dma("sp", lqk[:], D["lqk"][0].partition_broadcast(128), "lqk", writes=["lqk"])
    S.dma("sp", sgin[:], D["subln"], "sgin", writes=["sgin"])
    S.op("dve", lambda e: e.tensor_tensor(prod[:, 0:64], lqk[:, 0:64], lqk[:, 64:128], ALU.mult), reads=["lqk"], writes=["lprod"])
    S.op("dve", lambda e: e.tensor_tensor(prod[:, 64:128], lqk[:, 128:192], lqk[:, 192:256], ALU.mult), reads=["lqk"], writes=["lprod"])
    S.op("dve", lambda e: e.reduce_sum(s12[:], prod[:].rearrange("p (a n) -> p a n", a=2), AX.X), reads=["lprod"], writes=["ls12"])
    S.op("act", lambda e: e.activation(e12[:], s12[:], AF.Exp), reads=["ls12"], writes=["le12"])
    S.op("dve", lambda e: e.tensor_tensor(nlam[:], e12[:, 1:2], e12[:, 0:1], ALU.subtract), reads=["le12"], writes=["nlam"])
    S.op("dve", lambda e: e.tensor_scalar_add(nlam[:], nlam[:], -LAM_INIT), reads=["nlam"], writes=["nlam"])
    S.op("dve", lambda e: e.tensor_scalar_mul(sgs[:], sgin[:], 1.0 - LAM_INIT), reads=["sgin"], writes=["sgs"])

    if k.cfg.get("zfill", True):
        for e_ in range(NE):
            S.dma("sp", D["xdisp_d"][e_ * CAP:(e_ + 1) * CAP, :], D["zeros"], "zfill", writes=["xdisp"])
    for h in range(k.cfg.get("diff_heads", 8)):
        hh = h % 4
        if hh == 0:
            S.dma("pool", wv[:], D["w_v"][h // 4].rearrange("p (c n) -> p c n", n=512), "wv", writes=["wv"])
            for tc in range(16):
                pst, pkey = ps_in[tc % 2]
                for dc in range(16):
                    S.op("pe", lambda e: e.matmul(pst[:], k.xT[:, dc, tc * 128:(tc + 1) * 128], wv[:, dc, :],
                                                  start=(dc == 0), stop=(dc == 15)),
                         reads=["wv", ("xT", dc // 4)], writes=[pkey], inc=(dc == 15))
                S.op("act", lambda e: e.copy(v4[:, tc, :], pst[:]), reads=[pkey], writes=[("v4", tc)])
        qkb = qk[h % 2]
        qkey = ("qk", h % 2)
        for which, tile in ((0, 27 + h), (1, 35 + h)):
            def evac(g, pap, pkey, which=which):
                S.op("act", lambda e: e.copy(qkb[:, which, g * 512:(g + 1) * 512], pap), reads=[pkey], writes=[qkey])
            inproj_fm(k, tile, ps_in, evac)
        tiles = [(m, g, j) for g in range(4) for m in range(2) for j in range(4 * g + 4)]

        def emit_qk(n):
            m, g, j = tiles[n]
            i = j - 4 * g
            q0 = 128 * i if i > 0 else 0
            pst, pkey = ps_s[n % 3]
            S.op("pe", lambda e: e.matmul(pst[:, q0:512], qkb[64 * m:64 * m + 64, 1, j * 128:(j + 1) * 128],
                                          qkb[64 * m:64 * m + 64, 0, g * 512 + q0:(g + 1) * 512], start=True, stop=True),
                 reads=[qkey], writes=[pkey], rg=m)
        emit_qk(0)
        emit_qk(1)
        for n, (m, g, j) in enumerate(tiles):
            if n + 2 < len(tiles):
                emit_qk(n + 2)
            i = j - 4 * g
            q0 = 128 * i if i > 0 else 0
            pst, pkey = ps_s[n % 3]
            pt = pT[n % 3]
            ptk = ("pT", n % 3)
            acc = (2 * g + m) % 2
            S.op("act", lambda e: e.activation(pt[:, q0:512], pst[:, q0:512], AF.Exp, scale=SCALE), reads=[pkey], writes=[ptk])
            if i >= 0:
                S.op("pool", lambda e: e.tensor_tensor(pt[:, q0:q0 + 128], pt[:, q0:q0 + 128], k.m_incl_bf[:], ALU.mult),
                     reads=[ptk, "m_incl_bf"], writes=[ptk])
            last = (j == 4 * g + 3)
            S.op("pe", lambda e: e.matmul(ps_o[acc][0][:, q0:512], v4[:, j, hh * 128:(hh + 1) * 128], pt[:, q0:512],
                                          start=(j == 0), stop=last),
                 reads=[ptk, ("v4", j)], writes=[ps_o[acc][1]], inc=False)
            S.op("pe", lambda e: e.matmul(ps_l[acc][0][:, q0:512], k.ones_bf[:], pt[:, q0:512], start=(j == 0), stop=last),
                 reads=[ptk, "ones_bf"], writes=[ps_l[acc][1]], inc=True)
            if last:
                S.op("dve", lambda e: e.reciprocal(rl[:], ps_l[acc][0][:]), reads=[ps_l[acc][1]], writes=["rl"])
                if m == 0:
                    S.op("dve", lambda e: e.tensor_tensor(o0[:], ps_o[acc][0][:], rl[:], ALU.mult),
                         reads=[ps_o[acc][1], "rl"], writes=["o0"])
                else:
                    S.op("dve", lambda e: e.tensor_tensor(o1[:], ps_o[acc][0][:], rl[:], ALU.mult),
                         reads=[ps_o[acc][1], "rl"], writes=["o1"])
                    S.op("dve", lambda e: e.scalar_tensor_tensor(oo[:], o1[:], nlam[:], o0[:], ALU.mult, ALU.add),
                         reads=["o0", "o1", "nlam"], writes=["oo"])
                    S.op("pool", lambda e: e.tensor_tensor(sq[:], oo[:], oo[:], ALU.mult), reads=["oo"], writes=["sq"])
                    mp, mkey = ps_ms
                    S.op("pe", lambda e: e.matmul(mp[:], k.ones_bf[:], sq[:], start=True, stop=True),
                         reads=["sq", "ones_bf"], writes=[mkey])
                    S.op("act", lambda e: e.activation(rstd[:], mp[:], AF.Sqrt, bias=k.eps_rms[:], scale=1.0 / 128),
                         reads=[mkey, "eps"], writes=["rstd"])
                    S.op("dve", lambda e: e.reciprocal(rstd[:], rstd[:]), reads=["rstd"], writes=["rstd"])
                    hs = hst[g % 2]
                    S.op("dve", lambda e: e.scalar_tensor_tensor(hs[:], oo[:], sgs[:], rstd[:], ALU.mult, ALU.mult),
                         reads=["oo", "sgs", "rstd"], writes=[("hst", g % 2)])
                    S.dma("sp", D["hT_d"][8 + h, :, g * 512:(g + 1) * 512], hs[:], "hst%d" % (g % 2), reads=[("hst", g % 2)])


def phase_rwkv_pro(k, st2):
    nc, S, D = k.nc, k.S, k.D
    sb = lambda n, s, d=F32: k.sb(n, s, d, st2)
    ps = lambda n, s, d=F32: k.ps(n, s, d, st2)
    npairs = k.cfg.get("rwkv_pairs", 8)
    ps_in = [(ps("rps_in%d" % i, [128, 512]), ("rps_in", i)) for i in range(2)]
    NB = {}
    for n in ("Br", "Bk", "B1", "B4", "B5", "B6", "B8"):
        NB[n] = sb("rw_" + n, [128, SEQ])
    raw = sb("rw_raw", [128, SEQ + 4])
    NB["B2"] = raw[:, 1:SEQ + 1]
    NB["Bo"] = NB["B6"]; NB["Bg"] = NB["B5"]
    E2 = sb("rw_E2", [128, SEQ])
    RA = sb("rw_RA", [128, 16, 256], BF16)
    BT = sb("rw_BT", [128, SEQ], BF16); KT = sb("rw_KT", [128, SEQ], BF16); VB = sb("rw_VB", [128, SEQ], BF16)
    tb = sb("rw_tb", [128, SEQ], BF16); sqb = tb
    LW = sb("rw_LW", [128, SEQ], BF16); SG1 = sb("rw_SG1", [128, SEQ], BF16); SG2 = sb("rw_SG2", [32, SEQ], BF16)
    wup = sb("rw_wup", [64, 1024], BF16); aup = sb("rw_aup", [128, 1024], BF16)
    gup1 = sb("rw_gup1", [128, 1024], BF16); gup2 = sb("rw_gup2", [32, 1024], BF16)
    chv = sb("rw_chv", [128, 56]); omka = sb("rw_omka", [128, 8]); glt = sb("rw_glt", [128, 16])
    S.dma("sp", chv[:], D["chv"], "chv", writes=["chv"])
    S.dma("pool", wup[:], D["w_up"], "wup", writes=["wup"])
    S.dma("pool", aup[64:128, :], D["a_up"], "aup", writes=["aup"])
    S.dma("pool", gup1[:], D["g_up"][0:128, :], "gup1", writes=["gup1"])
    S.dma("pool", gup2[:], D["g_up"][128:160, :], "gup2", writes=["gup2"])
    S.op("dve", lambda e: e.tensor_scalar(omka[:], chv[:, 24:32], -1.0, 1.0, ALU.mult, ALU.add), reads=["chv"], writes=["omka"])
    S.op("dve", lambda e: e.memset(raw[:, 0:1], 0.0), writes=["B2"])
    CW0, CA0, CKK, CKA, CGG, CGB, CRK = [8 * i for i in range(7)]

    def proj(tile, dst, dkey, ncols=128):
        def evac(g, pap, pkey):
            S.op("act", lambda e: e.copy(raw[0:ncols, 1 + g * 512:1 + (g + 1) * 512], pap), reads=[pkey], writes=["B2"])
        inproj_fm(k, tile, ps_in, evac, ncols)
        S.op("dve", lambda e: e.tensor_scalar_mul(dst[0:ncols, :], raw[0:ncols, 0:SEQ], k.mu[0:ncols, tile:tile + 1]),
             reads=["B2", "mu"], writes=[dkey])
        S.op("dve", lambda e: e.scalar_tensor_tensor(dst[0:ncols, :], raw[0:ncols, 1:SEQ + 1], k.omm[0:ncols, tile:tile + 1],
                                                     dst[0:ncols, :], ALU.mult, ALU.add),
             reads=["B2", "omm", dkey], writes=[dkey])

    B1 = NB["B1"]
    proj(24, B1, "B1")
    S.op("act", lambda e: e.activation(LW[0:64, :], B1[0:64, :], AF.Tanh), reads=["B1"], writes=["LW"])
    S.op("act", lambda e: e.copy(LW[64:128, :], B1[64:128, :]), reads=["B1"], writes=["LW"])
    proj(25, B1, "B1")
    S.op("act", lambda e: e.activation(SG1[:], B1[:], AF.Sigmoid), reads=["B1"], writes=["SG1"])
    proj(26, B1, "B1", ncols=32)
    S.op("act", lambda e: e.activation(SG2[:], B1[0:32, :], AF.Sigmoid), reads=["B1"], writes=["SG2"])

    def gs(g):
        return slice(g * 512, (g + 1) * 512)

    for u in range(npairs):
        cs = slice(u * 128, (u + 1) * 128)
        Br, Bk, B2, B4, B5, B6, B8, Bo, Bg = (NB[n] for n in ("Br", "Bk", "B2", "B4", "B5", "B6", "B8", "Bo", "Bg"))
        proj(u, Br, "Br"); proj(8 + u, Bk, "Bk"); proj(16 + u, B8, "B8")
        S.op("act", lambda e: e.copy(VB[:], B8[:]), reads=["B8"], writes=["VB"])
        for g in range(4):
            pst, pkey = ps_in[g % 2]
            S.op("pe", lambda e: e.matmul(pst[:], wup[0:64, cs], LW[0:64, gs(g)], start=True, stop=True),
                 reads=["wup", "LW"], writes=[pkey])
            S.op("act", lambda e: e.activation(B1[:, gs(g)], pst[:], AF.Sigmoid, bias=chv[:, CW0 + u:CW0 + u + 1]),
                 reads=[pkey, "chv"], writes=["B1"])
        S.op("dve", lambda e: e.tensor_scalar_mul(B1[:], B1[:], -EXPM05), reads=["B1"], writes=["B1"])
        for n in range(16):
            S.op("dve", lambda e: e.tensor_tensor_scan(B2[:, n * 128:(n + 1) * 128], k.ones_f[:], B1[:, n * 128:(n + 1) * 128],
                                                       0.0, ALU.mult, ALU.add), reads=["B1", "ones_f"], writes=["B2"])
        for g in range(4):
            pst, pkey = ps_in[g % 2]
            S.op("pe", lambda e: e.matmul(pst[:], aup[64:128, cs], LW[64:128, gs(g)], start=True, stop=True),
                 reads=["aup", "LW"], writes=[pkey])
            S.op("act", lambda e: e.activation(B4[:, gs(g)], pst[:], AF.Sigmoid, bias=chv[:, CA0 + u:CA0 + u + 1]),
                 reads=[pkey, "chv"], writes=["B4"])
        S.op("dve", lambda e: e.tensor_scalar_mul(B5[:], Bk[:], chv[:, CKK + u:CKK + u + 1]), reads=["Bk", "chv"], writes=["B5"])
        S.op("pool", lambda e: e.tensor_tensor(sqb[:], B5[:], B5[:], ALU.mult), reads=["B5"], writes=["tb"])
        for g in range(4):
            pst, pkey = ps_in[g % 2]
            S.op("pe", lambda e: e.matmul(pst[:], k.blk_bf[:], sqb[:, gs(g)], start=True, stop=True),
                 reads=["blk_bf", "tb"], writes=[pkey])
            S.op("act", lambda e: e.activation(B6[:, gs(g)], pst[:], AF.Sqrt), reads=[pkey], writes=["B6"])
        S.op("dve", lambda e: e.tensor_scalar_max(B6[:], B6[:], 1e-12), reads=["B6"], writes=["B6"])
        S.op("dve", lambda e: e.reciprocal(B6[:], B6[:]), reads=["B6"], writes=["B6"])
        S.op("dve", lambda e: e.tensor_tensor(B5[:], B5[:], B6[:], ALU.mult), reads=["B5", "B6"], writes=["B5"])
        S.op("dve", lambda e: e.tensor_scalar(B6[:], B4[:], chv[:, CKA + u:CKA + u + 1], omka[:, u:u + 1], ALU.mult, ALU.add),
             reads=["B4", "chv", "omka"], writes=["B6"])
        S.op("pool", lambda e: e.tensor_tensor(Bk[:], Bk[:], B6[:], ALU.mult), reads=["Bk", "B6"], writes=["Bk"])
        S.op("act", lambda e: e.activation(B6[:], B2[:], AF.Exp), reads=["B2"], writes=["B6"])
        S.op("act", lambda e: e.activation(E2[:], B2[:], AF.Exp, scale=-1.0), reads=["B2"], writes=["E2"])
        S.op("dve", lambda e: e.tensor_tensor(B8[:], B2[:], B1[:], ALU.subtract), reads=["B2", "B1"], writes=["B8"])
        S.op("act", lambda e: e.activation(B8[:], B8[:], AF.Exp), reads=["B8"], writes=["B8"])
        S.op("dve", lambda e: e.tensor_copy(glt[:], B6[:].rearrange("p (c l) -> p c l", l=128)[:, :, 127]),
             reads=["B6"], writes=["glt"])
        S.op("pool", lambda e: e.tensor_tensor(RA[:, :, 0:128], Br[:].rearrange("p (c l) -> p c l", l=128),
                                               B6[:].rearrange("p (c l) -> p c l", l=128), ALU.mult),
             reads=["Br", "B6"], writes=["RA"])
        S.op("dve", lambda e: e.scalar_tensor_tensor(RA[:, :, 128:256], B5[:].rearrange("p (c l) -> p c l", l=128), -1.0,
                                                     B8[:].rearrange("p (c l) -> p c l", l=128), ALU.mult, ALU.mult),
             reads=["B5", "B8"], writes=["RA"])
        S.op("dve", lambda e: e.tensor_tensor(B5[:], B5[:], B4[:], ALU.mult), reads=["B5", "B4"], writes=["B5"])
        S.op("pool", lambda e: e.tensor_tensor(BT[:], B5[:], E2[:], ALU.mult), reads=["B5", "E2"], writes=["BT"])
        S.op("dve", lambda e: e.tensor_tensor(KT[:], Bk[:], E2[:], ALU.mult), reads=["Bk", "E2"], writes=["KT"])
        S.op("dve", lambda e: e.scalar_tensor_tensor(tb[:], Br[:], chv[:, CRK + u:CRK + u + 1], Bk[:], ALU.mult, ALU.mult),
             reads=["Br", "Bk", "chv"], writes=["tb"])
        for g in range(4):
            pst, pkey = ps_in[g % 2]
            S.op("pe", lambda e: e.matmul(pst[:], k.blk_bf[:], tb[:, gs(g)], start=True, stop=True),
                 reads=["blk_bf", "tb"], writes=[pkey])
            S.op("dve", lambda e: e.tensor_tensor(Bo[:, gs(g)], pst[:], VB[:, gs(g)], ALU.mult), reads=[pkey, "VB"], writes=["B6"])
        for g in range(4):
            pst, pkey = ps_in[g % 2]
            S.op("pe", lambda e: e.matmul(pst[:], gup1[:, cs], SG1[:, gs(g)], start=True, stop=False),
                 reads=["gup1", "SG1"], writes=[pkey], inc=False)
            S.op("pe", lambda e: e.matmul(pst[:], gup2[:, cs], SG2[:, gs(g)], start=False, stop=True),
                 reads=["gup2", "SG2"], writes=[pkey])
            S.op("act", lambda e: e.copy(Bg[:, gs(g)], pst[:]), reads=[pkey], writes=["B5"])
        S.dma("sp", D["ra_d"][u], RA[:].rearrange("p c n -> p (c n)"), "st_ra", reads=["RA"])
        S.dma("sp", D["bt_d"][u], BT[:], "st_bt", reads=["BT"])
        S.dma("sp", D["kt_d"][u], KT[:], "st_kt", reads=["KT"])
        S.dma("sp", D["vb_d"][u], VB[:], "st_vb", reads=["VB"])
        S.dma("sp", D["bo_d"][u], Bo[:], "st_bo", reads=["B6"])
        S.dma("sp", D["bg_d"][u], Bg[:], "st_bg", reads=["B5"])
        S.dma("sp", D["gl_d"][u], glt[:], "st_gl", reads=["glt"])


def phase_rwkv_scan(k, st2):
    nc, S, D = k.nc, k.S, k.D
    sb = lambda n, s, d=F32: k.sb(n, s, d, st2)
    ps = lambda n, s, d=F32: k.ps(n, s, d, st2)
    npairs = k.cfg.get("rwkv_pairs", 8)
    scm = k.cfg.get("sc_mode", 3)
    P1 = ps("sc_P1", [128, 4, 128]); P2 = ps("sc_P2", [128, 4, 128])
    PA = [ps("sc_PA%d" % i, [128, 4, 128]) for i in range(3)]
    PC = ps("sc_PC", [128, 4, 128])
    PM = [ps("sc_PM%d" % i, [128, 512]) for i in range(2)]
    chv = sb("sc_chv", [128, 56])
    S.dma("sp", chv[:], D["chv"], "chv2", writes=["chv2"])
    CGG, CGB = 32, 40
    RA = [sb("sc_RA%d" % i, [128, 16, 256], BF16) for i in range(2)]
    BT = [sb("sc_BT%d" % i, [128, SEQ], BF16) for i in range(2)]
    KT = [sb("sc_KT%d" % i, [128, SEQ], BF16) for i in range(2)]
    VB = [sb("sc_VB%d" % i, [128, SEQ], BF16) for i in range(2)]
    GL = [sb("sc_GL%d" % i, [128, 16]) for i in range(2)]
    TT = [sb("sc_TT%d" % i, [128, 16, 2, 128], BF16) for i in range(2)]
    ACH = [sb("sc_ACH%d" % i, [128, 16, 2, 3, 128], BF16) for i in range(2)]
    TOK = [sb("sc_TOK%d" % i, [128, 16, 3, 128], BF16) for i in range(2)]
    VPAD = [sb("sc_VPAD%d" % i, [128, 16, 2, 128], BF16) for i in range(2)]
    for i in range(2):
        S.op("pool", lambda e: e.memset(VPAD[i][:], 0.0), writes=[("VPAD", i)])
    Xb = [sb("sc_X%d" % i, [128, 4, 128], BF16) for i in range(2)]
    XTb = [sb("sc_XT%d" % i, [128, 4, 128], BF16) for i in range(2)]
    Pf = sb("sc_Pf", [128, 4, 128]); Pbf = sb("sc_Pbf", [128, 4, 128], BF16)
    Wsb = sb("sc_W", [128, 128], BF16); Ut = sb("sc_Ut", [128, 128], BF16)
    UPAD = [sb("sc_UPAD%d" % i, [128, 2, 128], BF16) for i in range(2)]
    for i in range(2):
        S.op("pool", lambda e: e.memset(UPAD[i][:], 0.0), writes=[("UPAD", i)])
    Sf = sb("sc_Sf", [128, 128]); t1 = sb("sc_t1", [128, 128])
    Sbf = [sb("sc_Sbf%d" % i, [128, 128], BF16) for i in range(2)]
    Yf = sb("sc_Yf", [128, SEQ]); Bo = sb("sc_Bo", [128, SEQ]); Bg = sb("sc_Bg", [128, SEQ])
    yb = sb("sc_yb", [128, SEQ], BF16); yc = sb("sc_yc", [128, SEQ]); rs = Yf
    hb = sb("sc_hb", [128, SEQ], BF16)

    def load(u):
        pp = u % 2
        S.dma("sp", RA[pp][:].rearrange("p c n -> p (c n)"), D["ra_d"][u], "ld_ra%d" % pp, writes=[("RA", pp)])
        S.dma("sp", BT[pp][:], D["bt_d"][u], "ld_bt%d" % pp, writes=[("BT", pp)])
        S.dma("sp", KT[pp][:], D["kt_d"][u], "ld_kt%d" % pp, writes=[("KT", pp)])
        S.dma("sp", VB[pp][:], D["vb_d"][u], "ld_vb%d" % pp, writes=[("VB", pp)])
        S.dma("sp", GL[pp][:], D["gl_d"][u], "ld_gl%d" % pp, writes=[("GL", pp)])

    def stage1(u):
        pp = u % 2
        ra, bt, kt, vb = RA[pp], BT[pp], KT[pp], VB[pp]
        rkeys = [("RA", pp), ("BT", pp), ("KT", pp), ("VB", pp)]
        for cp in range(8):
            sysl = [(2 * ci + hd, ci, hd) for hd in range(2) for ci in range(2)]
            for q, ci, hd in sysl:
                n = 2 * cp + ci
                ph = slice(64 * hd, 64 * hd + 64)
                cn = slice(n * 128, (n + 1) * 128)
                S.op("pe", lambda e: e.matmul(P1[:, q, :], bt[ph, cn], ra[ph, n, 128:256], start=True, stop=True),
                     reads=rkeys, writes=["P1"], inc=(ci == 1), rg=hd)
            for q, ci, hd in sysl:
                n = 2 * cp + ci
                ph = slice(64 * hd, 64 * hd + 64)
                cn = slice(n * 128, (n + 1) * 128)
                S.op("pe", lambda e: e.matmul(P2[:, q, :], ra[ph, n, 128:256], bt[ph, cn], start=True, stop=True),
                     reads=rkeys, writes=["P2"], inc=(ci == 1), rg=hd)
            for kind, (lt, rsl) in enumerate(((kt, slice(128, 256)), (bt, slice(0, 128)), (kt, slice(0, 128)))):
                for q, ci, hd in sysl:
                    n = 2 * cp + ci
                    ph = slice(64 * hd, 64 * hd + 64)
                    cn = slice(n * 128, (n + 1) * 128)
                    S.op("pe", lambda e: e.matmul(PA[kind][:, q, :], lt[ph, cn], ra[ph, n, rsl], start=True, stop=True),
                         reads=rkeys, writes=[("PA", kind)], inc=(ci == 1), rg=hd)
            S.op("dve", lambda e: e.tensor_tensor(Xb[0][:], P1[:], k.ms4[:], ALU.mult), reads=["P1", "ms4"], writes=[("X", 0)])
            S.op("dve", lambda e: e.tensor_tensor(XTb[0][:], P2[:], k.ml4[:], ALU.mult), reads=["P2", "ml4"], writes=[("XT", 0)])
            ach = ACH[pp][:, 2 * cp:2 * cp + 2, :, :, :].rearrange("p c h k t -> p (c h) k t")
            for kind, mk, mkey in ((0, k.ms4, "ms4"), (1, k.mi4, "mi4"), (2, k.mi4, "mi4")):
                S.op("dve" if kind != 1 else "dve", lambda e: e.tensor_tensor(ach[:, :, kind, :], PA[kind][:], mk[:], ALU.mult),
                     reads=[("PA", kind), mkey], writes=[("ACH", pp, cp)])
            S.op("dve", lambda e: e.tensor_tensor(Pf[:], Xb[0][:], k.I4[:], ALU.add), reads=[("X", 0), "I4"], writes=["Pf"])
            S.op("act", lambda e: e.copy(Pbf[:], Pf[:]), reads=["Pf"], writes=["Pbf"])
            if scm < 1:
                yield
                continue
            pT = PM[0][:].bitcast(BF16)
            for ci in range(2):
                n = 2 * cp + ci
                cn = slice(n * 128, (n + 1) * 128)
                for j, src in enumerate((bt, kt, vb)):
                    S.op("pe", lambda e: e.transpose(pT[:, (ci * 3 + j) * 128:(ci * 3 + j + 1) * 128], src[:, cn], k.ident_bf[:]),
                         reads=rkeys + ["ident_bf"], writes=[("PM", 0)], inc=(ci == 1 and j == 2))
            S.op("act", lambda e: e.copy(TOK[pp][:, 2 * cp:2 * cp + 2, :, :].rearrange("p c j t -> p (c j t)"), pT[:, 0:768]),
                 reads=[("PM", 0)], writes=[("TOK", pp, cp)])
            pT3 = pT[:, 0:768].rearrange("p (c j t) -> p c j t", c=2, j=3)
            for hd in range(2):
                S.op("act", lambda e: e.copy(VPAD[pp][:, 2 * cp:2 * cp + 2, hd, 64 * hd:64 * hd + 64], pT3[:, :, 2, 64 * hd:64 * hd + 64]),
                     reads=[("PM", 0)], writes=[("VPAD", pp)])
            yield
            for lv in range(0, 7 if scm >= 2 else 0):
                xi, xo = lv % 2, (lv + 1) % 2
                if lv >= 1:
                    for q in range(4):
                        S.op("pe", lambda e: e.matmul(PA[2][:, q, :], XTb[xi][:, q, :], Pbf[:, q, :], start=True, stop=True),
                             reads=[("XT", xi), "Pbf"], writes=[("PA", 2)], inc=(q == 3))
                if lv < 6:
                    for q in range(4):
                        S.op("pe", lambda e: e.matmul(PA[0][:, q, :], XTb[xi][:, q, :], Xb[xi][:, q, :], start=True, stop=True),
                             reads=[("X", xi), ("XT", xi)], writes=[("PA", 0)], inc=(q == 3))
                    for q in range(4):
                        S.op("pe", lambda e: e.matmul(PA[1][:, q, :], Xb[xi][:, q, :], XTb[xi][:, q, :], start=True, stop=True),
                             reads=[("X", xi), ("XT", xi)], writes=[("PA", 1)], inc=(q == 3))
                if lv >= 1:
                    S.op("dve", lambda e: e.tensor_tensor(Pf[:], Pf[:], PA[2][:], ALU.add), reads=["Pf", ("PA", 2)], writes=["Pf"])
                    if lv < 6:
                        S.op("act", lambda e: e.copy(Pbf[:], Pf[:]), reads=["Pf"], writes=["Pbf"])
                    else:
                        S.op("act", lambda e: e.copy(TT[pp][:, 2 * cp:2 * cp + 2, :, :].rearrange("p c h t -> p (c h) t"), Pf[:]),
                             reads=["Pf"], writes=[("TT", pp, cp)])
                if lv < 6:
                    S.op("act", lambda e: e.copy(Xb[xo][:], PA[0][:]), reads=[("PA", 0)], writes=[("X", xo)])
                    S.op("dve", lambda e: e.tensor_copy(XTb[xo][:], PA[1][:]), reads=[("PA", 1)], writes=[("XT", xo)])
                yield

    def chain(u):
        pp = u % 2
        ra = RA[pp]
        if scm < 3:
            return
        S.op("dve", lambda e: e.memset(Sf[:], 0.0), writes=["Sf"])
        S.op("dve", lambda e: e.memset(Sbf[0][:], 0.0), writes=[("Sbf", 0)])
        for n in range(16):
            cp, ci = n // 2, n % 2
            si, so = n % 2, (n + 1) % 2
            cn = slice(n * 128, (n + 1) * 128)
            akey, tkey, ttkey = ("ACH", pp, cp), ("TOK", pp, cp), ("TT", pp, cp)
            S.op("pe", lambda e: e.matmul(PC[:, 0, :], ra[:, n, 128:256], Sbf[si][:], start=True, stop=False),
                 reads=[("RA", pp), ("Sbf", si)], writes=["PC"], inc=False)
            for hd in range(2):
                hs = slice(64 * hd, 64 * hd + 64)
                S.op("pe", lambda e: e.matmul(PC[:, 0, hs], ACH[pp][:, n, hd, 0, :], TOK[pp][:, n, 2, hs], start=False, stop=(hd == 1)),
                     reads=[akey, tkey], writes=["PC"], inc=(hd == 1))
            S.op("act", lambda e: e.copy(Wsb[:], PC[:, 0, :]), reads=["PC"], writes=["Wsb"])
            yield
            import os
            sub = int(os.environ.get("SCSUB", "9"))
            if sub <= 1:
                continue
            for hd in range(2):
                hs = slice(64 * hd, 64 * hd + 64)
                S.op("pe", lambda e: e.matmul(PC[:, 1, hs], TT[pp][:, n, hd, :], Wsb[:, hs], start=True, stop=True),
                     reads=[ttkey, "Wsb"], writes=["PC"], inc=(hd == 1))
            S.op("dve", lambda e: e.tensor_copy(Ut[:], PC[:, 1, :]), reads=["PC"], writes=["Ut"])
            for hd in range(2 if os.environ.get("NOUPAD") is None else 0):
                hs = slice(64 * hd, 64 * hd + 64)
                S.op("dve", lambda e: e.tensor_copy(UPAD[ci][:, hd, hs], PC[:, 1, hs]), reads=["PC"], writes=[("UPAD", ci)])
            yield
            if sub <= 2:
                continue
            PY = PM[1][:, 0:128]
            S.op("pe", lambda e: e.matmul(PY, Sbf[si][:], ra[:, n, 0:128], start=True, stop=False),
                 reads=[("RA", pp), ("Sbf", si)], writes=[("PM", 1)], inc=False)
            for hd in range(2):
                S.op("pe", lambda e: e.matmul(PY, UPAD[ci][:, hd, :], ACH[pp][:, n, hd, 1, :], start=False, stop=False),
                     reads=[("UPAD", ci), akey], writes=[("PM", 1)], inc=False)
                S.op("pe", lambda e: e.matmul(PY, VPAD[pp][:, n, hd, :], ACH[pp][:, n, hd, 2, :], start=False, stop=(hd == 1)),
                     reads=[("VPAD", pp), akey], writes=[("PM", 1)], inc=(hd == 1))
            if sub <= 3:
                S.op("act", lambda e: e.copy(Yf[:, cn], PY), reads=[("PM", 1)], writes=["Yf"])
                continue
            S.op("pe", lambda e: e.matmul(PC[:, 3, :], TOK[pp][:, n, 0, :], Ut[:], start=True, stop=False),
                 reads=[tkey, "Ut"], writes=["PC"], inc=False)
            S.op("pe", lambda e: e.matmul(PC[:, 3, :], TOK[pp][:, n, 1, :], TOK[pp][:, n, 2, :], start=False, stop=True),
                 reads=[tkey], writes=["PC"])
            S.op("act", lambda e: e.copy(Yf[:, cn], PY), reads=[("PM", 1)], writes=["Yf"])
            S.op("dve", lambda e: e.scalar_tensor_tensor(t1[:], PC[:, 3, :], GL[pp][:, n:n + 1], k.blk_f[:], ALU.mult, ALU.mult),
                 reads=["PC", ("GL", pp), "blk_f"], writes=["t1"])
            S.op("dve", lambda e: e.scalar_tensor_tensor(Sbf[so][:], Sf[:], GL[pp][:, n:n + 1], t1[:], ALU.mult, ALU.add),
                 reads=["Sf", "t1", ("GL", pp)], writes=[("Sbf", so)])
            S.op("dve", lambda e: e.scalar_tensor_tensor(Sf[:], Sf[:], GL[pp][:, n:n + 1], t1[:], ALU.mult, ALU.add),
                 reads=["Sf", "t1", ("GL", pp)], writes=["Sf"])
            yield
        if sub <= 4:
            return
        S.dma("sp", Bo[:], D["bo_d"][u], "ld_bo", writes=["Bo"])
        S.dma("sp", Bg[:], D["bg_d"][u], "ld_bg", writes=["Bg"])
        S.op("act", lambda e: e.copy(yb[:], Yf[:]), reads=["Yf"], writes=["yb"])
        for g in range(4):
            gsl = slice(g * 512, (g + 1) * 512)
            S.op("pe", lambda e: e.matmul(PM[1][:], k.blk_bf[:], yb[:, gsl], start=True, stop=True),
                 reads=["blk_bf", "yb"], writes=[("PM", 1)])
            S.op("dve", lambda e: e.scalar_tensor_tensor(yc[:, gsl], PM[1][:], -1.0 / 64, Yf[:, gsl], ALU.mult, ALU.add),
                 reads=[("PM", 1), "Yf"], writes=["yc"])
        S.op("pool", lambda e: e.tensor_tensor(yb[:], yc[:], yc[:], ALU.mult), reads=["yc"], writes=["yb"])
        for g in range(4):
            gsl = slice(g * 512, (g + 1) * 512)
            S.op("pe", lambda e: e.matmul(PM[1][:], k.blk_bf[:], yb[:, gsl], start=True, stop=True),
                 reads=["blk_bf", "yb"], writes=[("PM", 1)])
            S.op("act", lambda e: e.activation(rs[:, gsl], PM[1][:], AF.Sqrt, bias=k.eps_gn[:], scale=1.0 / 64),
                 reads=[("PM", 1), "eps_gn"], writes=["Yf"])
        S.op("dve", lambda e: e.reciprocal(rs[:], rs[:]), reads=["Yf"], writes=["Yf"])
        S.op("dve", lambda e: e.tensor_tensor(yc[:], yc[:], rs[:], ALU.mult), reads=["yc", "Yf"], writes=["yc"])
        S.op("dve", lambda e: e.tensor_scalar(yc[:], yc[:], chv[:, CGG + u:CGG + u + 1], chv[:, CGB + u:CGB + u + 1], ALU.mult, ALU.add),
             reads=["yc", "chv2"], writes=["yc"])
        S.op("pool", lambda e: e.tensor_tensor(yc[:], yc[:], Bo[:], ALU.add), reads=["yc", "Bo"], writes=["yc"])
        S.op("dve", lambda e: e.tensor_tensor(hb[:], yc[:], Bg[:], ALU.mult), reads=["yc", "Bg"], writes=["hb"])
        S.dma("sp", D["hT_d"][u], hb[:], "st_hb", reads=["hb"])
        yield

    def drive(gens):
        gens = [g for g in gens if g is not None]
        while gens:
            for g in list(gens):
                try:
                    next(g)
                except StopIteration:
                    gens.remove(g)

    load(0)
    if scm < 0:
        return
    drive([stage1(0)])
    for u in range(npairs):
        if u + 1 < npairs:
            load(u + 1)
        drive([chain(u), stage1(u + 1) if u + 1 < npairs else None])


def layer_norm_rows(k, z, zkey, gbc, bbc, gbkey, out, okey, tmp):
    S, nc = k.S, k.nc
    st6, mv, rstd, nmr = tmp
    for i in range(4):
        S.op("dve", lambda e: e.bn_stats(st6[:, i, :], z[:, i * 512:(i + 1) * 512]), reads=[zkey], writes=["ln_st6"])
    S.op("dve", lambda e: e.bn_aggr(mv[:], st6[:].rearrange("p a b -> p (a b)")), reads=["ln_st6"], writes=["ln_mv"])
    S.op("act", lambda e: e.activation(rstd[:], mv[:, 1:2], AF.Sqrt, bias=k.eps_ln[:], scale=1.0), reads=["ln_mv", "eps_ln"], writes=["ln_rstd"])
    S.op("dve", lambda e: e.reciprocal(rstd[:], rstd[:]), reads=["ln_rstd"], writes=["ln_rstd"])
    S.op("dve", lambda e: e.scalar_tensor_tensor(nmr[:], mv[:, 0:1], -1.0, rstd[:], ALU.mult, ALU.mult),
         reads=["ln_mv", "ln_rstd"], writes=["ln_nmr"])
    S.op("act", lambda e: e.activation(z[:], z[:], AF.Identity, bias=nmr[:], scale=rstd[:]),
         reads=[zkey, "ln_nmr", "ln_rstd"], writes=[zkey])
    S.op("dve", lambda e: e.tensor_tensor(z[:], z[:], gbc[:], ALU.mult), reads=[zkey, gbkey], writes=[zkey])
    S.op("pool", lambda e: e.tensor_tensor(out[:], z[:], bbc[:], ALU.add), reads=[zkey, gbkey], writes=[okey])


def phase_b(k):
    nc, S, D = k.nc, k.S, k.D
    sb = lambda n, s, d=F32: k.sb(n, s, d, k.pst)
    ps = lambda n, s, d=F32: k.ps(n, s, d, k.pst)
    nchunks = k.cfg.get("b_chunks", 16)
    hTs = sb("b_hT", [128, 16, SEQ], BF16)
    wout = sb("b_wout", [128, 16, DM], BF16)
    for i in range(4):
        S.dma("sp", hTs[:, 4 * i:4 * i + 4, :], D["hT_d"][4 * i:4 * i + 4].rearrange("c p t -> p c t"), "b_hT%d" % i, writes=[("hT", i)])
    wov = D["w_out"].rearrange("(c p) n -> p c n", p=128)
    for i in range(4):
        S.dma("pool", wout[:, 4 * i:4 * i + 4, :], wov[:, 4 * i:4 * i + 4, :], "b_wout%d" % i, writes=[("wout", i)])
    gbc = sb("b_gbc", [128, DM]); bbc = sb("b_bbc", [128, DM])
    S.dma("sp", gbc[:], D["ln1"][0].partition_broadcast(128), "b_gbc", writes=["gb1"])
    S.dma("sp", bbc[:], D["ln1"][1].partition_broadcast(128), "b_bbc", writes=["gb1"])
    wr = sb("b_wr", [128, 16, NE]); brt = sb("b_brt", [128, NE])
    S.dma("sp", wr[:], D["w_router"].rearrange("(c p) e -> p c e", p=128), "b_wr", writes=["wr"])
    S.dma("sp", brt[:], D["b_router"][0].partition_broadcast(128), "b_brt", writes=["brt"])
    k.eps_ln = sb("eps_ln", [128, 1])
    S.op("dve", lambda e: e.memset(k.eps_ln[:], 1e-5), writes=["eps_ln"])
    ecap = sb("b_ecap", [128, NE])
    S.op("dve", lambda e: e.tensor_scalar_mul(ecap[:], k.iota_e[:], float(CAP)), reads=["iota_e"], writes=["ecap"])
    carry = sb("b_carry", [128, NE])
    S.op("dve", lambda e: e.memset(carry[:], 0.0), writes=["carry"])
    xc = [sb("b_xc%d" % i, [128, DM]) for i in range(2)]
    z = sb("b_z", [128, DM]); x1 = sb("b_x1", [128, DM]); x1T = sb("b_x1T", [128, 16, 128]); x1b = sb("b_x1b", [128, DM], BF16)
    lntmp = (sb("ln_st6", [128, 4, 6]), sb("ln_mv", [128, 2]), sb("ln_rstd", [128, 1]), sb("ln_nmr", [128, 1]))
    lg = sb("b_lg", [128, NE]); t8 = sb("b_t8", [128, 8]); oh = sb("b_oh", [128, 4, NE]); ma = sb("b_ma", [128, NE])
    mab = sb("b_mab", [128, NE], BF16); negm = sb("b_negm", [128, 1]); ev = sb("b_ev", [128, 4]); esum = sb("b_esum", [128, 1])
    pe_ = sb("b_pe", [128, NE]); ohp = sb("b_ohp", [128, 4, NE]); destf = sb("b_destf", [128, 4]); posk = sb("b_posk", [128, 4])
    ovf = sb("b_ovf", [128, 4])
    psM = [ps("b_psM%d" % i, [128, 512]) for i in range(4)]
    psT = [ps("b_psT%d" % i, [128, 4, 128]) for i in range(2)]
    psL = ps("b_psL", [128, 512])
    def mix_mm(c):
        tc_ = slice(c * 128, (c + 1) * 128)
        S.dma("sp", xc[c % 2][:], D["x"][tc_, :], "b_xc%d" % (c % 2), writes=[("xc", c % 2)])
        for nb in range(4):
            for fc in range(16):
                S.op("pe", lambda e: e.matmul(psM[nb][:], hTs[:, fc, tc_], wout[:, fc, nb * 512:(nb + 1) * 512],
                                              start=(fc == 0), stop=(fc == 15)),
                     reads=[("hT", fc // 4), ("wout", fc // 4)], writes=[("psM", nb)], inc=(fc == 15))

    mix_mm(0)
    for c in range(nchunks):
        tc_ = slice(c * 128, (c + 1) * 128)
        xcb = xc[c % 2]
        for nb in range(4):
            S.op("dve", lambda e: e.scalar_tensor_tensor(z[:, nb * 512:(nb + 1) * 512], xcb[:, nb * 512:(nb + 1) * 512], ALPHA,
                                                         psM[nb][:], ALU.mult, ALU.add),
                 reads=[("psM", nb), ("xc", c % 2)], writes=["z"])
        if c + 1 < nchunks:
            mix_mm(c + 1)
        layer_norm_rows(k, z, "z", gbc, bbc, "gb1", x1, "x1", lntmp)
        S.dma("sp", D["x1f_d"][tc_, :], x1[:], "b_x1st", reads=["x1"])
        S.op("act", lambda e: e.copy(x1b[:], x1[:]), reads=["x1"], writes=["x1b"])
        for r in range(4):
            pt = psT[r % 2]
            for i in range(4):
                dc = 4 * r + i
                S.op("pe", lambda e: e.transpose(pt[:, i, :], x1[:, dc * 128:(dc + 1) * 128], k.ident_f[:]),
                     reads=["x1", "ident_f"], writes=[("psT", r % 2)], inc=(i == 3))
            S.op("act", lambda e: e.copy(x1T[:, 4 * r:4 * r + 4, :], pt[:]), reads=[("psT", r % 2)], writes=["x1T"])
        for dc in range(16):
            S.op("pe", lambda e: e.matmul(psL[:, 0:NE], x1T[:, dc, :], wr[:, dc, :], start=(dc == 0), stop=(dc == 15)),
                 reads=["x1T", "wr"], writes=["psL"], inc=(dc == 15))
        S.op("dve", lambda e: e.tensor_tensor(lg[:], psL[:, 0:NE], brt[:], ALU.add), reads=["psL", "brt"], writes=["lg"])
        S.op("dve", lambda e: e.max(t8[:], lg[:]), reads=["lg"], writes=["t8"])
        for kk in range(4):
            S.op("dve", lambda e: e.tensor_scalar(oh[:, kk, :], lg[:], t8[:, kk:kk + 1], None, ALU.is_equal),
                 reads=["lg", "t8"], writes=["oh"])
        S.op("dve", lambda e: e.reduce_sum(ma[:], oh[:].rearrange("p k e -> p e k"), AX.X), reads=["oh"], writes=["ma"])
        S.op("dve", lambda e: e.tensor_copy(mab[:], ma[:]), reads=["ma"], writes=["mab"])
        S.op("dve", lambda e: e.tensor_scalar_mul(negm[:], t8[:, 0:1], -1.0), reads=["t8"], writes=["negm"])
        S.op("act", lambda e: e.activation(ev[:], t8[:, 0:4], AF.Exp, bias=negm[:], scale=1.0), reads=["t8", "negm"], writes=["ev"])
        S.op("dve", lambda e: e.reduce_sum(esum[:], ev[:], AX.X), reads=["ev"], writes=["esum"])
        S.op("dve", lambda e: e.reciprocal(esum[:], esum[:]), reads=["esum"], writes=["esum"])
        S.op("dve", lambda e: e.tensor_scalar_mul(k.gates_all[:, c, :], ev[:], esum[:, 0:1]), reads=["ev", "esum"], writes=["gates_all"])
        S.op("pe", lambda e: e.matmul(psL[:, 32:64], k.m_strict_bf[:], mab[:], start=True, stop=True),
             reads=["m_strict_bf", "mab"], writes=["psL"], inc=False)
        S.op("pe", lambda e: e.matmul(psL[:, 64:96], k.ones_bf[:], mab[:], start=True, stop=True),
             reads=["ones_bf", "mab"], writes=["psL"])
        S.op("dve", lambda e: e.tensor_tensor(pe_[:], psL[:, 32:64], carry[:], ALU.add), reads=["psL", "carry"], writes=["pe"])
        S.op("dve", lambda e: e.tensor_tensor(carry[:], psL[:, 64:96], carry[:], ALU.add), reads=["psL", "carry"], writes=["carry"])
        for kk in range(4):
            S.op("dve", lambda e: e.tensor_tensor(ohp[:, kk, :], oh[:, kk, :], pe_[:], ALU.mult), reads=["oh", "pe"], writes=["ohp"])
        S.op("dve", lambda e: e.reduce_sum(posk[:], ohp[:], AX.X), reads=["ohp"], writes=["posk"])
        for kk in range(4):
            S.op("dve", lambda e: e.tensor_tensor(ohp[:, kk, :], oh[:, kk, :], ecap[:], ALU.mult), reads=["oh", "ecap"], writes=["ohp"])
        S.op("dve", lambda e: e.reduce_sum(destf[:], ohp[:], AX.X), reads=["ohp"], writes=["destf"])
        S.op("dve", lambda e: e.tensor_scalar_min(posk[:], posk[:], float(CAP - 1)), reads=["posk"], writes=["posk"])
        S.op("dve", lambda e: e.tensor_tensor(destf[:], destf[:], posk[:], ALU.add), reads=["destf", "posk"], writes=["destf"])
        S.op("dve", lambda e: e.tensor_copy(k.dest_all[:, c, :], destf[:]), reads=["destf"], writes=["dest_all"])
        for kk in range(4):
            S.dma("pool", D["xdisp_d"][:, :], x1b[:], "b_disp", reads=["x1b", "dest_all", "xdisp"],
                  indirect=(bass.IndirectOffsetOnAxis(ap=k.dest_all[:, c, kk:kk + 1], axis=0), None))


def phase_c(k):
    nc, S, D = k.nc, k.S, k.D
    sb = lambda n, s, d=F32: k.sb(n, s, d, k.pst)
    ps = lambda n, s, d=F32: k.ps(n, s, d, k.pst)
    nexp = k.cfg.get("c_experts", NE)
    NJ = CAP // 128
    xtok = sb("c_xtok", [128, NJ, DM], BF16)
    xeT = [sb("c_xeT%d" % i, [128, 16, CAP], BF16) for i in range(2)]
    wg = [sb("c_wg%d" % i, [128, 16, 512], BF16) for i in range(3)]
    NWG = 3
    wd = [sb("c_wd%d" % i, [128, 16, 512], BF16) for i in range(2)]
    gg = sb("c_gg", [128, 4, CAP]); gsb = sb("c_gsb", [128, CAP]); sg = sb("c_sg", [128, CAP]); usb = sb("c_usb", [128, CAP])
    actT = sb("c_actT", [128, 16, CAP], BF16)
    ysb = [sb("c_ysb%d" % i, [128, 512]) for i in range(4)]
    nys = [0]
    bdn = sb("c_bdn", [128, DM])
    bgu = sb("c_bgu", [128, NE, 32])
    S.dma("sp", bgu[:].rearrange("p e f -> p (e f)"), D["b_gu_r"], "c_bgu", writes=["bgu"])
    psT = [ps("c_psT%d" % i, [128, 512]) for i in range(2)]
    psG = [ps("c_psG%d" % i, [128, 512]) for i in range(3)]
    psD = [ps("c_psD%d" % i, [128, 512]) for i in range(3)]
    nwg = [0]; nwd = [0]; npg = [0]; npd = [0]

    def load_expert_inputs(e):
        S.dma("sp", xtok[:], D["xdisp_d"][e * CAP:(e + 1) * CAP, :].rearrange("(j p) d -> p j d", p=128), "c_xtok",
              reads=["xdisp"], writes=["xtok"])

    def transposes(e):
        xe = xeT[e % 2]
        xkey = ("xeT", e % 2)
        tn = 0
        for j in range(NJ):
            for r in range(2):
                pt = psT[tn % 2]
                ptb = pt[:].bitcast(BF16)
                for i in range(8):
                    dc = 8 * r + i
                    S.op("pe", lambda e_: e_.transpose(ptb[:, i * 128:(i + 1) * 128], xtok[:, j, dc * 128:(dc + 1) * 128], k.ident_bf[:]),
                         reads=["xtok", "ident_bf"], writes=[("c_psT", tn % 2)], inc=(i == 7))
                S.op("act", lambda e_: e_.copy(xe[:, 8 * r:8 * r + 8, j * 128:(j + 1) * 128],
                                               ptb[:].rearrange("p (i s) -> p i s", s=128)),
                     reads=[("c_psT", tn % 2)], writes=[xkey])
                tn += 1

    load_expert_inputs(0)
    transposes(0)
    for e in range(nexp):
        S.dma("sp", bdn[:], D["b_dn"][e].partition_broadcast(128), "c_bdn", writes=["bdn"])
        if e + 1 < nexp:
            load_expert_inputs(e + 1)
        xe = xeT[e % 2]
        xkey = ("xeT", e % 2)
        for t in range(4):
            for half in range(2):
                w = wg[nwg[0] % 3]; wkey = ("wg", nwg[0] % 3); nwg[0] += 1
                c0 = half * DM + t * 512
                S.dma("pool", w[:], D["w_gu"][e, :, c0:c0 + 512].rearrange("(c p) n -> p c n", p=128), "c_wg%d" % ((nwg[0] - 1) % 3),
                      writes=[wkey])
                for fbi in range(4):
                    fb = t * 4 + fbi
                    pg = psG[npg[0] % 3]; pgkey = ("psG", npg[0] % 3); npg[0] += 1
                    for dc in range(16):
                        S.op("pe", lambda e_: e_.matmul(pg[:, 0:CAP], w[:, dc, fbi * 128:(fbi + 1) * 128], xe[:, dc, :],
                                                        start=(dc == 0), stop=(dc == 15)),
                             reads=[wkey, xkey], writes=[pgkey], inc=(dc == 15))
                    bcol = bgu[:, e, half * 16 + fb:half * 16 + fb + 1]
                    if half == 0:
                        S.op("dve", lambda e_: e_.tensor_scalar(gsb[:], pg[:, 0:CAP], bcol, 7.0, ALU.add, ALU.min),
                             reads=[pgkey, "bgu"], writes=["gsb"])
                        S.op("act", lambda e_: e_.activation(sg[:], gsb[:], AF.Sigmoid, scale=1.702), reads=["gsb"], writes=["sg"])
                        S.op("dve", lambda e_: e_.tensor_tensor(gg[:, fbi, :], gsb[:], sg[:], ALU.mult), reads=["gsb", "sg"], writes=[("gg", fbi)])
                    else:
                        S.op("dve", lambda e_: e_.tensor_scalar(usb[:], pg[:, 0:CAP], bcol, 7.0, ALU.add, ALU.min),
                             reads=[pgkey, "bgu"], writes=["usb"])
                        S.op("dve", lambda e_: e_.tensor_scalar(usb[:], usb[:], -7.0, 1.0, ALU.max, ALU.add), reads=["usb"], writes=["usb"])
                        S.op("dve", lambda e_: e_.tensor_tensor(actT[:, fb, :], usb[:], gg[:, fbi, :], ALU.mult),
                             reads=["usb", ("gg", fbi)], writes=["actT"])
        if e + 1 < nexp:
            transposes(e + 1)
        for db in range(4):
            w = wd[nwd[0] % 2]; wkey = ("wd", nwd[0] % 2); nwd[0] += 1
            S.dma("pool", w[:], D["w_dn"][e, :, db * 512:(db + 1) * 512].rearrange("(c p) n -> p c n", p=128), "c_wd%d" % ((nwd[0] - 1) % 2),
                  writes=[wkey])
            for j in range(NJ):
                pd = psD[npd[0] % 3]; pdkey = ("psD", npd[0] % 3); npd[0] += 1
                for fc in range(16):
                    S.op("pe", lambda e_: e_.matmul(pd[:], actT[:, fc, j * 128:(j + 1) * 128], w[:, fc, :], start=(fc == 0), stop=(fc == 15)),
                         reads=["actT", wkey], writes=[pdkey], inc=(fc == 15))
                yi = nys[0] % 4; nys[0] += 1
                S.op("dve", lambda e_: e_.tensor_tensor(ysb[yi][:], pd[:], bdn[:, db * 512:(db + 1) * 512], ALU.add),
                     reads=[pdkey, "bdn"], writes=[("ysb", yi)])
                S.dma("sp", D["y_d"][e * CAP + j * 128:e * CAP + (j + 1) * 128, db * 512:(db + 1) * 512], ysb[yi][:], "c_yst%d" % yi,
                      reads=[("ysb", yi)])


def phase_d(k):
    nc, S, D = k.nc, k.S, k.D
    sb = lambda n, s, d=F32: k.sb(n, s, d, k.pst)
    nchunks = k.cfg.get("b_chunks", 16)
    gbc = sb("d_gbc", [128, DM]); bbc = sb("d_bbc", [128, DM])
    S.dma("sp", gbc[:], D["ln2"][0].partition_broadcast(128), "d_gbc", writes=["gb2"])
    S.dma("sp", bbc[:], D["ln2"][1].partition_broadcast(128), "d_bbc", writes=["gb2"])
    if not hasattr(k, "eps_ln"):
        k.eps_ln = sb("eps_ln", [128, 1])
    else:
        k.eps_ln = sb("eps_ln2", [128, 1])
    S.op("dve", lambda e: e.memset(k.eps_ln[:], 1e-5), writes=["eps_ln"])
    lntmp = (sb("ln2_st6", [128, 4, 6]), sb("ln2_mv", [128, 2]), sb("ln2_rstd", [128, 1]), sb("ln2_nmr", [128, 1]))
    x1 = [sb("d_x1_%d" % i, [128, DM]) for i in range(2)]
    yk = [[sb("d_y%d_%d" % (i, kk), [128, DM]) for kk in range(4)] for i in range(2)]
    acc = sb("d_acc", [128, DM]); ob = [sb("d_ob%d" % i, [128, DM]) for i in range(2)]
    for c in range(nchunks):
        tc_ = slice(c * 128, (c + 1) * 128)
        b = c % 2
        S.dma("sp", x1[b][:], D["x1f_d"][tc_, :], "d_x1_%d" % b, writes=[("dx1", b)])
        for kk in range(4):
            S.dma("pool", yk[b][kk][:], D["y_d"][:, :], "d_y%d_%d" % (b, kk), reads=["dest_all"], writes=[("dy", b, kk)],
                  indirect=(None, bass.IndirectOffsetOnAxis(ap=k.dest_all[:, c, kk:kk + 1], axis=0)))
        S.op("dve", lambda e: e.tensor_scalar_mul(acc[:], x1[b][:], ALPHA), reads=[("dx1", b)], writes=["acc"])
        for kk in range(4):
            S.op("dve", lambda e: e.scalar_tensor_tensor(acc[:], yk[b][kk][:], k.gates_all[:, c, kk:kk + 1], acc[:], ALU.mult, ALU.add),
                 reads=[("dy", b, kk), "gates_all", "acc"], writes=["acc"])
        layer_norm_rows(k, acc, "acc", gbc, bbc, "gb2", ob[b], ("ob", b), lntmp)
        S.dma("sp", D["out"][tc_, :], ob[b][:], "d_ob%d" % b, reads=[("ob", b)])


def prep_shared(inp):
    f = lambda a: np.ascontiguousarray(a, dtype=np.float32)
    w_in = inp["w_in"][0]

    def fm_tile(c0, n=128):
        t = np.zeros((2048, 128), np.float32)
        t[:, :n] = w_in[:, c0:c0 + n]
        return t.reshape(16, 128, 128).transpose(1, 0, 2).reshape(128, 2048)
    cols = [(128 * u, 128) for u in range(24)] + [(3072, 128), (3200, 128), (3328, 32)]
    cols += [(3360 + 128 * h, 128) for h in range(8)] + [(4384 + 128 * h, 128) for h in range(8)]
    w_fm = np.stack([fm_tile(c, n) for c, n in cols])
    w_v = np.stack([w_in[:, 5408 + 512 * g:5408 + 512 * (g + 1)].reshape(16, 128, 512).transpose(1, 0, 2).reshape(128, 8192)
                    for g in range(2)])
    mu = np.zeros(27 * 128, np.float32)
    mu[:3360] = inp["shift_mu"][0]
    chv = np.concatenate([inp[n][0].reshape(8, 128).T for n in ("w0", "a0", "k_k", "k_a", "gn_g", "gn_b", "r_k")], axis=1)
    sh = {
        "w_fm": f(w_fm), "w_v": f(w_v), "mu": f(mu.reshape(27, 128).T), "chv": f(chv),
        "w_up": f(inp["w_up"][0]), "a_up": f(inp["a_up"][0]), "g_up": f(inp["g_up"][0]),
        "lqk": f(np.concatenate([inp["lq1"][0], inp["lk1"][0], inp["lq2"][0], inp["lk2"][0]])[None, :]),
        "subln": f(inp["subln_g"][0][:, None]),
        "w_out": f(inp["w_out"][0]), "ln1": f(np.stack([inp["ln1_g"][0], inp["ln1_b"][0]])),
        "ln2": f(np.stack([inp["ln2_g"][0], inp["ln2_b"][0]])),
        "w_router": f(inp["w_router"][0]), "b_router": f(inp["b_router"][0][None, :]),
        "w_gu": f(inp["w_gu"][0]), "b_gu_r": f(inp["b_gu"][0].reshape(32, 32, 128).transpose(2, 0, 1).reshape(128, 1024)),
        "w_dn": f(inp["w_dn"][0]), "b_dn": f(inp["b_dn"][0]),
        "zeros": np.zeros((CAP, DM), dtype=ml_dtypes.bfloat16),
    }
    return sh


def prep_core(inp, b):
    xb = np.asarray(inp["x"][b], dtype=np.float32)
    return {"xT": np.ascontiguousarray(xb.T), "x": np.ascontiguousarray(xb)}


def kernel(**inputs):
    nc, _ = build()
    sh = prep_shared(inputs)
    in_maps = [dict(sh, **prep_core(inputs, b)) for b in range(8)]
    res = run_bass_kernel_spmd(nc, in_maps, core_ids=list(range(8)))
    return np.stack([np.asarray(r["out"], dtype=np.float32) for r in res.results])
```

```python
import math
from contextlib import ExitStack
import numpy as np
import ml_dtypes
import concourse.bass as bass
import concourse.mybir as mybir
from concourse.bass_utils import run_bass_kernel_spmd

F32 = mybir.dt.float32
BF16 = mybir.dt.bfloat16
I32 = mybir.dt.int32
U32 = mybir.dt.uint32
AF = mybir.ActivationFunctionType
ALU = mybir.AluOpType
AX = mybir.AxisListType

SEQ = 2048
DM = 2048
NE = 32
CAP = 384
ALPHA = 2.0 ** 0.25
LAM_INIT = 0.8 - 0.6
SCALE = 64 ** -0.5
EXPM05 = math.exp(-0.5)
N_FM = 43


class Sched:
    def __init__(self, nc, stack):
        self.nc = nc
        self.stack = stack
        self.engs = {"pe": nc.tensor, "act": nc.scalar, "dve": nc.vector,
                     "pool": nc.gpsimd, "sp": nc.sync}
        self.sems = {}
        self.cnt = {}
        self.waited = {e: {} for e in self.engs}
        self.last_write = {}
        self.readers = {}
        for e in ("pe", "act", "dve", "pool"):
            self._sem("E_" + e)
        self.n_ins = 0
        self.last_rg = None
        self.last_pe_inc = True

    def _sem(self, name):
        if name not in self.sems:
            self.sems[name] = self.stack.enter_context(self.nc.semaphore(name))
            self.cnt[name] = 0
        return self.sems[name]

    def _emit_waits(self, eng, reads, writes):
        need = {}

        def add(tok, same_ok):
            if tok is None:
                return
            s, v = tok
            if same_ok and s == "E_" + eng and eng == "pe":
                return
            if need.get(s, 0) < v:
                need[s] = v
        for k in reads:
            add(self.last_write.get(k), False)
        for k in writes:
            add(self.last_write.get(k), True)
            for t in self.readers.get(k, ()):
                add(t, True)
        e = self.engs[eng]
        for s, v in need.items():
            if self.waited[eng].get(s, 0) >= v:
                continue
            e.wait_ge(self.sems[s], v)
            self.waited[eng][s] = v
            self.n_ins += 1

    def _record(self, tok, reads, writes):
        for k in reads:
            self.readers.setdefault(k, []).append(tok)
        for k in writes:
            self.last_write[k] = tok
            self.readers[k] = []

    def op(self, eng, fn, reads=(), writes=(), inc=True, rg=None):
        if eng == "pe":
            if rg is not None and self.last_rg is not None and rg != self.last_rg:
                assert self.last_pe_inc
                self.engs["pe"].wait_ge(self.sems["E_pe"], self.cnt["E_pe"])
                self.n_ins += 1
            self.last_rg = rg
            self.last_pe_inc = inc
        self._emit_waits(eng, reads, writes)
        ins = fn(self.engs[eng])
        self.n_ins += 1
        s = "E_" + eng
        if inc:
            self.cnt[s] += 1
            ins.then_inc(self.sems[s], 1)
            tok = (s, self.cnt[s])
        else:
            tok = (s, self.cnt[s] + 1)
        self._record(tok, reads, writes)
        return ins

    def dma(self, q, out, in_, semkey, reads=(), writes=(), indirect=None, **kw):
        self._emit_waits(q, reads, writes)
        s = "D_" + semkey
        self._sem(s)
        if indirect is None:
            ins = self.engs[q].dma_start(out=out, in_=in_, **kw)
        else:
            ins = self.engs[q].indirect_dma_start(out, indirect[0], in_, indirect[1], **kw)
        self.n_ins += 1
        self.cnt[s] += 16
        ins.then_inc(self.sems[s], 16)
        tok = (s, self.cnt[s])
        self._record(tok, reads, writes)
        return ins

    def barrier(self):
        for eng in self.engs:
            e = self.engs[eng]
            for s, v in self.cnt.items():
                if v == 0 or s == "E_" + eng:
                    continue
                if self.waited[eng].get(s, 0) >= v:
                    continue
                e.wait_ge(self.sems[s], v)
                self.waited[eng][s] = v
                self.n_ins += 1
        self.last_write = {}
        self.readers = {}


class K:
    pass


def build(cfg=None):
    cfg = cfg or {}
    phases = cfg.get("phases", "ABCD")
    dbg = cfg.get("dbg", ())
    nc = bass.Bass("TRN2", target_bir_lowering=False)
    k = K()
    k.nc, k.cfg, k.dbg = nc, cfg, dbg
    D = {}
    k.D = D

    def din(name, shape, dt=F32):
        D[name] = nc.dram_tensor(name, list(shape), dt, kind="ExternalInput").ap()

    def dout(name, shape, dt=F32):
        D[name] = nc.dram_tensor(name, list(shape), dt, kind="ExternalOutput").ap()

    def dscr(name, shape, dt):
        if name in cfg.get("ext_in", ()):
            D[name] = nc.dram_tensor(name, list(shape), dt, kind="ExternalInput").ap()
        elif name in cfg.get("ext", ()):
            D[name] = nc.dram_tensor(name, list(shape), dt, kind="ExternalOutput").ap()
        else:
            D[name] = nc.dram_tensor(name, list(shape), dt).ap()
    k.dout = dout

    din("xT", [DM, SEQ]); din("x", [SEQ, DM])
    din("w_fm", [N_FM, 128, 2048]); din("w_v", [2, 128, 16 * 512])
    din("mu", [128, 27]); din("chv", [128, 56])
    din("w_up", [64, 1024]); din("a_up", [64, 1024]); din("g_up", [160, 1024])
    din("lqk", [1, 256]); din("subln", [128, 1])
    din("w_out", [DM, DM]); din("ln1", [2, DM]); din("ln2", [2, DM])
    din("w_router", [DM, NE]); din("b_router", [1, NE])
    din("w_gu", [cfg.get("ne_decl", NE), 8, 128, 16 * 512]); din("b_gu_r", [128, NE * 32])
    din("w_dn", [cfg.get("ne_decl", NE), 4, 128, 16 * 512]); din("b_dn", [NE, DM])
    din("zeros", [CAP, DM], BF16)
    dout("out", [SEQ, DM])
    dscr("hT_d", [16, 128, SEQ], BF16)
    dscr("ra_d", [8, 128, 4096], BF16); dscr("bt_d", [8, 128, SEQ], BF16); dscr("kt_d", [8, 128, SEQ], BF16)
    dscr("vb_d", [8, 128, SEQ], BF16); dscr("bo_d", [8, 128, SEQ], F32); dscr("bg_d", [8, 128, SEQ], F32)
    dscr("gl_d", [8, 128, 16], F32)
    dscr("x1f_d", [SEQ, DM], F32)
    dscr("xdisp_d", [NE * CAP, DM], BF16)
    dscr("y_d", [NE * CAP, DM], F32)

    with ExitStack() as st:
        k.st = st
        k.S = Sched(nc, st)

        def sb(name, shape, dt=F32, stack=None):
            return (stack or st).enter_context(nc.sbuf_tensor(name, list(shape), dt))

        def ps(name, shape, dt=F32, stack=None):
            return (stack or st).enter_context(nc.psum_tensor(name, list(shape), dt))
        k.sb, k.ps = sb, ps
        setup_consts(k)
        k.dest_all = sb("dest_all", [128, 16, 4], I32)
        k.gates_all = sb("gates_all", [128, 16, 4], F32)
        k.S.op("dve", lambda e: e.memset(k.dest_all[:], 0), writes=["dest_all"])
        k.S.op("dve", lambda e: e.memset(k.gates_all[:], 0.0), writes=["gates_all"])
        if "A" in phases:
            with ExitStack() as pst:
                k.pst = pst
                if cfg.get("pro", True):
                    phase_a_setup(k)
                if cfg.get("rwkv", True) and cfg.get("pro", True):
                    with ExitStack() as st2:
                        phase_rwkv_pro(k, st2)
                    k.S.barrier()
                if cfg.get("diff", True):
                    with ExitStack() as st2:
                        phase_diff(k, st2)
            k.S.barrier()
            if cfg.get("rwkv", True) and cfg.get("scan", True):
                with ExitStack() as st2:
                    phase_rwkv_scan(k, st2)
                k.S.barrier()
        if "B" in phases:
            with ExitStack() as pst:
                k.pst = pst
                phase_b(k)
            k.S.barrier()
        if "route" in dbg:
            dout("dbg_dest", [128, 64], I32); dout("dbg_gates", [128, 64], F32)
            k.S.dma("sp", D["dbg_dest"], k.dest_all[:].rearrange("p c k -> p (c k)"), "dbg_dest", reads=["dest_all"])
            k.S.dma("sp", D["dbg_gates"], k.gates_all[:].rearrange("p c k -> p (c k)"), "dbg_gates", reads=["gates_all"])
            k.S.barrier()
        if "C" in phases:
            with ExitStack() as pst:
                k.pst = pst
                phase_c(k)
            k.S.barrier()
        if "D" in phases:
            with ExitStack() as pst:
                k.pst = pst
                phase_d(k)
        k.S.barrier()
    k.n_ins = k.S.n_ins
    return nc, k


def setup_consts(k):
    nc, S, sb = k.nc, k.S, k.sb
    k.iota_jp = sb("iota_jp", [128, 512], F32)
    S.op("pool", lambda e: e.iota(k.iota_jp[:], [[1, 512]], base=0, channel_multiplier=-1,
                                  allow_small_or_imprecise_dtypes=True), writes=["iota_jp"])
    k.ident_bf = sb("ident_bf", [128, 128], BF16)
    k.ident_f = sb("ident_f", [128, 128], F32)
    k.m_incl = sb("m_incl", [128, 128], F32)
    k.m_strict = sb("m_strict", [128, 128], F32)
    k.m_incl_bf = sb("m_incl_bf", [128, 128], BF16)
    k.m_strict_bf = sb("m_strict_bf", [128, 128], BF16)
    k.ones_bf = sb("ones_bf", [128, 128], BF16)
    k.ones_f = sb("ones_f", [128, 128], F32)
    k.blk_bf = sb("blk_bf", [128, 128], BF16)
    k.blk_f = sb("blk_f", [128, 128], F32)
    k.iota_e = sb("iota_e", [128, NE], F32)
    ij = k.iota_jp[:, 0:128]
    for t, op, key in ((k.ident_bf, ALU.is_equal, "ident_bf"), (k.ident_f, ALU.is_equal, "ident_f"),
                       (k.m_incl, ALU.is_ge, "m_incl"), (k.m_strict, ALU.is_gt, "m_strict"),
                       (k.m_incl_bf, ALU.is_ge, "m_incl_bf"), (k.m_strict_bf, ALU.is_gt, "m_strict_bf")):
        S.op("dve", lambda e, t=t, op=op: e.tensor_single_scalar(t[:], ij, 0.0, op),
             reads=["iota_jp"], writes=[key])
    S.op("dve", lambda e: e.memset(k.ones_bf[:], 1.0), writes=["ones_bf"])
    S.op("dve", lambda e: e.memset(k.ones_f[:], 1.0), writes=["ones_f"])
    for t, key in ((k.blk_bf, "blk_bf"), (k.blk_f, "blk_f")):
        S.op("dve", lambda e, t=t: e.memset(t[:], 0.0), writes=[key])
        S.op("dve", lambda e, t=t: e.memset(t[0:64, 0:64], 1.0), writes=[key])
        S.op("dve", lambda e, t=t: e.memset(t[64:128, 64:128], 1.0), writes=[key])
    S.op("pool", lambda e: e.iota(k.iota_e[:], [[1, NE]], base=0, channel_multiplier=0,
                                  allow_small_or_imprecise_dtypes=True), writes=["iota_e"])
    k.iota4 = sb("iota4", [128, 4, 128], F32)
    S.op("pool", lambda e: e.iota(k.iota4[:], [[0, 4], [1, 128]], base=0, channel_multiplier=-1,
                                  allow_small_or_imprecise_dtypes=True), writes=["iota4"])
    k.ms4 = sb("ms4", [128, 4, 128], BF16)
    k.ml4 = sb("ml4", [128, 4, 128], BF16)
    k.mi4 = sb("mi4", [128, 4, 128], BF16)
    k.I4 = sb("I4", [128, 4, 128], F32)
    for t, op, key in ((k.ms4, ALU.is_gt, "ms4"), (k.ml4, ALU.is_lt, "ml4"), (k.mi4, ALU.is_ge, "mi4"), (k.I4, ALU.is_equal, "I4")):
        S.op("dve", lambda e, t=t, op=op: e.tensor_single_scalar(t[:], k.iota4[:], 0.0, op),
             reads=["iota4"], writes=[key])
    k.eps_gn = sb("eps_gn", [128, 1], F32)
    S.op("dve", lambda e: e.memset(k.eps_gn[:], 64e-5), writes=["eps_gn"])
    k.eps_rms = sb("eps_rms", [128, 1], F32)
    S.op("dve", lambda e: e.memset(k.eps_rms[:], 1e-5), writes=["eps"])


def phase_a_setup(k):
    nc, S, D = k.nc, k.S, k.D
    sb = lambda n, s, d=F32: k.sb(n, s, d, k.pst)
    k.xT = sb("xT_sb", [128, 16, SEQ], BF16)
    xv = D["xT"].rearrange("(c p) t -> p c t", p=128)
    for i in range(4):
        S.dma("pool", k.xT[:, 4 * i:4 * i + 4, :], xv[:, 4 * i:4 * i + 4, :], "xT%d" % i, writes=[("xT", i)])
    k.xT_keys = [("xT", i) for i in range(4)]
    k.wfm = [sb("wfm%d" % i, [128, 16, 128], BF16) for i in range(2)]
    k.wfm_n = 0
    k.mu = sb("mu_sb", [128, 27]); k.omm = sb("omm_sb", [128, 27])
    S.dma("sp", k.mu[:], D["mu"], "mu", writes=["mu"])
    S.op("dve", lambda e: e.tensor_scalar(k.omm[:], k.mu[:], -1.0, 1.0, ALU.mult, ALU.add),
         reads=["mu"], writes=["omm"])


def load_wfm(k, tile):
    S, D = k.S, k.D
    i = k.wfm_n % 2
    k.wfm_n += 1
    S.dma("pool", k.wfm[i][:], D["w_fm"][tile].rearrange("p (c n) -> p c n", n=128), "wfm%d" % i,
          writes=[("wfm", i)])
    return k.wfm[i], ("wfm", i)


def inproj_fm(k, tile, ps_pair, evac, ncols=128):
    S = k.S
    wt, wkey = load_wfm(k, tile)
    for g in range(4):
        pst, pkey = ps_pair[g % 2]
        for dc in range(16):
            S.op("pe", lambda e: e.matmul(pst[0:ncols, :], wt[:, dc, 0:ncols], k.xT[:, dc, g * 512:(g + 1) * 512],
                                          start=(dc == 0), stop=(dc == 15)),
                 reads=[wkey, ("xT", dc // 4)], writes=[pkey], inc=(dc == 15))
        evac(g, pst[0:ncols, :], pkey)


def phase_diff(k, st2):
    nc, S, D = k.nc, k.S, k.D
    sb = lambda n, s, d=F32: k.sb(n, s, d, st2)
    ps = lambda n, s, d=F32: k.ps(n, s, d, st2)
    ps_s = [(ps("dps_s%d" % i, [128, 512]), ("dps_s", i)) for i in range(3)]
    ps_ms = (ps("dps_ms", [128, 512]), ("dps_ms", 0))
    ps_in = ps_s[0:2]
    ps_o = [(ps("dps_o%d" % i, [128, 512]), ("dps_o", i)) for i in range(2)]
    ps_l = [(ps("dps_l%d" % i, [128, 512]), ("dps_l", i)) for i in range(2)]
    qk = [sb("qk%d" % i, [128, 2, SEQ], BF16) for i in range(2)]
    v4 = sb("v4", [128, 16, 512], BF16)
    wv = sb("wv", [128, 16, 512], BF16)
    pT = [sb("pT%d" % i, [128, 512], BF16) for i in range(3)]
    rl = sb("rl", [128, 512]); o0 = sb("o0", [128, 512]); o1 = sb("o1", [128, 512]); oo = sb("oo", [128, 512])
    sq = sb("sq", [128, 512], BF16); rstd = sb("rstd", [128, 512])
    hst = [sb("hst%d" % i, [128, 512], BF16) for i in range(2)]
    lqk = sb("lqk_sb", [128, 256]); prod = sb("lprod", [128, 128]); s12 = sb("ls12", [128, 2]); e12 = sb("le12", [128, 2])
    nlam = sb("nlam", [128, 1]); sgs = sb("sgs", [128, 1]); sgin = sb("sgin", [128, 1])
    S.dma("sp", lqk[:], D["lqk"][0].partition_broadcast(128), "lqk", writes=["lqk"])
    S.dma("sp", sgin[:], D["subln"], "sgin", writes=["sgin"])
    S.op("dve", lambda e: e.tensor_tensor(prod[:, 0:64], lqk[:, 0:64], lqk[:, 64:128], ALU.mult), reads=["lqk"], writes=["lprod"])
    S.op("dve", lambda e: e.tensor_tensor(prod[:, 64:128], lqk[:, 128:192], lqk[:, 192:256], ALU.mult), reads=["lqk"], writes=["lprod"])
    S.op("dve", lambda e: e.reduce_sum(s12[:], prod[:].rearrange("p (a n) -> p a n", a=2), AX.X), reads=["lprod"], writes=["ls12"])
    S.op("act", lambda e: e.activation(e12[:], s12[:], AF.Exp), reads=["ls12"], writes=["le12"])
    S.op("dve", lambda e: e.tensor_tensor(nlam[:], e12[:, 1:2], e12[:, 0:1], ALU.subtract), reads=["le12"], writes=["nlam"])
    S.op("dve", lambda e: e.tensor_scalar_add(nlam[:], nlam[:], -LAM_INIT), reads=["nlam"], writes=["nlam"])
    S.op("dve", lambda e: e.tensor_scalar_mul(sgs[:], sgin[:], 1.0 - LAM_INIT), reads=["sgin"], writes=["sgs"])

    if k.cfg.get("zfill", True):
        for e_ in range(NE):
            S.dma("sp", D["xdisp_d"][e_ * CAP:(e_ + 1) * CAP, :], D["zeros"], "zfill", writes=["xdisp"])
    for h in range(k.cfg.get("diff_heads", 8)):
        hh = h % 4
        if hh == 0:
            S.dma("pool", wv[:], D["w_v"][h // 4].rearrange("p (c n) -> p c n", n=512), "wv", writes=["wv"])
            for tc in range(16):
                pst, pkey = ps_in[tc % 2]
                for dc in range(16):
                    S.op("pe", lambda e: e.matmul(pst[:], k.xT[:, dc, tc * 128:(tc + 1) * 128], wv[:, dc, :],
                                                  start=(dc == 0), stop=(dc == 15)),
                         reads=["wv", ("xT", dc // 4)], writes=[pkey], inc=(dc == 15))
                S.op("act", lambda e: e.copy(v4[:, tc, :], pst[:]), reads=[pkey], writes=[("v4", tc)])
        qkb = qk[h % 2]
        qkey = ("qk", h % 2)
        for which, tile in ((0, 27 + h), (1, 35 + h)):
            def evac(g, pap, pkey, which=which):
                S.op("act", lambda e: e.copy(qkb[:, which, g * 512:(g + 1) * 512], pap), reads=[pkey], writes=[qkey])
            inproj_fm(k, tile, ps_in, evac)
        tiles = [(m, g, j) for g in range(4) for m in range(2) for j in range(4 * g + 4)]

        def emit_qk(n):
            m, g, j = tiles[n]
            i = j - 4 * g
            q0 = 128 * i if i > 0 else 0
            pst, pkey = ps_s[n % 3]
            S.op("pe", lambda e: e.matmul(pst[:, q0:512], qkb[64 * m:64 * m + 64, 1, j * 128:(j + 1) * 128],
                                          qkb[64 * m:64 * m + 64, 0, g * 512 + q0:(g + 1) * 512], start=True, stop=True),
                 reads=[qkey], writes=[pkey], rg=m)
        emit_qk(0)
        emit_qk(1)
        for n, (m, g, j) in enumerate(tiles):
            if n + 2 < len(tiles):
                emit_qk(n + 2)
            i = j - 4 * g
            q0 = 128 * i if i > 0 else 0
            pst, pkey = ps_s[n % 3]
            pt = pT[n % 3]
            ptk = ("pT", n % 3)
            acc = (2 * g + m) % 2
            S.op("act", lambda e: e.activation(pt[:, q0:512], pst[:, q0:512], AF.Exp, scale=SCALE), reads=[pkey], writes=[ptk])
            if i >= 0:
                S.op("pool", lambda e: e.tensor_tensor(pt[:, q0:q0 + 128], pt[:, q0:q0 + 128], k.m_incl_bf[:], ALU.mult),
                     reads=[ptk, "m_incl_bf"], writes=[ptk])
            last = (j == 4 * g + 3)
            S.op("pe", lambda e: e.matmul(ps_o[acc][0][:, q0:512], v4[:, j, hh * 128:(hh + 1) * 128], pt[:, q0:512],
                                          start=(j == 0), stop=last),
                 reads=[ptk, ("v4", j)], writes=[ps_o[acc][1]], inc=False)
            S.op("pe", lambda e: e.matmul(ps_l[acc][0][:, q0:512], k.ones_bf[:], pt[:, q0:512], start=(j == 0), stop=last),
                 reads=[ptk, "ones_bf"], writes=[ps_l[acc][1]], inc=True)
            if last:
                S.op("dve", lambda e: e.reciprocal(rl[:], ps_l[acc][0][:]), reads=[ps_l[acc][1]], writes=["rl"])
                if m == 0:
                    S.op("dve", lambda e: e.tensor_tensor(o0[:], ps_o[acc][0][:], rl[:], ALU.mult),
                         reads=[ps_o[acc][1], "rl"], writes=["o0"])
                else:
                    S.op("dve", lambda e: e.tensor_tensor(o1[:], ps_o[acc][0][:], rl[:], ALU.mult),
                         reads=[ps_o[acc][1], "rl"], writes=["o1"])
                    S.op("dve", lambda e: e.scalar_tensor_tensor(oo[:], o1[:], nlam[:], o0[:], ALU.mult, ALU.add),
                         reads=["o0", "o1", "nlam"], writes=["oo"])
                    S.op("pool", lambda e: e.tensor_tensor(sq[:], oo[:], oo[:], ALU.mult), reads=["oo"], writes=["sq"])
                    mp, mkey = ps_ms
                    S.op("pe", lambda e: e.matmul(mp[:], k.ones_bf[:], sq[:], start=True, stop=True),
                         reads=["sq", "ones_bf"], writes=[mkey])
                    S.op("act", lambda e: e.activation(rstd[:], mp[:], AF.Sqrt, bias=k.eps_rms[:], scale=1.0 / 128),
                         reads=[mkey, "eps"], writes=["rstd"])
                    S.op("dve", lambda e: e.reciprocal(rstd[:], rstd[:]), reads=["rstd"], writes=["rstd"])
                    hs = hst[g % 2]
                    S.op("dve", lambda e: e.scalar_tensor_tensor(hs[:], oo[:], sgs[:], rstd[:], ALU.mult, ALU.mult),
                         reads=["oo", "sgs", "rstd"], writes=[("hst", g % 2)])
                    S.dma("sp", D["hT_d"][8 + h, :, g * 512:(g + 1) * 512], hs[:], "hst%d" % (g % 2), reads=[("hst", g % 2)])


def phase_rwkv_pro(k, st2):
    nc, S, D = k.nc, k.S, k.D
    sb = lambda n, s, d=F32: k.sb(n, s, d, st2)
    ps = lambda n, s, d=F32: k.ps(n, s, d, st2)
    npairs = k.cfg.get("rwkv_pairs", 8)
    ps_in = [(ps("rps_in%d" % i, [128, 512]), ("rps_in", i)) for i in range(2)]
    NB = {}
    for n in ("Br", "Bk", "B1", "B4", "B5", "B6", "B8"):
        NB[n] = sb("rw_" + n, [128, SEQ])
    raw = sb("rw_raw", [128, SEQ + 4])
    NB["B2"] = raw[:, 1:SEQ + 1]
    NB["Bo"] = NB["B6"]; NB["Bg"] = NB["B5"]
    E2 = sb("rw_E2", [128, SEQ])
    RA = sb("rw_RA", [128, 16, 256], BF16)
    BT = sb("rw_BT", [128, SEQ], BF16); KT = sb("rw_KT", [128, SEQ], BF16); VB = sb("rw_VB", [128, SEQ], BF16)
    tb = sb("rw_tb", [128, SEQ], BF16); sqb = tb
    LW = sb("rw_LW", [128, SEQ], BF16); SG1 = sb("rw_SG1", [128, SEQ], BF16); SG2 = sb("rw_SG2", [32, SEQ], BF16)
    wup = sb("rw_wup", [64, 1024], BF16); aup = sb("rw_aup", [128, 1024], BF16)
    gup1 = sb("rw_gup1", [128, 1024], BF16); gup2 = sb("rw_gup2", [32, 1024], BF16)
    chv = sb("rw_chv", [128, 56]); omka = sb("rw_omka", [128, 8]); glt = sb("rw_glt", [128, 16])
    S.dma("sp", chv[:], D["chv"], "chv", writes=["chv"])
    S.dma("pool", wup[:], D["w_up"], "wup", writes=["wup"])
    S.dma("pool", aup[64:128, :], D["a_up"], "aup", writes=["aup"])
    S.dma("pool", gup1[:], D["g_up"][0:128, :], "gup1", writes=["gup1"])
    S.dma("pool", gup2[:], D["g_up"][128:160, :], "gup2", writes=["gup2"])
    S.op("dve", lambda e: e.tensor_scalar(omka[:], chv[:, 24:32], -1.0, 1.0, ALU.mult, ALU.add), reads=["chv"], writes=["omka"])
    S.op("dve", lambda e: e.memset(raw[:, 0:1], 0.0), writes=["B2"])
    CW0, CA0, CKK, CKA, CGG, CGB, CRK = [8 * i for i in range(7)]

    def proj(tile, dst, dkey, ncols=128):
        def evac(g, pap, pkey):
            S.op("act", lambda e: e.copy(raw[0:ncols, 1 + g * 512:1 + (g + 1) * 512], pap), reads=[pkey], writes=["B2"])
        inproj_fm(k, tile, ps_in, evac, ncols)
        S.op("dve", lambda e: e.tensor_scalar_mul(dst[0:ncols, :], raw[0:ncols, 0:SEQ], k.mu[0:ncols, tile:tile + 1]),
             reads=["B2", "mu"], writes=[dkey])
        S.op("dve", lambda e: e.scalar_tensor_tensor(dst[0:ncols, :], raw[0:ncols, 1:SEQ + 1], k.omm[0:ncols, tile:tile + 1],
                                                     dst[0:ncols, :], ALU.mult, ALU.add),
             reads=["B2", "omm", dkey], writes=[dkey])

    B1 = NB["B1"]
    proj(24, B1, "B1")
    S.op("act", lambda e: e.activation(LW[0:64, :], B1[0:64, :], AF.Tanh), reads=["B1"], writes=["LW"])
    S.op("act", lambda e: e.copy(LW[64:128, :], B1[64:128, :]), reads=["B1"], writes=["LW"])
    proj(25, B1, "B1")
    S.op("act", lambda e: e.activation(SG1[:], B1[:], AF.Sigmoid), reads=["B1"], writes=["SG1"])
    proj(26, B1, "B1", ncols=32)
    S.op("act", lambda e: e.activation(SG2[:], B1[0:32, :], AF.Sigmoid), reads=["B1"], writes=["SG2"])

    def gs(g):
        return slice(g * 512, (g + 1) * 512)

    for u in range(npairs):
        cs = slice(u * 128, (u + 1) * 128)
        Br, Bk, B2, B4, B5, B6, B8, Bo, Bg = (NB[n] for n in ("Br", "Bk", "B2", "B4", "B5", "B6", "B8", "Bo", "Bg"))
        proj(u, Br, "Br"); proj(8 + u, Bk, "Bk"); proj(16 + u, B8, "B8")
        S.op("act", lambda e: e.copy(VB[:], B8[:]), reads=["B8"], writes=["VB"])
        for g in range(4):
            pst, pkey = ps_in[g % 2]
            S.op("pe", lambda e: e.matmul(pst[:], wup[0:64, cs], LW[0:64, gs(g)], start=True, stop=True),
                 reads=["wup", "LW"], writes=[pkey])
            S.op("act", lambda e: e.activation(B1[:, gs(g)], pst[:], AF.Sigmoid, bias=chv[:, CW0 + u:CW0 + u + 1]),
                 reads=[pkey, "chv"], writes=["B1"])
        S.op("dve", lambda e: e.tensor_scalar_mul(B1[:], B1[:], -EXPM05), reads=["B1"], writes=["B1"])
        for n in range(16):
            S.op("dve", lambda e: e.tensor_tensor_scan(B2[:, n * 128:(n + 1) * 128], k.ones_f[:], B1[:, n * 128:(n + 1) * 128],
                                                       0.0, ALU.mult, ALU.add), reads=["B1", "ones_f"], writes=["B2"])
        for g in range(4):
            pst, pkey = ps_in[g % 2]
            S.op("pe", lambda e: e.matmul(pst[:], aup[64:128, cs], LW[64:128, gs(g)], start=True, stop=True),
                 reads=["aup", "LW"], writes=[pkey])
            S.op("act", lambda e: e.activation(B4[:, gs(g)], pst[:], AF.Sigmoid, bias=chv[:, CA0 + u:CA0 + u + 1]),
                 reads=[pkey, "chv"], writes=["B4"])
        S.op("dve", lambda e: e.tensor_scalar_mul(B5[:], Bk[:], chv[:, CKK + u:CKK + u + 1]), reads=["Bk", "chv"], writes=["B5"])
        S.op("pool", lambda e: e.tensor_tensor(sqb[:], B5[:], B5[:], ALU.mult), reads=["B5"], writes=["tb"])
        for g in range(4):
            pst, pkey = ps_in[g % 2]
            S.op("pe", lambda e: e.matmul(pst[:], k.blk_bf[:], sqb[:, gs(g)], start=True, stop=True),
                 reads=["blk_bf", "tb"], writes=[pkey])
            S.op("act", lambda e: e.activation(B6[:, gs(g)], pst[:], AF.Sqrt), reads=[pkey], writes=["B6"])
        S.op("dve", lambda e: e.tensor_scalar_max(B6[:], B6[:], 1e-12), reads=["B6"], writes=["B6"])
        S.op("dve", lambda e: e.reciprocal(B6[:], B6[:]), reads=["B6"], writes=["B6"])
        S.op("dve", lambda e: e.tensor_tensor(B5[:], B5[:], B6[:], ALU.mult), reads=["B5", "B6"], writes=["B5"])
        S.op("dve", lambda e: e.tensor_scalar(B6[:], B4[:], chv[:, CKA + u:CKA + u + 1], omka[:, u:u + 1], ALU.mult, ALU.add),
             reads=["B4", "chv", "omka"], writes=["B6"])
        S.op("pool", lambda e: e.tensor_tensor(Bk[:], Bk[:], B6[:], ALU.mult), reads=["Bk", "B6"], writes=["Bk"])
        S.op("act", lambda e: e.activation(B6[:], B2[:], AF.Exp), reads=["B2"], writes=["B6"])
        S.op("act", lambda e: e.activation(E2[:], B2[:], AF.Exp, scale=-1.0), reads=["B2"], writes=["E2"])
        S.op("dve", lambda e: e.tensor_tensor(B8[:], B2[:], B1[:], ALU.subtract), reads=["B2", "B1"], writes=["B8"])
        S.op("act", lambda e: e.activation(B8[:], B8[:], AF.Exp), reads=["B8"], writes=["B8"])
        S.op("dve", lambda e: e.tensor_copy(glt[:], B6[:].rearrange("p (c l) -> p c l", l=128)[:, :, 127]),
             reads=["B6"], writes=["glt"])
        S.op("pool", lambda e: e.tensor_tensor(RA[:, :, 0:128], Br[:].rearrange("p (c l) -> p c l", l=128),
                                               B6[:].rearrange("p (c l) -> p c l", l=128), ALU.mult),
             reads=["Br", "B6"], writes=["RA"])
        S.op("dve", lambda e: e.scalar_tensor_tensor(RA[:, :, 128:256], B5[:].rearrange("p (c l) -> p c l", l=128), -1.0,
                                                     B8[:].rearrange("p (c l) -> p c l", l=128), ALU.mult, ALU.mult),
             reads=["B5", "B8"], writes=["RA"])
        S.op("dve", lambda e: e.tensor_tensor(B5[:], B5[:], B4[:], ALU.mult), reads=["B5", "B4"], writes=["B5"])
        S.op("pool", lambda e: e.tensor_tensor(BT[:], B5[:], E2[:], ALU.mult), reads=["B5", "E2"], writes=["BT"])
        S.op("dve", lambda e: e.tensor_tensor(KT[:], Bk[:], E2[:], ALU.mult), reads=["Bk", "E2"], writes=["KT"])
        S.op("dve", lambda e: e.scalar_tensor_tensor(tb[:], Br[:], chv[:, CRK + u:CRK + u + 1], Bk[:], ALU.mult, ALU.mult),
             reads=["Br", "Bk", "chv"], writes=["tb"])
        for g in range(4):
            pst, pkey = ps_in[g % 2]
            S.op("pe", lambda e: e.matmul(pst[:], k.blk_bf[:], tb[:, gs(g)], start=True, stop=True),
                 reads=["blk_bf", "tb"], writes=[pkey])
            S.op("dve", lambda e: e.tensor_tensor(Bo[:, gs(g)], pst[:], VB[:, gs(g)], ALU.mult), reads=[pkey, "VB"], writes=["B6"])
        for g in range(4):
            pst, pkey = ps_in[g % 2]
            S.op("pe", lambda e: e.matmul(pst[:], gup1[:, cs], SG1[:, gs(g)], start=True, stop=False),
                 reads=["gup1", "SG1"], writes=[pkey], inc=False)
            S.op("pe", lambda e: e.matmul(pst[:], gup2[:, cs], SG2[:, gs(g)], start=False, stop=True),
                 reads=["gup2", "SG2"], writes=[pkey])
            S.op("act", lambda e: e.copy(Bg[:, gs(g)], pst[:]), reads=[pkey], writes=["B5"])
        S.dma("sp", D["ra_d"][u], RA[:].rearrange("p c n -> p (c n)"), "st_ra", reads=["RA"])
        S.dma("sp", D["bt_d"][u], BT[:], "st_bt", reads=["BT"])
        S.dma("sp", D["kt_d"][u], KT[:], "st_kt", reads=["KT"])
        S.dma("sp", D["vb_d"][u], VB[:], "st_vb", reads=["VB"])
        S.dma("sp", D["bo_d"][u], Bo[:], "st_bo", reads=["B6"])
        S.dma("sp", D["bg_d"][u], Bg[:], "st_bg", reads=["B5"])
        S.dma("sp", D["gl_d"][u], glt[:], "st_gl", reads=["glt"])


def phase_rwkv_scan(k, st2):
    nc, S, D = k.nc, k.S, k.D
    sb = lambda n, s, d=F32: k.sb(n, s, d, st2)
    ps = lambda n, s, d=F32: k.ps(n, s, d, st2)
    npairs = k.cfg.get("rwkv_pairs", 8)
    scm = k.cfg.get("sc_mode", 3)
    P1 = ps("sc_P1", [128, 4, 128]); P2 = ps("sc_P2", [128, 4, 128])
    PA = [ps("sc_PA%d" % i, [128, 4, 128]) for i in range(3)]
    PC = ps("sc_PC", [128, 4, 128])
    PM = [ps("sc_PM%d" % i, [128, 512]) for i in range(2)]
    chv = sb("sc_chv", [128, 56])
    S.dma("sp", chv[:], D["chv"], "chv2", writes=["chv2"])
    CGG, CGB = 32, 40
    RA = [sb("sc_RA%d" % i, [128, 16, 256], BF16) for i in range(2)]
    BT = [sb("sc_BT%d" % i, [128, SEQ], BF16) for i in range(2)]
    KT = [sb("sc_KT%d" % i, [128, SEQ], BF16) for i in range(2)]
    VB = [sb("sc_VB%d" % i, [128, SEQ], BF16) for i in range(2)]
    GL = [sb("sc_GL%d" % i, [128, 16]) for i in range(2)]
    TT = [sb("sc_TT%d" % i, [128, 16, 2, 128], BF16) for i in range(2)]
    ACH = [sb("sc_ACH%d" % i, [128, 16, 2, 3, 128], BF16) for i in range(2)]
    TOK = [sb("sc_TOK%d" % i, [128, 16, 3, 128], BF16) for i in range(2)]
    VPAD = [sb("sc_VPAD%d" % i, [128, 16, 2, 128], BF16) for i in range(2)]
    for i in range(2):
        S.op("pool", lambda e: e.memset(VPAD[i][:], 0.0), writes=[("VPAD", i)])
    Xb = [sb("sc_X%d" % i, [128, 4, 128], BF16) for i in range(2)]
    XTb = [sb("sc_XT%d" % i, [128, 4, 128], BF16) for i in range(2)]
    Pf = sb("sc_Pf", [128, 4, 128]); Pbf = sb("sc_Pbf", [128, 4, 128], BF16)
    Wsb = sb("sc_W", [128, 128], BF16); Ut = sb("sc_Ut", [128, 128], BF16)
    UPAD = [sb("sc_UPAD%d" % i, [128, 2, 128], BF16) for i in range(2)]
    for i in range(2):
        S.op("pool", lambda e: e.memset(UPAD[i][:], 0.0), writes=[("UPAD", i)])
    Sf = sb("sc_Sf", [128, 128]); t1 = sb("sc_t1", [128, 128])
    Sbf = [sb("sc_Sbf%d" % i, [128, 128], BF16) for i in range(2)]
    Yf = sb("sc_Yf", [128, SEQ]); Bo = sb("sc_Bo", [128, SEQ]); Bg = sb("sc_Bg", [128, SEQ])
    yb = sb("sc_yb", [128, SEQ], BF16); yc = sb("sc_yc", [128, SEQ]); rs = Yf
    hb = sb("sc_hb", [128, SEQ], BF16)

    def load(u):
        pp = u % 2
        S.dma("sp", RA[pp][:].rearrange("p c n -> p (c n)"), D["ra_d"][u], "ld_ra%d" % pp, writes=[("RA", pp)])
        S.dma("sp", BT[pp][:], D["bt_d"][u], "ld_bt%d" % pp, writes=[("BT", pp)])
        S.dma("sp", KT[pp][:], D["kt_d"][u], "ld_kt%d" % pp, writes=[("KT", pp)])
        S.dma("sp", VB[pp][:], D["vb_d"][u], "ld_vb%d" % pp, writes=[("VB", pp)])
        S.dma("sp", GL[pp][:], D["gl_d"][u], "ld_gl%d" % pp, writes=[("GL", pp)])

    def stage1(u):
        pp = u % 2
        ra, bt, kt, vb = RA[pp], BT[pp], KT[pp], VB[pp]
        rkeys = [("RA", pp), ("BT", pp), ("KT", pp), ("VB", pp)]
        for cp in range(8):
            sysl0 = [(2 * ci + hd, ci, hd) for hd in range(2) for ci in range(2)]
            sysl1 = [(2 * ci + hd, ci, hd) for hd in (1, 0) for ci in range(2)]
            sysl = sysl0
            for q, ci, hd in sysl0:
                n = 2 * cp + ci
                ph = slice(64 * hd, 64 * hd + 64)
                cn = slice(n * 128, (n + 1) * 128)
                S.op("pe", lambda e: e.matmul(P1[:, q, :], bt[ph, cn], ra[ph, n, 128:256], start=True, stop=True),
                     reads=rkeys, writes=["P1"], inc=(ci == 1), rg=hd)
            for q, ci, hd in sysl1:
                n = 2 * cp + ci
                ph = slice(64 * hd, 64 * hd + 64)
                cn = slice(n * 128, (n + 1) * 128)
                S.op("pe", lambda e: e.matmul(P2[:, q, :], ra[ph, n, 128:256], bt[ph, cn], start=True, stop=True),
                     reads=rkeys, writes=["P2"], inc=(ci == 1), rg=hd)
            for kind, (lt, rsl) in enumerate(((kt, slice(128, 256)), (bt, slice(0, 128)), (kt, slice(0, 128)))):
                for q, ci, hd in (sysl0 if kind % 2 == 0 else sysl1):
                    n = 2 * cp + ci
                    ph = slice(64 * hd, 64 * hd + 64)
                    cn = slice(n * 128, (n + 1) * 128)
                    S.op("pe", lambda e: e.matmul(PA[kind][:, q, :], lt[ph, cn], ra[ph, n, rsl], start=True, stop=True),
                         reads=rkeys, writes=[("PA", kind)], inc=(ci == 1), rg=hd)
            S.op("dve", lambda e: e.tensor_tensor(Xb[0][:], P1[:], k.ms4[:], ALU.mult), reads=["P1", "ms4"], writes=[("X", 0)])
            S.op("dve", lambda e: e.tensor_tensor(XTb[0][:], P2[:], k.ml4[:], ALU.mult), reads=["P2", "ml4"], writes=[("XT", 0)])
            ach = ACH[pp][:, 2 * cp:2 * cp + 2, :, :, :].rearrange("p c h k t -> p (c h) k t")
            for kind, mk, mkey in ((0, k.ms4, "ms4"), (1, k.mi4, "mi4"), (2, k.mi4, "mi4")):
                S.op("act", lambda e: e.copy(ach[:, :, kind, :], PA[kind][:]), reads=[("PA", kind)], writes=[("ACH", pp, cp)])
                S.op("pool", lambda e: e.tensor_tensor(ach[:, :, kind, :], ach[:, :, kind, :], mk[:], ALU.mult),
                     reads=[("ACH", pp, cp), mkey], writes=[("ACH", pp, cp)])
            S.op("dve", lambda e: e.tensor_tensor(Pf[:], Xb[0][:], k.I4[:], ALU.add), reads=[("X", 0), "I4"], writes=["Pf"])
            S.op("act", lambda e: e.copy(Pbf[:], Pf[:]), reads=["Pf"], writes=["Pbf"])
            if scm < 1:
                yield
                continue
            pT = PM[0][:].bitcast(BF16)
            for ci in range(2):
                n = 2 * cp + ci
                cn = slice(n * 128, (n + 1) * 128)
                for j, src in enumerate((bt, kt, vb)):
                    S.op("pe", lambda e: e.transpose(pT[:, (ci * 3 + j) * 128:(ci * 3 + j + 1) * 128], src[:, cn], k.ident_bf[:]),
                         reads=rkeys + ["ident_bf"], writes=[("PM", 0)], inc=(ci == 1 and j == 2))
            S.op("act", lambda e: e.copy(TOK[pp][:, 2 * cp:2 * cp + 2, :, :].rearrange("p c j t -> p (c j t)"), pT[:, 0:768]),
                 reads=[("PM", 0)], writes=[("TOK", pp, cp)])
            pT3 = pT[:, 0:768].rearrange("p (c j t) -> p c j t", c=2, j=3)
            for hd in range(2):
                S.op("act", lambda e: e.copy(VPAD[pp][:, 2 * cp:2 * cp + 2, hd, 64 * hd:64 * hd + 64], pT3[:, :, 2, 64 * hd:64 * hd + 64]),
                     reads=[("PM", 0)], writes=[("VPAD", pp)])
            yield
            for lv in range(0, 7 if scm >= 2 else 0):
                xi, xo = lv % 2, (lv + 1) % 2
                if lv >= 1:
                    for q in range(4):
                        S.op("pe", lambda e: e.matmul(PA[2][:, q, :], XTb[xi][:, q, :], Pbf[:, q, :], start=True, stop=True),
                             reads=[("XT", xi), "Pbf"], writes=[("PA", 2)], inc=(q == 3))
                if lv < 6:
                    for q in range(4):
                        S.op("pe", lambda e: e.matmul(PA[0][:, q, :], XTb[xi][:, q, :], Xb[xi][:, q, :], start=True, stop=True),
                             reads=[("X", xi), ("XT", xi)], writes=[("PA", 0)], inc=(q == 3))
                    for q in range(4):
                        S.op("pe", lambda e: e.matmul(PA[1][:, q, :], Xb[xi][:, q, :], XTb[xi][:, q, :], start=True, stop=True),
                             reads=[("X", xi), ("XT", xi)], writes=[("PA", 1)], inc=(q == 3))
                if lv >= 1:
                    S.op("dve", lambda e: e.tensor_tensor(Pf[:], Pf[:], PA[2][:], ALU.add), reads=["Pf", ("PA", 2)], writes=["Pf"])
                    if lv < 6:
                        S.op("act", lambda e: e.copy(Pbf[:], Pf[:]), reads=["Pf"], writes=["Pbf"])
                    else:
                        S.op("act", lambda e: e.copy(TT[pp][:, 2 * cp:2 * cp + 2, :, :].rearrange("p c h t -> p (c h) t"), Pf[:]),
                             reads=["Pf"], writes=[("TT", pp, cp)])
                if lv < 6:
                    S.op("act", lambda e: e.copy(Xb[xo][:], PA[0][:]), reads=[("PA", 0)], writes=[("X", xo)])
                    S.op("dve", lambda e: e.tensor_copy(XTb[xo][:], PA[1][:]), reads=[("PA", 1)], writes=[("XT", xo)])
                yield

    def chain(u):
        pp = u % 2
        ra = RA[pp]
        if scm < 3:
            return
        S.op("dve", lambda e: e.memset(Sf[:], 0.0), writes=["Sf"])
        S.op("dve", lambda e: e.memset(Sbf[0][:], 0.0), writes=[("Sbf", 0)])
        for n in range(16):
            cp, ci = n // 2, n % 2
            si, so = n % 2, (n + 1) % 2
            cn = slice(n * 128, (n + 1) * 128)
            akey, tkey, ttkey = ("ACH", pp, cp), ("TOK", pp, cp), ("TT", pp, cp)
            S.op("pe", lambda e: e.matmul(PC[:, 0, :], ra[:, n, 128:256], Sbf[si][:], start=True, stop=False),
                 reads=[("RA", pp), ("Sbf", si)], writes=["PC"], inc=False)
            for hd in range(2):
                hs = slice(64 * hd, 64 * hd + 64)
                S.op("pe", lambda e: e.matmul(PC[:, 0, hs], ACH[pp][:, n, hd, 0, :], TOK[pp][:, n, 2, hs], start=False, stop=(hd == 1)),
                     reads=[akey, tkey], writes=["PC"], inc=(hd == 1))
            S.op("act", lambda e: e.copy(Wsb[:], PC[:, 0, :]), reads=["PC"], writes=["Wsb"])
            yield
            import os
            sub = int(os.environ.get("SCSUB", "9"))
            if sub <= 1:
                continue
            for hd in range(2):
                hs = slice(64 * hd, 64 * hd + 64)
                S.op("pe", lambda e: e.matmul(PC[:, 1, hs], TT[pp][:, n, hd, :], Wsb[:, hs], start=True, stop=True),
                     reads=[ttkey, "Wsb"], writes=["PC"], inc=(hd == 1))
            S.op("dve", lambda e: e.tensor_copy(Ut[:], PC[:, 1, :]), reads=["PC"], writes=["Ut"])
            for hd in range(2 if os.environ.get("NOUPAD") is None else 0):
                hs = slice(64 * hd, 64 * hd + 64)
                S.op("dve", lambda e: e.tensor_copy(UPAD[ci][:, hd, hs], PC[:, 1, hs]), reads=["PC"], writes=[("UPAD", ci)])
            yield
            if sub <= 2:
                continue
            PY = PM[1][:, 0:128]
            S.op("pe", lambda e: e.matmul(PY, Sbf[si][:], ra[:, n, 0:128], start=True, stop=False),
                 reads=[("RA", pp), ("Sbf", si)], writes=[("PM", 1)], inc=False)
            for hd in range(2):
                S.op("pe", lambda e: e.matmul(PY, UPAD[ci][:, hd, :], ACH[pp][:, n, hd, 1, :], start=False, stop=False),
                     reads=[("UPAD", ci), akey], writes=[("PM", 1)], inc=False)
                S.op("pe", lambda e: e.matmul(PY, VPAD[pp][:, n, hd, :], ACH[pp][:, n, hd, 2, :], start=False, stop=(hd == 1)),
                     reads=[("VPAD", pp), akey], writes=[("PM", 1)], inc=(hd == 1))
            if sub <= 3:
                S.op("act", lambda e: e.copy(Yf[:, cn], PY), reads=[("PM", 1)], writes=["Yf"])
                continue
            S.op("pe", lambda e: e.matmul(PC[:, 3, :], TOK[pp][:, n, 0, :], Ut[:], start=True, stop=False),
                 reads=[tkey, "Ut"], writes=["PC"], inc=False)
            S.op("pe", lambda e: e.matmul(PC[:, 3, :], TOK[pp][:, n, 1, :], TOK[pp][:, n, 2, :], start=False, stop=True),
                 reads=[tkey], writes=["PC"])
            S.op("act", lambda e: e.copy(Yf[:, cn], PY), reads=[("PM", 1)], writes=["Yf"])
            S.op("dve", lambda e: e.scalar_tensor_tensor(t1[:], PC[:, 3, :], GL[pp][:, n:n + 1], k.blk_f[:], ALU.mult, ALU.mult),
                 reads=["PC", ("GL", pp), "blk_f"], writes=["t1"])
            S.op("dve", lambda e: e.scalar_tensor_tensor(Sbf[so][:], Sf[:], GL[pp][:, n:n + 1], t1[:], ALU.mult, ALU.add),
                 reads=["Sf", "t1", ("GL", pp)], writes=[("Sbf", so)])
            S.op("dve", lambda e: e.scalar_tensor_tensor(Sf[:], Sf[:], GL[pp][:, n:n + 1], t1[:], ALU.mult, ALU.add),
                 reads=["Sf", "t1", ("GL", pp)], writes=["Sf"])
            yield
        if sub <= 4:
            return
        S.dma("sp", Bo[:], D["bo_d"][u], "ld_bo", writes=["Bo"])
        S.dma("sp", Bg[:], D["bg_d"][u], "ld_bg", writes=["Bg"])
        S.op("act", lambda e: e.copy(yb[:], Yf[:]), reads=["Yf"], writes=["yb"])
        for g in range(4):
            gsl = slice(g * 512, (g + 1) * 512)
            S.op("pe", lambda e: e.matmul(PM[1][:], k.blk_bf[:], yb[:, gsl], start=True, stop=True),
                 reads=["blk_bf", "yb"], writes=[("PM", 1)])
            S.op("dve", lambda e: e.scalar_tensor_tensor(yc[:, gsl], PM[1][:], -1.0 / 64, Yf[:, gsl], ALU.mult, ALU.add),
                 reads=[("PM", 1), "Yf"], writes=["yc"])
        S.op("pool", lambda e: e.tensor_tensor(yb[:], yc[:], yc[:], ALU.mult), reads=["yc"], writes=["yb"])
        for g in range(4):
            gsl = slice(g * 512, (g + 1) * 512)
            S.op("pe", lambda e: e.matmul(PM[1][:], k.blk_bf[:], yb[:, gsl], start=True, stop=True),
                 reads=["blk_bf", "yb"], writes=[("PM", 1)])
            S.op("act", lambda e: e.activation(rs[:, gsl], PM[1][:], AF.Sqrt, bias=k.eps_gn[:], scale=1.0 / 64),
                 reads=[("PM", 1), "eps_gn"], writes=["Yf"])
        S.op("dve", lambda e: e.reciprocal(rs[:], rs[:]), reads=["Yf"], writes=["Yf"])
        S.op("dve", lambda e: e.tensor_tensor(yc[:], yc[:], rs[:], ALU.mult), reads=["yc", "Yf"], writes=["yc"])
        S.op("dve", lambda e: e.tensor_scalar(yc[:], yc[:], chv[:, CGG + u:CGG + u + 1], chv[:, CGB + u:CGB + u + 1], ALU.mult, ALU.add),
             reads=["yc", "chv2"], writes=["yc"])
        S.op("pool", lambda e: e.tensor_tensor(yc[:], yc[:], Bo[:], ALU.add), reads=["yc", "Bo"], writes=["yc"])
        S.op("dve", lambda e: e.tensor_tensor(hb[:], yc[:], Bg[:], ALU.mult), reads=["yc", "Bg"], writes=["hb"])
        S.dma("sp", D["hT_d"][u], hb[:], "st_hb", reads=["hb"])
        yield

    def drive(gens):
        gens = [g for g in gens if g is not None]
        while gens:
            for g in list(gens):
                try:
                    next(g)
                except StopIteration:
                    gens.remove(g)

    load(0)
    if scm < 0:
        return
    drive([stage1(0)])
    for u in range(npairs):
        if u + 1 < npairs:
            load(u + 1)
        drive([chain(u), stage1(u + 1) if u + 1 < npairs else None])


def layer_norm_rows(k, z, zkey, gbc, bbc, gbkey, out, okey, tmp):
    S, nc = k.S, k.nc
    st6, mv, rstd, nmr = tmp
    for i in range(4):
        S.op("dve", lambda e: e.bn_stats(st6[:, i, :], z[:, i * 512:(i + 1) * 512]), reads=[zkey], writes=["ln_st6"])
    S.op("dve", lambda e: e.bn_aggr(mv[:], st6[:].rearrange("p a b -> p (a b)")), reads=["ln_st6"], writes=["ln_mv"])
    S.op("act", lambda e: e.activation(rstd[:], mv[:, 1:2], AF.Sqrt, bias=k.eps_ln[:], scale=1.0), reads=["ln_mv", "eps_ln"], writes=["ln_rstd"])
    S.op("dve", lambda e: e.reciprocal(rstd[:], rstd[:]), reads=["ln_rstd"], writes=["ln_rstd"])
    S.op("dve", lambda e: e.scalar_tensor_tensor(nmr[:], mv[:, 0:1], -1.0, rstd[:], ALU.mult, ALU.mult),
         reads=["ln_mv", "ln_rstd"], writes=["ln_nmr"])
    S.op("act", lambda e: e.activation(z[:], z[:], AF.Identity, bias=nmr[:], scale=rstd[:]),
         reads=[zkey, "ln_nmr", "ln_rstd"], writes=[zkey])
    S.op("dve", lambda e: e.tensor_tensor(z[:], z[:], gbc[:], ALU.mult), reads=[zkey, gbkey], writes=[zkey])
    S.op("pool", lambda e: e.tensor_tensor(out[:], z[:], bbc[:], ALU.add), reads=[zkey, gbkey], writes=[okey])


def phase_b(k):
    nc, S, D = k.nc, k.S, k.D
    sb = lambda n, s, d=F32: k.sb(n, s, d, k.pst)
    ps = lambda n, s, d=F32: k.ps(n, s, d, k.pst)
    nchunks = k.cfg.get("b_chunks", 16)
    hTs = sb("b_hT", [128, 16, SEQ], BF16)
    wout = sb("b_wout", [128, 16, DM], BF16)
    for i in range(4):
        S.dma("sp", hTs[:, 4 * i:4 * i + 4, :], D["hT_d"][4 * i:4 * i + 4].rearrange("c p t -> p c t"), "b_hT%d" % i, writes=[("hT", i)])
    wov = D["w_out"].rearrange("(c p) n -> p c n", p=128)
    for i in range(4):
        S.dma("pool", wout[:, 4 * i:4 * i + 4, :], wov[:, 4 * i:4 * i + 4, :], "b_wout%d" % i, writes=[("wout", i)])
    gbc = sb("b_gbc", [128, DM]); bbc = sb("b_bbc", [128, DM])
    S.dma("sp", gbc[:], D["ln1"][0].partition_broadcast(128), "b_gbc", writes=["gb1"])
    S.dma("sp", bbc[:], D["ln1"][1].partition_broadcast(128), "b_bbc", writes=["gb1"])
    wr = sb("b_wr", [128, 16, NE]); brt = sb("b_brt", [128, NE])
    S.dma("sp", wr[:], D["w_router"].rearrange("(c p) e -> p c e", p=128), "b_wr", writes=["wr"])
    S.dma("sp", brt[:], D["b_router"][0].partition_broadcast(128), "b_brt", writes=["brt"])
    k.eps_ln = sb("eps_ln", [128, 1])
    S.op("dve", lambda e: e.memset(k.eps_ln[:], 1e-5), writes=["eps_ln"])
    ecap = sb("b_ecap", [128, NE])
    S.op("dve", lambda e: e.tensor_scalar_mul(ecap[:], k.iota_e[:], float(CAP)), reads=["iota_e"], writes=["ecap"])
    carry = sb("b_carry", [128, NE])
    S.op("dve", lambda e: e.memset(carry[:], 0.0), writes=["carry"])
    xc = [sb("b_xc%d" % i, [128, DM]) for i in range(2)]
    z = sb("b_z", [128, DM]); x1 = sb("b_x1", [128, DM]); x1T = sb("b_x1T", [128, 16, 128]); x1b = sb("b_x1b", [128, DM], BF16)
    lntmp = (sb("ln_st6", [128, 4, 6]), sb("ln_mv", [128, 2]), sb("ln_rstd", [128, 1]), sb("ln_nmr", [128, 1]))
    lg = sb("b_lg", [128, NE]); t8 = sb("b_t8", [128, 8]); oh = sb("b_oh", [128, 4, NE]); ma = sb("b_ma", [128, NE])
    mab = sb("b_mab", [128, NE], BF16); negm = sb("b_negm", [128, 1]); ev = sb("b_ev", [128, 4]); esum = sb("b_esum", [128, 1])
    pe_ = sb("b_pe", [128, NE]); ohp = sb("b_ohp", [128, 4, NE]); destf = sb("b_destf", [128, 4]); posk = sb("b_posk", [128, 4])
    ovf = sb("b_ovf", [128, 4])
    psM = [ps("b_psM%d" % i, [128, 512]) for i in range(4)]
    psT = [ps("b_psT%d" % i, [128, 4, 128]) for i in range(2)]
    psL = ps("b_psL", [128, 512])
    def mix_mm(c):
        tc_ = slice(c * 128, (c + 1) * 128)
        S.dma("sp", xc[c % 2][:], D["x"][tc_, :], "b_xc%d" % (c % 2), writes=[("xc", c % 2)])
        for nb in range(4):
            for fc in range(16):
                S.op("pe", lambda e: e.matmul(psM[nb][:], hTs[:, fc, tc_], wout[:, fc, nb * 512:(nb + 1) * 512],
                                              start=(fc == 0), stop=(fc == 15)),
                     reads=[("hT", fc // 4), ("wout", fc // 4)], writes=[("psM", nb)], inc=(fc == 15))

    mix_mm(0)
    for c in range(nchunks):
        tc_ = slice(c * 128, (c + 1) * 128)
        xcb = xc[c % 2]
        for nb in range(4):
            S.op("dve", lambda e: e.scalar_tensor_tensor(z[:, nb * 512:(nb + 1) * 512], xcb[:, nb * 512:(nb + 1) * 512], ALPHA,
                                                         psM[nb][:], ALU.mult, ALU.add),
                 reads=[("psM", nb), ("xc", c % 2)], writes=["z"])
        if c + 1 < nchunks:
            mix_mm(c + 1)
        layer_norm_rows(k, z, "z", gbc, bbc, "gb1", x1, "x1", lntmp)
        S.dma("sp", D["x1f_d"][tc_, :], x1[:], "b_x1st", reads=["x1"])
        S.op("act", lambda e: e.copy(x1b[:], x1[:]), reads=["x1"], writes=["x1b"])
        for r in range(4):
            pt = psT[r % 2]
            for i in range(4):
                dc = 4 * r + i
                S.op("pe", lambda e: e.transpose(pt[:, i, :], x1[:, dc * 128:(dc + 1) * 128], k.ident_f[:]),
                     reads=["x1", "ident_f"], writes=[("psT", r % 2)], inc=(i == 3))
            S.op("act", lambda e: e.copy(x1T[:, 4 * r:4 * r + 4, :], pt[:]), reads=[("psT", r % 2)], writes=["x1T"])
        for dc in range(16):
            S.op("pe", lambda e: e.matmul(psL[:, 0:NE], x1T[:, dc, :], wr[:, dc, :], start=(dc == 0), stop=(dc == 15)),
                 reads=["x1T", "wr"], writes=["psL"], inc=(dc == 15))
        S.op("dve", lambda e: e.tensor_tensor(lg[:], psL[:, 0:NE], brt[:], ALU.add), reads=["psL", "brt"], writes=["lg"])
        S.op("dve", lambda e: e.max(t8[:], lg[:]), reads=["lg"], writes=["t8"])
        for kk in range(4):
            S.op("dve", lambda e: e.tensor_scalar(oh[:, kk, :], lg[:], t8[:, kk:kk + 1], None, ALU.is_equal),
                 reads=["lg", "t8"], writes=["oh"])
        S.op("dve", lambda e: e.reduce_sum(ma[:], oh[:].rearrange("p k e -> p e k"), AX.X), reads=["oh"], writes=["ma"])
        S.op("dve", lambda e: e.tensor_copy(mab[:], ma[:]), reads=["ma"], writes=["mab"])
        S.op("dve", lambda e: e.tensor_scalar_mul(negm[:], t8[:, 0:1], -1.0), reads=["t8"], writes=["negm"])
        S.op("act", lambda e: e.activation(ev[:], t8[:, 0:4], AF.Exp, bias=negm[:], scale=1.0), reads=["t8", "negm"], writes=["ev"])
        S.op("dve", lambda e: e.reduce_sum(esum[:], ev[:], AX.X), reads=["ev"], writes=["esum"])
        S.op("dve", lambda e: e.reciprocal(esum[:], esum[:]), reads=["esum"], writes=["esum"])
        S.op("dve", lambda e: e.tensor_scalar_mul(k.gates_all[:, c, :], ev[:], esum[:, 0:1]), reads=["ev", "esum"], writes=["gates_all"])
        S.op("pe", lambda e: e.matmul(psL[:, 32:64], k.m_strict_bf[:], mab[:], start=True, stop=True),
             reads=["m_strict_bf", "mab"], writes=["psL"], inc=False)
        S.op("pe", lambda e: e.matmul(psL[:, 64:96], k.ones_bf[:], mab[:], start=True, stop=True),
             reads=["ones_bf", "mab"], writes=["psL"])
        S.op("dve", lambda e: e.tensor_tensor(pe_[:], psL[:, 32:64], carry[:], ALU.add), reads=["psL", "carry"], writes=["pe"])
        S.op("dve", lambda e: e.tensor_tensor(carry[:], psL[:, 64:96], carry[:], ALU.add), reads=["psL", "carry"], writes=["carry"])
        for kk in range(4):
            S.op("dve", lambda e: e.tensor_tensor(ohp[:, kk, :], oh[:, kk, :], pe_[:], ALU.mult), reads=["oh", "pe"], writes=["ohp"])
        S.op("dve", lambda e: e.reduce_sum(posk[:], ohp[:], AX.X), reads=["ohp"], writes=["posk"])
        for kk in range(4):
            S.op("dve", lambda e: e.tensor_tensor(ohp[:, kk, :], oh[:, kk, :], ecap[:], ALU.mult), reads=["oh", "ecap"], writes=["ohp"])
        S.op("dve", lambda e: e.reduce_sum(destf[:], ohp[:], AX.X), reads=["ohp"], writes=["destf"])
        S.op("dve", lambda e: e.tensor_scalar_min(posk[:], posk[:], float(CAP - 1)), reads=["posk"], writes=["posk"])
        S.op("dve", lambda e: e.tensor_tensor(destf[:], destf[:], posk[:], ALU.add), reads=["destf", "posk"], writes=["destf"])
        S.op("dve", lambda e: e.tensor_copy(k.dest_all[:, c, :], destf[:]), reads=["destf"], writes=["dest_all"])
        for kk in range(4):
            S.dma("pool", D["xdisp_d"][:, :], x1b[:], "b_disp", reads=["x1b", "dest_all", "xdisp"],
                  indirect=(bass.IndirectOffsetOnAxis(ap=k.dest_all[:, c, kk:kk + 1], axis=0), None))


def phase_c(k):
    nc, S, D = k.nc, k.S, k.D
    sb = lambda n, s, d=F32: k.sb(n, s, d, k.pst)
    ps = lambda n, s, d=F32: k.ps(n, s, d, k.pst)
    nexp = k.cfg.get("c_experts", NE)
    NJ = CAP // 128
    xtok = sb("c_xtok", [128, NJ, DM], BF16)
    xeT = [sb("c_xeT%d" % i, [128, 16, CAP], BF16) for i in range(2)]
    wg = [sb("c_wg%d" % i, [128, 16, 512], BF16) for i in range(3)]
    NWG = 3
    wd = [sb("c_wd%d" % i, [128, 16, 512], BF16) for i in range(2)]
    gg = sb("c_gg", [128, 4, CAP]); gsb = sb("c_gsb", [128, CAP]); sg = sb("c_sg", [128, CAP]); usb = sb("c_usb", [128, CAP])
    actT = sb("c_actT", [128, 16, CAP], BF16)
    ysb = [sb("c_ysb%d" % i, [128, 512]) for i in range(4)]
    nys = [0]
    bdn = sb("c_bdn", [128, DM])
    bgu = sb("c_bgu", [128, NE, 32])
    S.dma("sp", bgu[:].rearrange("p e f -> p (e f)"), D["b_gu_r"], "c_bgu", writes=["bgu"])
    psT = [ps("c_psT%d" % i, [128, 512]) for i in range(2)]
    psG = [ps("c_psG%d" % i, [128, 512]) for i in range(3)]
    psD = [ps("c_psD%d" % i, [128, 512]) for i in range(3)]
    nwg = [0]; nwd = [0]; npg = [0]; npd = [0]

    def load_expert_inputs(e):
        S.dma("sp", xtok[:], D["xdisp_d"][e * CAP:(e + 1) * CAP, :].rearrange("(j p) d -> p j d", p=128), "c_xtok",
              reads=["xdisp"], writes=["xtok"])

    def transposes(e):
        xe = xeT[e % 2]
        xkey = ("xeT", e % 2)
        tn = 0
        for j in range(NJ):
            for r in range(2):
                pt = psT[tn % 2]
                ptb = pt[:].bitcast(BF16)
                for i in range(8):
                    dc = 8 * r + i
                    S.op("pe", lambda e_: e_.transpose(ptb[:, i * 128:(i + 1) * 128], xtok[:, j, dc * 128:(dc + 1) * 128], k.ident_bf[:]),
                         reads=["xtok", "ident_bf"], writes=[("c_psT", tn % 2)], inc=(i == 7))
                S.op("act", lambda e_: e_.copy(xe[:, 8 * r:8 * r + 8, j * 128:(j + 1) * 128],
                                               ptb[:].rearrange("p (i s) -> p i s", s=128)),
                     reads=[("c_psT", tn % 2)], writes=[xkey])
                tn += 1

    load_expert_inputs(0)
    transposes(0)
    for e in range(nexp):
        S.dma("sp", bdn[:], D["b_dn"][e].partition_broadcast(128), "c_bdn", writes=["bdn"])
        if e + 1 < nexp:
            load_expert_inputs(e + 1)
        xe = xeT[e % 2]
        xkey = ("xeT", e % 2)
        for t in range(4):
            for half in range(2):
                w = wg[nwg[0] % 3]; wkey = ("wg", nwg[0] % 3); nwg[0] += 1
                S.dma("pool", w[:], D["w_gu"][e, half * 4 + t].rearrange("p (c n) -> p c n", n=512), "c_wg%d" % ((nwg[0] - 1) % 3),
                      writes=[wkey])
                for fbi in range(4):
                    fb = t * 4 + fbi
                    pg = psG[npg[0] % 3]; pgkey = ("psG", npg[0] % 3); npg[0] += 1
                    for dc in range(16):
                        S.op("pe", lambda e_: e_.matmul(pg[:, 0:CAP], w[:, dc, fbi * 128:(fbi + 1) * 128], xe[:, dc, :],
                                                        start=(dc == 0), stop=(dc == 15)),
                             reads=[wkey, xkey], writes=[pgkey], inc=(dc == 15))
                    bcol = bgu[:, e, half * 16 + fb:half * 16 + fb + 1]
                    if half == 0:
                        S.op("dve", lambda e_: e_.tensor_scalar(gsb[:], pg[:, 0:CAP], bcol, 7.0, ALU.add, ALU.min),
                             reads=[pgkey, "bgu"], writes=["gsb"])
                        S.op("act", lambda e_: e_.activation(sg[:], gsb[:], AF.Sigmoid, scale=1.702), reads=["gsb"], writes=["sg"])
                        S.op("dve", lambda e_: e_.tensor_tensor(gg[:, fbi, :], gsb[:], sg[:], ALU.mult), reads=["gsb", "sg"], writes=[("gg", fbi)])
                    else:
                        S.op("dve", lambda e_: e_.tensor_scalar(usb[:], pg[:, 0:CAP], bcol, 7.0, ALU.add, ALU.min),
                             reads=[pgkey, "bgu"], writes=["usb"])
                        S.op("dve", lambda e_: e_.tensor_scalar(usb[:], usb[:], -7.0, 1.0, ALU.max, ALU.add), reads=["usb"], writes=["usb"])
                        S.op("dve", lambda e_: e_.tensor_tensor(actT[:, fb, :], usb[:], gg[:, fbi, :], ALU.mult),
                             reads=["usb", ("gg", fbi)], writes=["actT"])
        if e + 1 < nexp:
            transposes(e + 1)
        for db in range(4):
            w = wd[nwd[0] % 2]; wkey = ("wd", nwd[0] % 2); nwd[0] += 1
            S.dma("pool", w[:], D["w_dn"][e, db].rearrange("p (c n) -> p c n", n=512), "c_wd%d" % ((nwd[0] - 1) % 2),
                  writes=[wkey])
            for j in range(NJ):
                pd = psD[npd[0] % 3]; pdkey = ("psD", npd[0] % 3); npd[0] += 1
                for fc in range(16):
                    S.op("pe", lambda e_: e_.matmul(pd[:], actT[:, fc, j * 128:(j + 1) * 128], w[:, fc, :], start=(fc == 0), stop=(fc == 15)),
                         reads=["actT", wkey], writes=[pdkey], inc=(fc == 15))
                yi = nys[0] % 4; nys[0] += 1
                S.op("dve", lambda e_: e_.tensor_tensor(ysb[yi][:], pd[:], bdn[:, db * 512:(db + 1) * 512], ALU.add),
                     reads=[pdkey, "bdn"], writes=[("ysb", yi)])
                S.dma("sp", D["y_d"][e * CAP + j * 128:e * CAP + (j + 1) * 128, db * 512:(db + 1) * 512], ysb[yi][:], "c_yst%d" % yi,
                      reads=[("ysb", yi)])


def phase_d(k):
    nc, S, D = k.nc, k.S, k.D
    sb = lambda n, s, d=F32: k.sb(n, s, d, k.pst)
    nchunks = k.cfg.get("b_chunks", 16)
    gbc = sb("d_gbc", [128, DM]); bbc = sb("d_bbc", [128, DM])
    S.dma("sp", gbc[:], D["ln2"][0].partition_broadcast(128), "d_gbc", writes=["gb2"])
    S.dma("sp", bbc[:], D["ln2"][1].partition_broadcast(128), "d_bbc", writes=["gb2"])
    if not hasattr(k, "eps_ln"):
        k.eps_ln = sb("eps_ln", [128, 1])
    else:
        k.eps_ln = sb("eps_ln2", [128, 1])
    S.op("dve", lambda e: e.memset(k.eps_ln[:], 1e-5), writes=["eps_ln"])
    lntmp = (sb("ln2_st6", [128, 4, 6]), sb("ln2_mv", [128, 2]), sb("ln2_rstd", [128, 1]), sb("ln2_nmr", [128, 1]))
    x1 = [sb("d_x1_%d" % i, [128, DM]) for i in range(2)]
    yk = [[sb("d_y%d_%d" % (i, kk), [128, DM]) for kk in range(4)] for i in range(2)]
    acc = sb("d_acc", [128, DM]); ob = [sb("d_ob%d" % i, [128, DM]) for i in range(2)]
    def loads(c):
        tc_ = slice(c * 128, (c + 1) * 128)
        b = c % 2
        S.dma("sp", x1[b][:], D["x1f_d"][tc_, :], "d_x1_%d" % b, writes=[("dx1", b)])
        for kk in range(4):
            S.dma("pool", yk[b][kk][:], D["y_d"][:, :], "d_y%d_%d" % (b, kk), reads=["dest_all"], writes=[("dy", b, kk)],
                  indirect=(None, bass.IndirectOffsetOnAxis(ap=k.dest_all[:, c, kk:kk + 1], axis=0)))

    loads(0)
    for c in range(nchunks):
        tc_ = slice(c * 128, (c + 1) * 128)
        b = c % 2
        if c + 1 < nchunks:
            loads(c + 1)
        S.op("dve", lambda e: e.tensor_scalar_mul(acc[:], x1[b][:], ALPHA), reads=[("dx1", b)], writes=["acc"])
        for kk in range(4):
            S.op("dve", lambda e: e.scalar_tensor_tensor(acc[:], yk[b][kk][:], k.gates_all[:, c, kk:kk + 1], acc[:], ALU.mult, ALU.add),
                 reads=[("dy", b, kk), "gates_all", "acc"], writes=["acc"])
        layer_norm_rows(k, acc, "acc", gbc, bbc, "gb2", ob[b], ("ob", b), lntmp)
        S.dma("sp", D["out"][tc_, :], ob[b][:], "d_ob%d" % b, reads=[("ob", b)])


def prep_shared(inp):
    f = lambda a: np.ascontiguousarray(a, dtype=np.float32)
    w_in = inp["w_in"][0]

    def fm_tile(c0, n=128):
        t = np.zeros((2048, 128), np.float32)
        t[:, :n] = w_in[:, c0:c0 + n]
        return t.reshape(16, 128, 128).transpose(1, 0, 2).reshape(128, 2048)
    cols = [(128 * u, 128) for u in range(24)] + [(3072, 128), (3200, 128), (3328, 32)]
    cols += [(3360 + 128 * h, 128) for h in range(8)] + [(4384 + 128 * h, 128) for h in range(8)]
    w_fm = np.stack([fm_tile(c, n) for c, n in cols])
    w_v = np.stack([w_in[:, 5408 + 512 * g:5408 + 512 * (g + 1)].reshape(16, 128, 512).transpose(1, 0, 2).reshape(128, 8192)
                    for g in range(2)])
    mu = np.zeros(27 * 128, np.float32)
    mu[:3360] = inp["shift_mu"][0]
    chv = np.concatenate([inp[n][0].reshape(8, 128).T for n in ("w0", "a0", "k_k", "k_a", "gn_g", "gn_b", "r_k")], axis=1)
    sh = {
        "w_fm": f(w_fm), "w_v": f(w_v), "mu": f(mu.reshape(27, 128).T), "chv": f(chv),
        "w_up": f(inp["w_up"][0]), "a_up": f(inp["a_up"][0]), "g_up": f(inp["g_up"][0]),
        "lqk": f(np.concatenate([inp["lq1"][0], inp["lk1"][0], inp["lq2"][0], inp["lk2"][0]])[None, :]),
        "subln": f(inp["subln_g"][0][:, None]),
        "w_out": f(inp["w_out"][0]), "ln1": f(np.stack([inp["ln1_g"][0], inp["ln1_b"][0]])),
        "ln2": f(np.stack([inp["ln2_g"][0], inp["ln2_b"][0]])),
        "w_router": f(inp["w_router"][0]), "b_router": f(inp["b_router"][0][None, :]),
        "w_gu": f(np.asarray(inp["w_gu"][0]).reshape(-1, 16, 128, 8, 512).transpose(0, 3, 2, 1, 4).reshape(-1, 8, 128, 8192)),
        "b_gu_r": f(inp["b_gu"][0].reshape(32, 32, 128).transpose(2, 0, 1).reshape(128, 1024)),
        "w_dn": f(np.asarray(inp["w_dn"][0]).reshape(-1, 16, 128, 4, 512).transpose(0, 3, 2, 1, 4).reshape(-1, 4, 128, 8192)),
        "b_dn": f(inp["b_dn"][0]),
        "zeros": np.zeros((CAP, DM), dtype=ml_dtypes.bfloat16),
    }
    return sh


def prep_core(inp, b):
    xb = np.asarray(inp["x"][b], dtype=np.float32)
    return {"xT": np.ascontiguousarray(xb.T), "x": np.ascontiguousarray(xb)}


def kernel(**inputs):
    nc, _ = build()
    sh = prep_shared(inputs)
    in_maps = [dict(sh, **prep_core(inputs, b)) for b in range(8)]
    res = run_bass_kernel_spmd(nc, in_maps, core_ids=list(range(8)))
    return np.stack([np.asarray(r["out"], dtype=np.float32) for r in res.results])
```

```python
import math
from contextlib import ExitStack
import numpy as np
import ml_dtypes
import concourse.bass as bass
import concourse.mybir as mybir
from concourse.bass_utils import run_bass_kernel_spmd

F32 = mybir.dt.float32
BF16 = mybir.dt.bfloat16
I32 = mybir.dt.int32
U32 = mybir.dt.uint32
AF = mybir.ActivationFunctionType
ALU = mybir.AluOpType
AX = mybir.AxisListType

SEQ = 2048
DM = 2048
NE = 32
CAP = 384
ALPHA = 2.0 ** 0.25
LAM_INIT = 0.8 - 0.6
SCALE = 64 ** -0.5
EXPM05 = math.exp(-0.5)
N_FM = 43


class Sched:
    def __init__(self, nc, stack):
        self.nc = nc
        self.stack = stack
        self.engs = {"pe": nc.tensor, "act": nc.scalar, "dve": nc.vector,
                     "pool": nc.gpsimd, "sp": nc.sync}
        self.sems = {}
        self.cnt = {}
        self.waited = {e: {} for e in self.engs}
        self.last_write = {}
        self.readers = {}
        for e in ("pe", "act", "dve", "pool"):
            self._sem("E_" + e)
        self.n_ins = 0
        self.last_rg = None
        self.last_pe_inc = True

    def _sem(self, name):
        if name not in self.sems:
            self.sems[name] = self.stack.enter_context(self.nc.semaphore(name))
            self.cnt[name] = 0
        return self.sems[name]

    def _emit_waits(self, eng, reads, writes):
        need = {}

        def add(tok, same_ok):
            if tok is None:
                return
            s, v = tok
            if same_ok and s == "E_" + eng and eng == "pe":
                return
            if need.get(s, 0) < v:
                need[s] = v
        for k in reads:
            add(self.last_write.get(k), False)
        for k in writes:
            add(self.last_write.get(k), True)
            for t in self.readers.get(k, ()):
                add(t, True)
        e = self.engs[eng]
        for s, v in need.items():
            if self.waited[eng].get(s, 0) >= v:
                continue
            e.wait_ge(self.sems[s], v)
            self.waited[eng][s] = v
            self.n_ins += 1

    def _record(self, tok, reads, writes):
        for k in reads:
            self.readers.setdefault(k, []).append(tok)
        for k in writes:
            self.last_write[k] = tok
            self.readers[k] = []

    def op(self, eng, fn, reads=(), writes=(), inc=True, rg=None):
        if eng == "pe":
            if rg is not None and self.last_rg is not None and rg != self.last_rg:
                assert self.last_pe_inc
                self.engs["pe"].wait_ge(self.sems["E_pe"], self.cnt["E_pe"])
                self.n_ins += 1
            self.last_rg = rg
            self.last_pe_inc = inc
        self._emit_waits(eng, reads, writes)
        ins = fn(self.engs[eng])
        self.n_ins += 1
        s = "E_" + eng
        if inc:
            self.cnt[s] += 1
            ins.then_inc(self.sems[s], 1)
            tok = (s, self.cnt[s])
        else:
            tok = (s, self.cnt[s] + 1)
        self._record(tok, reads, writes)
        return ins

    def dma(self, q, out, in_, semkey, reads=(), writes=(), indirect=None, **kw):
        self._emit_waits(q, reads, writes)
        s = "D_" + semkey
        self._sem(s)
        if indirect is None:
            ins = self.engs[q].dma_start(out=out, in_=in_, **kw)
        else:
            ins = self.engs[q].indirect_dma_start(out, indirect[0], in_, indirect[1], **kw)
        self.n_ins += 1
        self.cnt[s] += 16
        ins.then_inc(self.sems[s], 16)
        tok = (s, self.cnt[s])
        self._record(tok, reads, writes)
        return ins

    def barrier(self):
        for eng in self.engs:
            e = self.engs[eng]
            for s, v in self.cnt.items():
                if v == 0 or s == "E_" + eng:
                    continue
                if self.waited[eng].get(s, 0) >= v:
                    continue
                e.wait_ge(self.sems[s], v)
                self.waited[eng][s] = v
                self.n_ins += 1
        self.last_write = {}
        self.readers = {}


class K:
    pass


def build(cfg=None):
    cfg = cfg or {}
    phases = cfg.get("phases", "ABCD")
    dbg = cfg.get("dbg", ())
    nc = bass.Bass("TRN2", target_bir_lowering=False)
    k = K()
    k.nc, k.cfg, k.dbg = nc, cfg, dbg
    D = {}
    k.D = D

    def din(name, shape, dt=F32):
        D[name] = nc.dram_tensor(name, list(shape), dt, kind="ExternalInput").ap()

    def dout(name, shape, dt=F32):
        D[name] = nc.dram_tensor(name, list(shape), dt, kind="ExternalOutput").ap()

    def dscr(name, shape, dt):
        if name in cfg.get("ext_in", ()):
            D[name] = nc.dram_tensor(name, list(shape), dt, kind="ExternalInput").ap()
        elif name in cfg.get("ext", ()):
            D[name] = nc.dram_tensor(name, list(shape), dt, kind="ExternalOutput").ap()
        else:
            D[name] = nc.dram_tensor(name, list(shape), dt).ap()
    k.dout = dout

    din("xT", [DM, SEQ]); din("x", [SEQ, DM])
    din("w_fm", [N_FM, 128, 2048]); din("w_v", [2, 128, 16 * 512])
    din("mu", [128, 27]); din("chv", [128, 56])
    din("w_up", [64, 1024]); din("a_up", [64, 1024]); din("g_up", [160, 1024])
    din("lqk", [1, 256]); din("subln", [128, 1])
    din("w_out", [DM, DM]); din("ln1", [2, DM]); din("ln2", [2, DM])
    din("w_router", [DM, NE]); din("b_router", [1, NE])
    din("w_gu", [cfg.get("ne_decl", NE), DM, 2 * DM]); din("b_gu_r", [128, NE * 32])
    din("w_dn", [cfg.get("ne_decl", NE), DM, DM]); din("b_dn", [NE, DM])
    din("zeros", [CAP, DM], BF16)
    dout("out", [SEQ, DM])
    dscr("hT_d", [16, 128, SEQ], BF16)
    dscr("ra_d", [8, 128, 4096], BF16); dscr("bt_d", [8, 128, SEQ], BF16); dscr("kt_d", [8, 128, SEQ], BF16)
    dscr("vb_d", [8, 128, SEQ], BF16); dscr("bo_d", [8, 128, SEQ], F32); dscr("bg_d", [8, 128, SEQ], F32)
    dscr("gl_d", [8, 128, 16], F32)
    dscr("x1f_d", [SEQ, DM], F32)
    dscr("xdisp_d", [NE * CAP, DM], BF16)
    dscr("y_d", [NE * CAP, DM], F32)

    with ExitStack() as st:
        k.st = st
        k.S = Sched(nc, st)

        def sb(name, shape, dt=F32, stack=None):
            return (stack or st).enter_context(nc.sbuf_tensor(name, list(shape), dt))

        def ps(name, shape, dt=F32, stack=None):
            return (stack or st).enter_context(nc.psum_tensor(name, list(shape), dt))
        k.sb, k.ps = sb, ps
        setup_consts(k)
        k.dest_all = sb("dest_all", [128, 16, 4], I32)
        k.gates_all = sb("gates_all", [128, 16, 4], F32)
        k.S.op("dve", lambda e: e.memset(k.dest_all[:], 0), writes=["dest_all"])
        k.S.op("dve", lambda e: e.memset(k.gates_all[:], 0.0), writes=["gates_all"])
        if "A" in phases:
            with ExitStack() as pst:
                k.pst = pst
                if cfg.get("pro", True):
                    phase_a_setup(k)
                if cfg.get("rwkv", True) and cfg.get("pro", True):
                    with ExitStack() as st2:
                        phase_rwkv_pro(k, st2)
                    k.S.barrier()
                if cfg.get("diff", True):
                    with ExitStack() as st2:
                        phase_diff(k, st2)
            k.S.barrier()
            if cfg.get("rwkv", True) and cfg.get("scan", True):
                with ExitStack() as st2:
                    phase_rwkv_scan(k, st2)
                k.S.barrier()
        if "B" in phases:
            with ExitStack() as pst:
                k.pst = pst
                phase_b(k)
            k.S.barrier()
        if "route" in dbg:
            dout("dbg_dest", [128, 64], I32); dout("dbg_gates", [128, 64], F32)
            k.S.dma("sp", D["dbg_dest"], k.dest_all[:].rearrange("p c k -> p (c k)"), "dbg_dest", reads=["dest_all"])
            k.S.dma("sp", D["dbg_gates"], k.gates_all[:].rearrange("p c k -> p (c k)"), "dbg_gates", reads=["gates_all"])
            k.S.barrier()
        if "C" in phases:
            with ExitStack() as pst:
                k.pst = pst
                phase_c(k)
            k.S.barrier()
        if "D" in phases:
            with ExitStack() as pst:
                k.pst = pst
                phase_d(k)
        k.S.barrier()
    k.n_ins = k.S.n_ins
    return nc, k


def setup_consts(k):
    nc, S, sb = k.nc, k.S, k.sb
    k.iota_jp = sb("iota_jp", [128, 512], F32)
    S.op("pool", lambda e: e.iota(k.iota_jp[:], [[1, 512]], base=0, channel_multiplier=-1,
                                  allow_small_or_imprecise_dtypes=True), writes=["iota_jp"])
    k.ident_bf = sb("ident_bf", [128, 128], BF16)
    k.ident_f = sb("ident_f", [128, 128], F32)
    k.m_incl = sb("m_incl", [128, 128], F32)
    k.m_strict = sb("m_strict", [128, 128], F32)
    k.m_incl_bf = sb("m_incl_bf", [128, 128], BF16)
    k.m_strict_bf = sb("m_strict_bf", [128, 128], BF16)
    k.ones_bf = sb("ones_bf", [128, 128], BF16)
    k.ones_f = sb("ones_f", [128, 128], F32)
    k.blk_bf = sb("blk_bf", [128, 128], BF16)
    k.blk_f = sb("blk_f", [128, 128], F32)
    k.iota_e = sb("iota_e", [128, NE], F32)
    ij = k.iota_jp[:, 0:128]
    for t, op, key in ((k.ident_bf, ALU.is_equal, "ident_bf"), (k.ident_f, ALU.is_equal, "ident_f"),
                       (k.m_incl, ALU.is_ge, "m_incl"), (k.m_strict, ALU.is_gt, "m_strict"),
                       (k.m_incl_bf, ALU.is_ge, "m_incl_bf"), (k.m_strict_bf, ALU.is_gt, "m_strict_bf")):
        S.op("dve", lambda e, t=t, op=op: e.tensor_single_scalar(t[:], ij, 0.0, op),
             reads=["iota_jp"], writes=[key])
    S.op("dve", lambda e: e.memset(k.ones_bf[:], 1.0), writes=["ones_bf"])
    S.op("dve", lambda e: e.memset(k.ones_f[:], 1.0), writes=["ones_f"])
    for t, key in ((k.blk_bf, "blk_bf"), (k.blk_f, "blk_f")):
        S.op("dve", lambda e, t=t: e.memset(t[:], 0.0), writes=[key])
        S.op("dve", lambda e, t=t: e.memset(t[0:64, 0:64], 1.0), writes=[key])
        S.op("dve", lambda e, t=t: e.memset(t[64:128, 64:128], 1.0), writes=[key])
    S.op("pool", lambda e: e.iota(k.iota_e[:], [[1, NE]], base=0, channel_multiplier=0,
                                  allow_small_or_imprecise_dtypes=True), writes=["iota_e"])
    k.iota4 = sb("iota4", [128, 4, 128], F32)
    S.op("pool", lambda e: e.iota(k.iota4[:], [[0, 4], [1, 128]], base=0, channel_multiplier=-1,
                                  allow_small_or_imprecise_dtypes=True), writes=["iota4"])
    k.ms4 = sb("ms4", [128, 4, 128], BF16)
    k.ml4 = sb("ml4", [128, 4, 128], BF16)
    k.mi4 = sb("mi4", [128, 4, 128], BF16)
    k.I4 = sb("I4", [128, 4, 128], F32)
    for t, op, key in ((k.ms4, ALU.is_gt, "ms4"), (k.ml4, ALU.is_lt, "ml4"), (k.mi4, ALU.is_ge, "mi4"), (k.I4, ALU.is_equal, "I4")):
        S.op("dve", lambda e, t=t, op=op: e.tensor_single_scalar(t[:], k.iota4[:], 0.0, op),
             reads=["iota4"], writes=[key])
    k.eps_gn = sb("eps_gn", [128, 1], F32)
    S.op("dve", lambda e: e.memset(k.eps_gn[:], 64e-5), writes=["eps_gn"])
    k.eps_rms = sb("eps_rms", [128, 1], F32)
    S.op("dve", lambda e: e.memset(k.eps_rms[:], 1e-5), writes=["eps"])


def phase_a_setup(k):
    nc, S, D = k.nc, k.S, k.D
    sb = lambda n, s, d=F32: k.sb(n, s, d, k.pst)
    k.xT = sb("xT_sb", [128, 16, SEQ], BF16)
    xv = D["xT"].rearrange("(c p) t -> p c t", p=128)
    for i in range(4):
        S.dma("pool", k.xT[:, 4 * i:4 * i + 4, :], xv[:, 4 * i:4 * i + 4, :], "xT%d" % i, writes=[("xT", i)])
    k.xT_keys = [("xT", i) for i in range(4)]
    k.wfm = [sb("wfm%d" % i, [128, 16, 128], BF16) for i in range(2)]
    k.wfm_n = 0
    k.mu = sb("mu_sb", [128, 27]); k.omm = sb("omm_sb", [128, 27])
    S.dma("sp", k.mu[:], D["mu"], "mu", writes=["mu"])
    S.op("dve", lambda e: e.tensor_scalar(k.omm[:], k.mu[:], -1.0, 1.0, ALU.mult, ALU.add),
         reads=["mu"], writes=["omm"])


def load_wfm(k, tile):
    S, D = k.S, k.D
    i = k.wfm_n % 2
    k.wfm_n += 1
    S.dma("pool", k.wfm[i][:], D["w_fm"][tile].rearrange("p (c n) -> p c n", n=128), "wfm%d" % i,
          writes=[("wfm", i)])
    return k.wfm[i], ("wfm", i)


def inproj_fm(k, tile, ps_pair, evac, ncols=128):
    S = k.S
    wt, wkey = load_wfm(k, tile)
    for g in range(4):
        pst, pkey = ps_pair[g % 2]
        for dc in range(16):
            S.op("pe", lambda e: e.matmul(pst[0:ncols, :], wt[:, dc, 0:ncols], k.xT[:, dc, g * 512:(g + 1) * 512],
                                          start=(dc == 0), stop=(dc == 15)),
                 reads=[wkey, ("xT", dc // 4)], writes=[pkey], inc=(dc == 15))
        evac(g, pst[0:ncols, :], pkey)


def phase_diff(k, st2):
    nc, S, D = k.nc, k.S, k.D
    sb = lambda n, s, d=F32: k.sb(n, s, d, st2)
    ps = lambda n, s, d=F32: k.ps(n, s, d, st2)
    ps_s = [(ps("dps_s%d" % i, [128, 512]), ("dps_s", i)) for i in range(3)]
    ps_ms = (ps("dps_ms", [128, 512]), ("dps_ms", 0))
    ps_in = ps_s[0:2]
    ps_o = [(ps("dps_o%d" % i, [128, 512]), ("dps_o", i)) for i in range(2)]
    ps_l = [(ps("dps_l%d" % i, [128, 512]), ("dps_l", i)) for i in range(2)]
    qk = [sb("qk%d" % i, [128, 2, SEQ], BF16) for i in range(2)]
    v4 = sb("v4", [128, 16, 512], BF16)
    wv = sb("wv", [128, 16, 512], BF16)
    pT = [sb("pT%d" % i, [128, 512], BF16) for i in range(3)]
    rl = sb("rl", [128, 512]); o0 = sb("o0", [128, 512]); o1 = sb("o1", [128, 512]); oo = sb("oo", [128, 512])
    sq = sb("sq", [128, 512], BF16); rstd = sb("rstd", [128, 512])
    hst = [sb("hst%d" % i, [128, 512], BF16) for i in range(2)]
    lqk = sb("lqk_sb", [128, 256]); prod = sb("lprod", [128, 128]); s12 = sb("ls12", [128, 2]); e12 = sb("le12", [128, 2])
    nlam = sb("nlam", [128, 1]); sgs = sb("sgs", [128, 1]); sgin = sb("sgin", [128, 1])
    S.dma("sp", lqk[:], D["lqk"][0].partition_broadcast(128), "lqk", writes=["lqk"])
    S.dma("sp", sgin[:], D["subln"], "sgin", writes=["sgin"])
    S.op("dve", lambda e: e.tensor_tensor(prod[:, 0:64], lqk[:, 0:64], lqk[:, 64:128], ALU.mult), reads=["lqk"], writes=["lprod"])
    S.op("dve", lambda e: e.tensor_tensor(prod[:, 64:128], lqk[:, 128:192], lqk[:, 192:256], ALU.mult), reads=["lqk"], writes=["lprod"])
    S.op("dve", lambda e: e.reduce_sum(s12[:], prod[:].rearrange("p (a n) -> p a n", a=2), AX.X), reads=["lprod"], writes=["ls12"])
    S.op("act", lambda e: e.activation(e12[:], s12[:], AF.Exp), reads=["ls12"], writes=["le12"])
    S.op("dve", lambda e: e.tensor_tensor(nlam[:], e12[:, 1:2], e12[:, 0:1], ALU.subtract), reads=["le12"], writes=["nlam"])
    S.op("dve", lambda e: e.tensor_scalar_add(nlam[:], nlam[:], -LAM_INIT), reads=["nlam"], writes=["nlam"])
    S.op("dve", lambda e: e.tensor_scalar_mul(sgs[:], sgin[:], 1.0 - LAM_INIT), reads=["sgin"], writes=["sgs"])

    if k.cfg.get("zfill", True):
        for e_ in range(NE):
            S.dma("sp", D["xdisp_d"][e_ * CAP:(e_ + 1) * CAP, :], D["zeros"], "zfill", writes=["xdisp"])
    for h in range(k.cfg.get("diff_heads", 8)):
        hh = h % 4
        if hh == 0:
            S.dma("pool", wv[:], D["w_v"][h // 4].rearrange("p (c n) -> p c n", n=512), "wv", writes=["wv"])
            for tc in range(16):
                pst, pkey = ps_in[tc % 2]
                for dc in range(16):
                    S.op("pe", lambda e: e.matmul(pst[:], k.xT[:, dc, tc * 128:(tc + 1) * 128], wv[:, dc, :],
                                                  start=(dc == 0), stop=(dc == 15)),
                         reads=["wv", ("xT", dc // 4)], writes=[pkey], inc=(dc == 15))
                S.op("act", lambda e: e.copy(v4[:, tc, :], pst[:]), reads=[pkey], writes=[("v4", tc)])
        qkb = qk[h % 2]
        qkey = ("qk", h % 2)
        for which, tile in ((0, 27 + h), (1, 35 + h)):
            def evac(g, pap, pkey, which=which):
                S.op("act", lambda e: e.copy(qkb[:, which, g * 512:(g + 1) * 512], pap), reads=[pkey], writes=[qkey])
            inproj_fm(k, tile, ps_in, evac)
        tiles = [(m, g, j) for g in range(4) for m in range(2) for j in range(4 * g + 4)]

        def emit_qk(n):
            m, g, j = tiles[n]
            i = j - 4 * g
            q0 = 128 * i if i > 0 else 0
            pst, pkey = ps_s[n % 3]
            S.op("pe", lambda e: e.matmul(pst[:, q0:512], qkb[64 * m:64 * m + 64, 1, j * 128:(j + 1) * 128],
                                          qkb[64 * m:64 * m + 64, 0, g * 512 + q0:(g + 1) * 512], start=True, stop=True),
                 reads=[qkey], writes=[pkey], rg=m)
        emit_qk(0)
        emit_qk(1)
        for n, (m, g, j) in enumerate(tiles):
            if n + 2 < len(tiles):
                emit_qk(n + 2)
            i = j - 4 * g
            q0 = 128 * i if i > 0 else 0
            pst, pkey = ps_s[n % 3]
            pt = pT[n % 3]
            ptk = ("pT", n % 3)
            acc = (2 * g + m) % 2
            S.op("act", lambda e: e.activation(pt[:, q0:512], pst[:, q0:512], AF.Exp, scale=SCALE), reads=[pkey], writes=[ptk])
            if i >= 0:
                S.op("pool", lambda e: e.tensor_tensor(pt[:, q0:q0 + 128], pt[:, q0:q0 + 128], k.m_incl_bf[:], ALU.mult),
                     reads=[ptk, "m_incl_bf"], writes=[ptk])
            last = (j == 4 * g + 3)
            S.op("pe", lambda e: e.matmul(ps_o[acc][0][:, q0:512], v4[:, j, hh * 128:(hh + 1) * 128], pt[:, q0:512],
                                          start=(j == 0), stop=last),
                 reads=[ptk, ("v4", j)], writes=[ps_o[acc][1]], inc=False)
            S.op("pe", lambda e: e.matmul(ps_l[acc][0][:, q0:512], k.ones_bf[:], pt[:, q0:512], start=(j == 0), stop=last),
                 reads=[ptk, "ones_bf"], writes=[ps_l[acc][1]], inc=True)
            if last:
                S.op("dve", lambda e: e.reciprocal(rl[:], ps_l[acc][0][:]), reads=[ps_l[acc][1]], writes=["rl"])
                if m == 0:
                    S.op("dve", lambda e: e.tensor_tensor(o0[:], ps_o[acc][0][:], rl[:], ALU.mult),
                         reads=[ps_o[acc][1], "rl"], writes=["o0"])
                else:
                    S.op("dve", lambda e: e.tensor_tensor(o1[:], ps_o[acc][0][:], rl[:], ALU.mult),
                         reads=[ps_o[acc][1], "rl"], writes=["o1"])
                    S.op("dve", lambda e: e.scalar_tensor_tensor(oo[:], o1[:], nlam[:], o0[:], ALU.mult, ALU.add),
                         reads=["o0", "o1", "nlam"], writes=["oo"])
                    S.op("pool", lambda e: e.tensor_tensor(sq[:], oo[:], oo[:], ALU.mult), reads=["oo"], writes=["sq"])
                    mp, mkey = ps_ms
                    S.op("pe", lambda e: e.matmul(mp[:], k.ones_bf[:], sq[:], start=True, stop=True),
                         reads=["sq", "ones_bf"], writes=[mkey])
                    S.op("act", lambda e: e.activation(rstd[:], mp[:], AF.Sqrt, bias=k.eps_rms[:], scale=1.0 / 128),
                         reads=[mkey, "eps"], writes=["rstd"])
                    S.op("dve", lambda e: e.reciprocal(rstd[:], rstd[:]), reads=["rstd"], writes=["rstd"])
                    hs = hst[g % 2]
                    S.op("dve", lambda e: e.scalar_tensor_tensor(hs[:], oo[:], sgs[:], rstd[:], ALU.mult, ALU.mult),
                         reads=["oo", "sgs", "rstd"], writes=[("hst", g % 2)])
                    S.dma("sp", D["hT_d"][8 + h, :, g * 512:(g + 1) * 512], hs[:], "hst%d" % (g % 2), reads=[("hst", g % 2)])


def phase_rwkv_pro(k, st2):
    nc, S, D = k.nc, k.S, k.D
    sb = lambda n, s, d=F32: k.sb(n, s, d, st2)
    ps = lambda n, s, d=F32: k.ps(n, s, d, st2)
    npairs = k.cfg.get("rwkv_pairs", 8)
    ps_in = [(ps("rps_in%d" % i, [128, 512]), ("rps_in", i)) for i in range(2)]
    NB = {}
    for n in ("Br", "Bk", "B1", "B4", "B5", "B6", "B8"):
        NB[n] = sb("rw_" + n, [128, SEQ])
    raw = sb("rw_raw", [128, SEQ + 4])
    NB["B2"] = raw[:, 1:SEQ + 1]
    NB["Bo"] = NB["B6"]; NB["Bg"] = NB["B5"]
    E2 = sb("rw_E2", [128, SEQ])
    RA = sb("rw_RA", [128, 16, 256], BF16)
    BT = sb("rw_BT", [128, SEQ], BF16); KT = sb("rw_KT", [128, SEQ], BF16); VB = sb("rw_VB", [128, SEQ], BF16)
    tb = sb("rw_tb", [128, SEQ], BF16); sqb = tb
    LW = sb("rw_LW", [128, SEQ], BF16); SG1 = sb("rw_SG1", [128, SEQ], BF16); SG2 = sb("rw_SG2", [32, SEQ], BF16)
    wup = sb("rw_wup", [64, 1024], BF16); aup = sb("rw_aup", [128, 1024], BF16)
    gup1 = sb("rw_gup1", [128, 1024], BF16); gup2 = sb("rw_gup2", [32, 1024], BF16)
    chv = sb("rw_chv", [128, 56]); omka = sb("rw_omka", [128, 8]); glt = sb("rw_glt", [128, 16])
    S.dma("sp", chv[:], D["chv"], "chv", writes=["chv"])
    S.dma("pool", wup[:], D["w_up"], "wup", writes=["wup"])
    S.dma("pool", aup[64:128, :], D["a_up"], "aup", writes=["aup"])
    S.dma("pool", gup1[:], D["g_up"][0:128, :], "gup1", writes=["gup1"])
    S.dma("pool", gup2[:], D["g_up"][128:160, :], "gup2", writes=["gup2"])
    S.op("dve", lambda e: e.tensor_scalar(omka[:], chv[:, 24:32], -1.0, 1.0, ALU.mult, ALU.add), reads=["chv"], writes=["omka"])
    S.op("dve", lambda e: e.memset(raw[:, 0:1], 0.0), writes=["B2"])
    CW0, CA0, CKK, CKA, CGG, CGB, CRK = [8 * i for i in range(7)]

    def proj(tile, dst, dkey, ncols=128):
        def evac(g, pap, pkey):
            S.op("act", lambda e: e.copy(raw[0:ncols, 1 + g * 512:1 + (g + 1) * 512], pap), reads=[pkey], writes=["B2"])
        inproj_fm(k, tile, ps_in, evac, ncols)
        S.op("dve", lambda e: e.tensor_scalar_mul(dst[0:ncols, :], raw[0:ncols, 0:SEQ], k.mu[0:ncols, tile:tile + 1]),
             reads=["B2", "mu"], writes=[dkey])
        S.op("dve", lambda e: e.scalar_tensor_tensor(dst[0:ncols, :], raw[0:ncols, 1:SEQ + 1], k.omm[0:ncols, tile:tile + 1],
                                                     dst[0:ncols, :], ALU.mult, ALU.add),
             reads=["B2", "omm", dkey], writes=[dkey])

    B1 = NB["B1"]
    proj(24, B1, "B1")
    S.op("act", lambda e: e.activation(LW[0:64, :], B1[0:64, :], AF.Tanh), reads=["B1"], writes=["LW"])
    S.op("act", lambda e: e.copy(LW[64:128, :], B1[64:128, :]), reads=["B1"], writes=["LW"])
    proj(25, B1, "B1")
    S.op("act", lambda e: e.activation(SG1[:], B1[:], AF.Sigmoid), reads=["B1"], writes=["SG1"])
    proj(26, B1, "B1", ncols=32)
    S.op("act", lambda e: e.activation(SG2[:], B1[0:32, :], AF.Sigmoid), reads=["B1"], writes=["SG2"])

    def gs(g):
        return slice(g * 512, (g + 1) * 512)

    for u in range(npairs):
        cs = slice(u * 128, (u + 1) * 128)
        Br, Bk, B2, B4, B5, B6, B8, Bo, Bg = (NB[n] for n in ("Br", "Bk", "B2", "B4", "B5", "B6", "B8", "Bo", "Bg"))
        proj(u, Br, "Br"); proj(8 + u, Bk, "Bk"); proj(16 + u, B8, "B8")
        S.op("act", lambda e: e.copy(VB[:], B8[:]), reads=["B8"], writes=["VB"])
        for g in range(4):
            pst, pkey = ps_in[g % 2]
            S.op("pe", lambda e: e.matmul(pst[:], wup[0:64, cs], LW[0:64, gs(g)], start=True, stop=True),
                 reads=["wup", "LW"], writes=[pkey])
            S.op("act", lambda e: e.activation(B1[:, gs(g)], pst[:], AF.Sigmoid, bias=chv[:, CW0 + u:CW0 + u + 1]),
                 reads=[pkey, "chv"], writes=["B1"])
        S.op("dve", lambda e: e.tensor_scalar_mul(B1[:], B1[:], -EXPM05), reads=["B1"], writes=["B1"])
        for n in range(16):
            S.op("dve", lambda e: e.tensor_tensor_scan(B2[:, n * 128:(n + 1) * 128], k.ones_f[:], B1[:, n * 128:(n + 1) * 128],
                                                       0.0, ALU.mult, ALU.add), reads=["B1", "ones_f"], writes=["B2"])
        for g in range(4):
            pst, pkey = ps_in[g % 2]
            S.op("pe", lambda e: e.matmul(pst[:], aup[64:128, cs], LW[64:128, gs(g)], start=True, stop=True),
                 reads=["aup", "LW"], writes=[pkey])
            S.op("act", lambda e: e.activation(B4[:, gs(g)], pst[:], AF.Sigmoid, bias=chv[:, CA0 + u:CA0 + u + 1]),
                 reads=[pkey, "chv"], writes=["B4"])
        S.op("dve", lambda e: e.tensor_scalar_mul(B5[:], Bk[:], chv[:, CKK + u:CKK + u + 1]), reads=["Bk", "chv"], writes=["B5"])
        S.op("pool", lambda e: e.tensor_tensor(sqb[:], B5[:], B5[:], ALU.mult), reads=["B5"], writes=["tb"])
        for g in range(4):
            pst, pkey = ps_in[g % 2]
            S.op("pe", lambda e: e.matmul(pst[:], k.blk_bf[:], sqb[:, gs(g)], start=True, stop=True),
                 reads=["blk_bf", "tb"], writes=[pkey])
            S.op("act", lambda e: e.activation(B6[:, gs(g)], pst[:], AF.Sqrt), reads=[pkey], writes=["B6"])
        S.op("dve", lambda e: e.tensor_scalar_max(B6[:], B6[:], 1e-12), reads=["B6"], writes=["B6"])
        S.op("dve", lambda e: e.reciprocal(B6[:], B6[:]), reads=["B6"], writes=["B6"])
        S.op("dve", lambda e: e.tensor_tensor(B5[:], B5[:], B6[:], ALU.mult), reads=["B5", "B6"], writes=["B5"])
        S.op("dve", lambda e: e.tensor_scalar(B6[:], B4[:], chv[:, CKA + u:CKA + u + 1], omka[:, u:u + 1], ALU.mult, ALU.add),
             reads=["B4", "chv", "omka"], writes=["B6"])
        S.op("pool", lambda e: e.tensor_tensor(Bk[:], Bk[:], B6[:], ALU.mult), reads=["Bk", "B6"], writes=["Bk"])
        S.op("act", lambda e: e.activation(B6[:], B2[:], AF.Exp), reads=["B2"], writes=["B6"])
        S.op("act", lambda e: e.activation(E2[:], B2[:], AF.Exp, scale=-1.0), reads=["B2"], writes=["E2"])
        S.op("dve", lambda e: e.tensor_tensor(B8[:], B2[:], B1[:], ALU.subtract), reads=["B2", "B1"], writes=["B8"])
        S.op("act", lambda e: e.activation(B8[:], B8[:], AF.Exp), reads=["B8"], writes=["B8"])
        S.op("dve", lambda e: e.tensor_copy(glt[:], B6[:].rearrange("p (c l) -> p c l", l=128)[:, :, 127]),
             reads=["B6"], writes=["glt"])
        S.op("pool", lambda e: e.tensor_tensor(RA[:, :, 0:128], Br[:].rearrange("p (c l) -> p c l", l=128),
                                               B6[:].rearrange("p (c l) -> p c l", l=128), ALU.mult),
             reads=["Br", "B6"], writes=["RA"])
        S.op("dve", lambda e: e.scalar_tensor_tensor(RA[:, :, 128:256], B5[:].rearrange("p (c l) -> p c l", l=128), -1.0,
                                                     B8[:].rearrange("p (c l) -> p c l", l=128), ALU.mult, ALU.mult),
             reads=["B5", "B8"], writes=["RA"])
        S.op("dve", lambda e: e.tensor_tensor(B5[:], B5[:], B4[:], ALU.mult), reads=["B5", "B4"], writes=["B5"])
        S.op("pool", lambda e: e.tensor_tensor(BT[:], B5[:], E2[:], ALU.mult), reads=["B5", "E2"], writes=["BT"])
        S.op("dve", lambda e: e.tensor_tensor(KT[:], Bk[:], E2[:], ALU.mult), reads=["Bk", "E2"], writes=["KT"])
        S.op("dve", lambda e: e.scalar_tensor_tensor(tb[:], Br[:], chv[:, CRK + u:CRK + u + 1], Bk[:], ALU.mult, ALU.mult),
             reads=["Br", "Bk", "chv"], writes=["tb"])
        for g in range(4):
            pst, pkey = ps_in[g % 2]
            S.op("pe", lambda e: e.matmul(pst[:], k.blk_bf[:], tb[:, gs(g)], start=True, stop=True),
                 reads=["blk_bf", "tb"], writes=[pkey])
            S.op("dve", lambda e: e.tensor_tensor(Bo[:, gs(g)], pst[:], VB[:, gs(g)], ALU.mult), reads=[pkey, "VB"], writes=["B6"])
        for g in range(4):
            pst, pkey = ps_in[g % 2]
            S.op("pe", lambda e: e.matmul(pst[:], gup1[:, cs], SG1[:, gs(g)], start=True, stop=False),
                 reads=["gup1", "SG1"], writes=[pkey], inc=False)
            S.op("pe", lambda e: e.matmul(pst[:], gup2[:, cs], SG2[:, gs(g)], start=False, stop=True),
                 reads=["gup2", "SG2"], writes=[pkey])
            S.op("act", lambda e: e.copy(Bg[:, gs(g)], pst[:]), reads=[pkey], writes=["B5"])
        S.dma("sp", D["ra_d"][u], RA[:].rearrange("p c n -> p (c n)"), "st_ra", reads=["RA"])
        S.dma("sp", D["bt_d"][u], BT[:], "st_bt", reads=["BT"])
        S.dma("sp", D["kt_d"][u], KT[:], "st_kt", reads=["KT"])
        S.dma("sp", D["vb_d"][u], VB[:], "st_vb", reads=["VB"])
        S.dma("sp", D["bo_d"][u], Bo[:], "st_bo", reads=["B6"])
        S.dma("sp", D["bg_d"][u], Bg[:], "st_bg", reads=["B5"])
        S.dma("sp", D["gl_d"][u], glt[:], "st_gl", reads=["glt"])


def phase_rwkv_scan(k, st2):
    nc, S, D = k.nc, k.S, k.D
    sb = lambda n, s, d=F32: k.sb(n, s, d, st2)
    ps = lambda n, s, d=F32: k.ps(n, s, d, st2)
    npairs = k.cfg.get("rwkv_pairs", 8)
    scm = k.cfg.get("sc_mode", 3)
    P1 = ps("sc_P1", [128, 4, 128]); P2 = ps("sc_P2", [128, 4, 128])
    PA = [ps("sc_PA%d" % i, [128, 4, 128]) for i in range(3)]
    PC = ps("sc_PC", [128, 4, 128])
    PM = [ps("sc_PM%d" % i, [128, 512]) for i in range(2)]
    chv = sb("sc_chv", [128, 56])
    S.dma("sp", chv[:], D["chv"], "chv2", writes=["chv2"])
    CGG, CGB = 32, 40
    RA = [sb("sc_RA%d" % i, [128, 16, 256], BF16) for i in range(2)]
    BT = [sb("sc_BT%d" % i, [128, SEQ], BF16) for i in range(2)]
    KT = [sb("sc_KT%d" % i, [128, SEQ], BF16) for i in range(2)]
    VB = [sb("sc_VB%d" % i, [128, SEQ], BF16) for i in range(2)]
    GL = [sb("sc_GL%d" % i, [128, 16]) for i in range(2)]
    TT = [sb("sc_TT%d" % i, [128, 16, 2, 128], BF16) for i in range(2)]
    ACH = [sb("sc_ACH%d" % i, [128, 16, 2, 3, 128], BF16) for i in range(2)]
    TOK = [sb("sc_TOK%d" % i, [128, 16, 3, 128], BF16) for i in range(2)]
    VPAD = [sb("sc_VPAD%d" % i, [128, 16, 2, 128], BF16) for i in range(2)]
    for i in range(2):
        S.op("pool", lambda e: e.memset(VPAD[i][:], 0.0), writes=[("VPAD", i)])
    Xb = [sb("sc_X%d" % i, [128, 4, 128], BF16) for i in range(2)]
    XTb = [sb("sc_XT%d" % i, [128, 4, 128], BF16) for i in range(2)]
    Pf = sb("sc_Pf", [128, 4, 128]); Pbf = sb("sc_Pbf", [128, 4, 128], BF16)
    Wsb = sb("sc_W", [128, 128], BF16); Ut = sb("sc_Ut", [128, 128], BF16)
    UPAD = [sb("sc_UPAD%d" % i, [128, 2, 128], BF16) for i in range(2)]
    for i in range(2):
        S.op("pool", lambda e: e.memset(UPAD[i][:], 0.0), writes=[("UPAD", i)])
    Sf = sb("sc_Sf", [128, 128]); t1 = sb("sc_t1", [128, 128])
    Sbf = [sb("sc_Sbf%d" % i, [128, 128], BF16) for i in range(2)]
    Yf = sb("sc_Yf", [128, SEQ]); Bo = sb("sc_Bo", [128, SEQ]); Bg = sb("sc_Bg", [128, SEQ])
    yb = sb("sc_yb", [128, SEQ], BF16); yc = sb("sc_yc", [128, SEQ]); rs = Yf
    hb = sb("sc_hb", [128, SEQ], BF16)

    def load(u):
        pp = u % 2
        S.dma("sp", RA[pp][:].rearrange("p c n -> p (c n)"), D["ra_d"][u], "ld_ra%d" % pp, writes=[("RA", pp)])
        S.dma("sp", BT[pp][:], D["bt_d"][u], "ld_bt%d" % pp, writes=[("BT", pp)])
        S.dma("sp", KT[pp][:], D["kt_d"][u], "ld_kt%d" % pp, writes=[("KT", pp)])
        S.dma("sp", VB[pp][:], D["vb_d"][u], "ld_vb%d" % pp, writes=[("VB", pp)])
        S.dma("sp", GL[pp][:], D["gl_d"][u], "ld_gl%d" % pp, writes=[("GL", pp)])

    def stage1(u):
        pp = u % 2
        ra, bt, kt, vb = RA[pp], BT[pp], KT[pp], VB[pp]
        rkeys = [("RA", pp), ("BT", pp), ("KT", pp), ("VB", pp)]
        for cp in range(8):
            sysl0 = [(2 * ci + hd, ci, hd) for hd in range(2) for ci in range(2)]
            sysl1 = [(2 * ci + hd, ci, hd) for hd in (1, 0) for ci in range(2)]
            sysl = sysl0
            for q, ci, hd in sysl0:
                n = 2 * cp + ci
                ph = slice(64 * hd, 64 * hd + 64)
                cn = slice(n * 128, (n + 1) * 128)
                S.op("pe", lambda e: e.matmul(P1[:, q, :], bt[ph, cn], ra[ph, n, 128:256], start=True, stop=True),
                     reads=rkeys, writes=["P1"], inc=(ci == 1), rg=hd)
            for q, ci, hd in sysl1:
                n = 2 * cp + ci
                ph = slice(64 * hd, 64 * hd + 64)
                cn = slice(n * 128, (n + 1) * 128)
                S.op("pe", lambda e: e.matmul(P2[:, q, :], ra[ph, n, 128:256], bt[ph, cn], start=True, stop=True),
                     reads=rkeys, writes=["P2"], inc=(ci == 1), rg=hd)
            for kind, (lt, rsl) in enumerate(((kt, slice(128, 256)), (bt, slice(0, 128)), (kt, slice(0, 128)))):
                for q, ci, hd in (sysl0 if kind % 2 == 0 else sysl1):
                    n = 2 * cp + ci
                    ph = slice(64 * hd, 64 * hd + 64)
                    cn = slice(n * 128, (n + 1) * 128)
                    S.op("pe", lambda e: e.matmul(PA[kind][:, q, :], lt[ph, cn], ra[ph, n, rsl], start=True, stop=True),
                         reads=rkeys, writes=[("PA", kind)], inc=(ci == 1), rg=hd)
            S.op("dve", lambda e: e.tensor_tensor(Xb[0][:], P1[:], k.ms4[:], ALU.mult), reads=["P1", "ms4"], writes=[("X", 0)])
            S.op("dve", lambda e: e.tensor_tensor(XTb[0][:], P2[:], k.ml4[:], ALU.mult), reads=["P2", "ml4"], writes=[("XT", 0)])
            ach = ACH[pp][:, 2 * cp:2 * cp + 2, :, :, :].rearrange("p c h k t -> p (c h) k t")
            for kind, mk, mkey in ((0, k.ms4, "ms4"), (1, k.mi4, "mi4"), (2, k.mi4, "mi4")):
                S.op("act", lambda e: e.copy(ach[:, :, kind, :], PA[kind][:]), reads=[("PA", kind)], writes=[("ACH", pp, cp)])
                S.op("pool", lambda e: e.tensor_tensor(ach[:, :, kind, :], ach[:, :, kind, :], mk[:], ALU.mult),
                     reads=[("ACH", pp, cp), mkey], writes=[("ACH", pp, cp)])
            S.op("dve", lambda e: e.tensor_tensor(Pf[:], Xb[0][:], k.I4[:], ALU.add), reads=[("X", 0), "I4"], writes=["Pf"])
            S.op("act", lambda e: e.copy(Pbf[:], Pf[:]), reads=["Pf"], writes=["Pbf"])
            if scm < 1:
                yield
                continue
            pT = PM[0][:].bitcast(BF16)
            for ci in range(2):
                n = 2 * cp + ci
                cn = slice(n * 128, (n + 1) * 128)
                for j, src in enumerate((bt, kt, vb)):
                    S.op("pe", lambda e: e.transpose(pT[:, (ci * 3 + j) * 128:(ci * 3 + j + 1) * 128], src[:, cn], k.ident_bf[:]),
                         reads=rkeys + ["ident_bf"], writes=[("PM", 0)], inc=(ci == 1 and j == 2))
            S.op("act", lambda e: e.copy(TOK[pp][:, 2 * cp:2 * cp + 2, :, :].rearrange("p c j t -> p (c j t)"), pT[:, 0:768]),
                 reads=[("PM", 0)], writes=[("TOK", pp, cp)])
            pT3 = pT[:, 0:768].rearrange("p (c j t) -> p c j t", c=2, j=3)
            for hd in range(2):
                S.op("act", lambda e: e.copy(VPAD[pp][:, 2 * cp:2 * cp + 2, hd, 64 * hd:64 * hd + 64], pT3[:, :, 2, 64 * hd:64 * hd + 64]),
                     reads=[("PM", 0)], writes=[("VPAD", pp)])
            yield
            for lv in range(0, 7 if scm >= 2 else 0):
                xi, xo = lv % 2, (lv + 1) % 2
                if lv >= 1:
                    for q in range(4):
                        S.op("pe", lambda e: e.matmul(PA[2][:, q, :], XTb[xi][:, q, :], Pbf[:, q, :], start=True, stop=True),
                             reads=[("XT", xi), "Pbf"], writes=[("PA", 2)], inc=(q == 3))
                if lv < 6:
                    for q in range(4):
                        S.op("pe", lambda e: e.matmul(PA[0][:, q, :], XTb[xi][:, q, :], Xb[xi][:, q, :], start=True, stop=True),
                             reads=[("X", xi), ("XT", xi)], writes=[("PA", 0)], inc=(q == 3))
                    for q in range(4):
                        S.op("pe", lambda e: e.matmul(PA[1][:, q, :], Xb[xi][:, q, :], XTb[xi][:, q, :], start=True, stop=True),
                             reads=[("X", xi), ("XT", xi)], writes=[("PA", 1)], inc=(q == 3))
                if lv >= 1:
                    S.op("dve", lambda e: e.tensor_tensor(Pf[:], Pf[:], PA[2][:], ALU.add), reads=["Pf", ("PA", 2)], writes=["Pf"])
                    if lv < 6:
                        S.op("act", lambda e: e.copy(Pbf[:], Pf[:]), reads=["Pf"], writes=["Pbf"])
                    else:
                        S.op("act", lambda e: e.copy(TT[pp][:, 2 * cp:2 * cp + 2, :, :].rearrange("p c h t -> p (c h) t"), Pf[:]),
                             reads=["Pf"], writes=[("TT", pp, cp)])
                if lv < 6:
                    S.op("act", lambda e: e.copy(Xb[xo][:], PA[0][:]), reads=[("PA", 0)], writes=[("X", xo)])
                    S.op("dve", lambda e: e.tensor_copy(XTb[xo][:], PA[1][:]), reads=[("PA", 1)], writes=[("XT", xo)])
                yield

    def chain(u):
        pp = u % 2
        ra = RA[pp]
        if scm < 3:
            return
        S.op("dve", lambda e: e.memset(Sf[:], 0.0), writes=["Sf"])
        S.op("dve", lambda e: e.memset(Sbf[0][:], 0.0), writes=[("Sbf", 0)])
        for n in range(16):
            cp, ci = n // 2, n % 2
            si, so = n % 2, (n + 1) % 2
            cn = slice(n * 128, (n + 1) * 128)
            akey, tkey, ttkey = ("ACH", pp, cp), ("TOK", pp, cp), ("TT", pp, cp)
            S.op("pe", lambda e: e.matmul(PC[:, 0, :], ra[:, n, 128:256], Sbf[si][:], start=True, stop=False),
                 reads=[("RA", pp), ("Sbf", si)], writes=["PC"], inc=False)
            for hd in range(2):
                hs = slice(64 * hd, 64 * hd + 64)
                S.op("pe", lambda e: e.matmul(PC[:, 0, hs], ACH[pp][:, n, hd, 0, :], TOK[pp][:, n, 2, hs], start=False, stop=(hd == 1)),
                     reads=[akey, tkey], writes=["PC"], inc=(hd == 1))
            S.op("act", lambda e: e.copy(Wsb[:], PC[:, 0, :]), reads=["PC"], writes=["Wsb"])
            yield
            import os
            sub = int(os.environ.get("SCSUB", "9"))
            if sub <= 1:
                continue
            for hd in range(2):
                hs = slice(64 * hd, 64 * hd + 64)
                S.op("pe", lambda e: e.matmul(PC[:, 1, hs], TT[pp][:, n, hd, :], Wsb[:, hs], start=True, stop=True),
                     reads=[ttkey, "Wsb"], writes=["PC"], inc=(hd == 1))
            S.op("dve", lambda e: e.tensor_copy(Ut[:], PC[:, 1, :]), reads=["PC"], writes=["Ut"])
            for hd in range(2 if os.environ.get("NOUPAD") is None else 0):
                hs = slice(64 * hd, 64 * hd + 64)
                S.op("dve", lambda e: e.tensor_copy(UPAD[ci][:, hd, hs], PC[:, 1, hs]), reads=["PC"], writes=[("UPAD", ci)])
            yield
            if sub <= 2:
                continue
            PY = PM[1][:, 0:128]
            S.op("pe", lambda e: e.matmul(PY, Sbf[si][:], ra[:, n, 0:128], start=True, stop=False),
                 reads=[("RA", pp), ("Sbf", si)], writes=[("PM", 1)], inc=False)
            for hd in range(2):
                S.op("pe", lambda e: e.matmul(PY, UPAD[ci][:, hd, :], ACH[pp][:, n, hd, 1, :], start=False, stop=False),
                     reads=[("UPAD", ci), akey], writes=[("PM", 1)], inc=False)
                S.op("pe", lambda e: e.matmul(PY, VPAD[pp][:, n, hd, :], ACH[pp][:, n, hd, 2, :], start=False, stop=(hd == 1)),
                     reads=[("VPAD", pp), akey], writes=[("PM", 1)], inc=(hd == 1))
            if sub <= 3:
                S.op("act", lambda e: e.copy(Yf[:, cn], PY), reads=[("PM", 1)], writes=["Yf"])
                continue
            S.op("pe", lambda e: e.matmul(PC[:, 3, :], TOK[pp][:, n, 0, :], Ut[:], start=True, stop=False),
                 reads=[tkey, "Ut"], writes=["PC"], inc=False)
            S.op("pe", lambda e: e.matmul(PC[:, 3, :], TOK[pp][:, n, 1, :], TOK[pp][:, n, 2, :], start=False, stop=True),
                 reads=[tkey], writes=["PC"])
            S.op("act", lambda e: e.copy(Yf[:, cn], PY), reads=[("PM", 1)], writes=["Yf"])
            S.op("dve", lambda e: e.scalar_tensor_tensor(t1[:], PC[:, 3, :], GL[pp][:, n:n + 1], k.blk_f[:], ALU.mult, ALU.mult),
                 reads=["PC", ("GL", pp), "blk_f"], writes=["t1"])
            S.op("dve", lambda e: e.scalar_tensor_tensor(Sbf[so][:], Sf[:], GL[pp][:, n:n + 1], t1[:], ALU.mult, ALU.add),
                 reads=["Sf", "t1", ("GL", pp)], writes=[("Sbf", so)])
            S.op("dve", lambda e: e.scalar_tensor_tensor(Sf[:], Sf[:], GL[pp][:, n:n + 1], t1[:], ALU.mult, ALU.add),
                 reads=["Sf", "t1", ("GL", pp)], writes=["Sf"])
            yield
        if sub <= 4:
            return
        S.dma("sp", Bo[:], D["bo_d"][u], "ld_bo", writes=["Bo"])
        S.dma("sp", Bg[:], D["bg_d"][u], "ld_bg", writes=["Bg"])
        S.op("act", lambda e: e.copy(yb[:], Yf[:]), reads=["Yf"], writes=["yb"])
        for g in range(4):
            gsl = slice(g * 512, (g + 1) * 512)
            S.op("pe", lambda e: e.matmul(PM[1][:], k.blk_bf[:], yb[:, gsl], start=True, stop=True),
                 reads=["blk_bf", "yb"], writes=[("PM", 1)])
            S.op("dve", lambda e: e.scalar_tensor_tensor(yc[:, gsl], PM[1][:], -1.0 / 64, Yf[:, gsl], ALU.mult, ALU.add),
                 reads=[("PM", 1), "Yf"], writes=["yc"])
        S.op("pool", lambda e: e.tensor_tensor(yb[:], yc[:], yc[:], ALU.mult), reads=["yc"], writes=["yb"])
        for g in range(4):
            gsl = slice(g * 512, (g + 1) * 512)
            S.op("pe", lambda e: e.matmul(PM[1][:], k.blk_bf[:], yb[:, gsl], start=True, stop=True),
                 reads=["blk_bf", "yb"], writes=[("PM", 1)])
            S.op("act", lambda e: e.activation(rs[:, gsl], PM[1][:], AF.Sqrt, bias=k.eps_gn[:], scale=1.0 / 64),
                 reads=[("PM", 1), "eps_gn"], writes=["Yf"])
        S.op("dve", lambda e: e.reciprocal(rs[:], rs[:]), reads=["Yf"], writes=["Yf"])
        S.op("dve", lambda e: e.tensor_tensor(yc[:], yc[:], rs[:], ALU.mult), reads=["yc", "Yf"], writes=["yc"])
        S.op("dve", lambda e: e.tensor_scalar(yc[:], yc[:], chv[:, CGG + u:CGG + u + 1], chv[:, CGB + u:CGB + u + 1], ALU.mult, ALU.add),
             reads=["yc", "chv2"], writes=["yc"])
        S.op("pool", lambda e: e.tensor_tensor(yc[:], yc[:], Bo[:], ALU.add), reads=["yc", "Bo"], writes=["yc"])
        S.op("dve", lambda e: e.tensor_tensor(hb[:], yc[:], Bg[:], ALU.mult), reads=["yc", "Bg"], writes=["hb"])
        S.dma("sp", D["hT_d"][u], hb[:], "st_hb", reads=["hb"])
        yield

    def drive(gens):
        gens = [g for g in gens if g is not None]
        while gens:
            for g in list(gens):
                try:
                    next(g)
                except StopIteration:
                    gens.remove(g)

    load(0)
    if scm < 0:
        return
    drive([stage1(0)])
    for u in range(npairs):
        if u + 1 < npairs:
            load(u + 1)
        drive([chain(u), stage1(u + 1) if u + 1 < npairs else None])


def layer_norm_rows(k, z, zkey, gbc, bbc, gbkey, out, okey, tmp):
    S, nc = k.S, k.nc
    st6, mv, rstd, nmr = tmp
    for i in range(4):
        S.op("dve", lambda e: e.bn_stats(st6[:, i, :], z[:, i * 512:(i + 1) * 512]), reads=[zkey], writes=["ln_st6"])
    S.op("dve", lambda e: e.bn_aggr(mv[:], st6[:].rearrange("p a b -> p (a b)")), reads=["ln_st6"], writes=["ln_mv"])
    S.op("act", lambda e: e.activation(rstd[:], mv[:, 1:2], AF.Sqrt, bias=k.eps_ln[:], scale=1.0), reads=["ln_mv", "eps_ln"], writes=["ln_rstd"])
    S.op("dve", lambda e: e.reciprocal(rstd[:], rstd[:]), reads=["ln_rstd"], writes=["ln_rstd"])
    S.op("dve", lambda e: e.scalar_tensor_tensor(nmr[:], mv[:, 0:1], -1.0, rstd[:], ALU.mult, ALU.mult),
         reads=["ln_mv", "ln_rstd"], writes=["ln_nmr"])
    S.op("act", lambda e: e.activation(z[:], z[:], AF.Identity, bias=nmr[:], scale=rstd[:]),
         reads=[zkey, "ln_nmr", "ln_rstd"], writes=[zkey])
    S.op("dve", lambda e: e.tensor_tensor(z[:], z[:], gbc[:], ALU.mult), reads=[zkey, gbkey], writes=[zkey])
    S.op("pool", lambda e: e.tensor_tensor(out[:], z[:], bbc[:], ALU.add), reads=[zkey, gbkey], writes=[okey])


def phase_b(k):
    nc, S, D = k.nc, k.S, k.D
    sb = lambda n, s, d=F32: k.sb(n, s, d, k.pst)
    ps = lambda n, s, d=F32: k.ps(n, s, d, k.pst)
    nchunks = k.cfg.get("b_chunks", 16)
    hTs = sb("b_hT", [128, 16, SEQ], BF16)
    wout = sb("b_wout", [128, 16, DM], BF16)
    for i in range(4):
        S.dma("sp", hTs[:, 4 * i:4 * i + 4, :], D["hT_d"][4 * i:4 * i + 4].rearrange("c p t -> p c t"), "b_hT%d" % i, writes=[("hT", i)])
    wov = D["w_out"].rearrange("(c p) n -> p c n", p=128)
    for i in range(4):
        S.dma("pool", wout[:, 4 * i:4 * i + 4, :], wov[:, 4 * i:4 * i + 4, :], "b_wout%d" % i, writes=[("wout", i)])
    gbc = sb("b_gbc", [128, DM]); bbc = sb("b_bbc", [128, DM])
    S.dma("sp", gbc[:], D["ln1"][0].partition_broadcast(128), "b_gbc", writes=["gb1"])
    S.dma("sp", bbc[:], D["ln1"][1].partition_broadcast(128), "b_bbc", writes=["gb1"])
    wr = sb("b_wr", [128, 16, NE]); brt = sb("b_brt", [128, NE])
    S.dma("sp", wr[:], D["w_router"].rearrange("(c p) e -> p c e", p=128), "b_wr", writes=["wr"])
    S.dma("sp", brt[:], D["b_router"][0].partition_broadcast(128), "b_brt", writes=["brt"])
    k.eps_ln = sb("eps_ln", [128, 1])
    S.op("dve", lambda e: e.memset(k.eps_ln[:], 1e-5), writes=["eps_ln"])
    ecap = sb("b_ecap", [128, NE])
    S.op("dve", lambda e: e.tensor_scalar_mul(ecap[:], k.iota_e[:], float(CAP)), reads=["iota_e"], writes=["ecap"])
    carry = sb("b_carry", [128, NE])
    S.op("dve", lambda e: e.memset(carry[:], 0.0), writes=["carry"])
    xc = [sb("b_xc%d" % i, [128, DM]) for i in range(2)]
    z = sb("b_z", [128, DM]); x1 = sb("b_x1", [128, DM]); x1T = sb("b_x1T", [128, 16, 128]); x1b = sb("b_x1b", [128, DM], BF16)
    lntmp = (sb("ln_st6", [128, 4, 6]), sb("ln_mv", [128, 2]), sb("ln_rstd", [128, 1]), sb("ln_nmr", [128, 1]))
    lg = sb("b_lg", [128, NE]); t8 = sb("b_t8", [128, 8]); oh = sb("b_oh", [128, 4, NE]); ma = sb("b_ma", [128, NE])
    mab = sb("b_mab", [128, NE], BF16); negm = sb("b_negm", [128, 1]); ev = sb("b_ev", [128, 4]); esum = sb("b_esum", [128, 1])
    pe_ = sb("b_pe", [128, NE]); ohp = sb("b_ohp", [128, 4, NE]); destf = sb("b_destf", [128, 4]); posk = sb("b_posk", [128, 4])
    ovf = sb("b_ovf", [128, 4])
    psM = [ps("b_psM%d" % i, [128, 512]) for i in range(4)]
    psT = [ps("b_psT%d" % i, [128, 4, 128]) for i in range(2)]
    psL = ps("b_psL", [128, 512])
    def mix_mm(c):
        tc_ = slice(c * 128, (c + 1) * 128)
        S.dma("sp", xc[c % 2][:], D["x"][tc_, :], "b_xc%d" % (c % 2), writes=[("xc", c % 2)])
        for nb in range(4):
            for fc in range(16):
                S.op("pe", lambda e: e.matmul(psM[nb][:], hTs[:, fc, tc_], wout[:, fc, nb * 512:(nb + 1) * 512],
                                              start=(fc == 0), stop=(fc == 15)),
                     reads=[("hT", fc // 4), ("wout", fc // 4)], writes=[("psM", nb)], inc=(fc == 15))

    mix_mm(0)
    for c in range(nchunks):
        tc_ = slice(c * 128, (c + 1) * 128)
        xcb = xc[c % 2]
        for nb in range(4):
            S.op("dve", lambda e: e.scalar_tensor_tensor(z[:, nb * 512:(nb + 1) * 512], xcb[:, nb * 512:(nb + 1) * 512], ALPHA,
                                                         psM[nb][:], ALU.mult, ALU.add),
                 reads=[("psM", nb), ("xc", c % 2)], writes=["z"])
        if c + 1 < nchunks:
            mix_mm(c + 1)
        layer_norm_rows(k, z, "z", gbc, bbc, "gb1", x1, "x1", lntmp)
        S.dma("sp", D["x1f_d"][tc_, :], x1[:], "b_x1st", reads=["x1"])
        S.op("act", lambda e: e.copy(x1b[:], x1[:]), reads=["x1"], writes=["x1b"])
        for r in range(4):
            pt = psT[r % 2]
            for i in range(4):
                dc = 4 * r + i
                S.op("pe", lambda e: e.transpose(pt[:, i, :], x1[:, dc * 128:(dc + 1) * 128], k.ident_f[:]),
                     reads=["x1", "ident_f"], writes=[("psT", r % 2)], inc=(i == 3))
            S.op("act", lambda e: e.copy(x1T[:, 4 * r:4 * r + 4, :], pt[:]), reads=[("psT", r % 2)], writes=["x1T"])
        for dc in range(16):
            S.op("pe", lambda e: e.matmul(psL[:, 0:NE], x1T[:, dc, :], wr[:, dc, :], start=(dc == 0), stop=(dc == 15)),
                 reads=["x1T", "wr"], writes=["psL"], inc=(dc == 15))
        S.op("dve", lambda e: e.tensor_tensor(lg[:], psL[:, 0:NE], brt[:], ALU.add), reads=["psL", "brt"], writes=["lg"])
        S.op("dve", lambda e: e.max(t8[:], lg[:]), reads=["lg"], writes=["t8"])
        for kk in range(4):
            S.op("dve", lambda e: e.tensor_scalar(oh[:, kk, :], lg[:], t8[:, kk:kk + 1], None, ALU.is_equal),
                 reads=["lg", "t8"], writes=["oh"])
        S.op("dve", lambda e: e.reduce_sum(ma[:], oh[:].rearrange("p k e -> p e k"), AX.X), reads=["oh"], writes=["ma"])
        S.op("dve", lambda e: e.tensor_copy(mab[:], ma[:]), reads=["ma"], writes=["mab"])
        S.op("dve", lambda e: e.tensor_scalar_mul(negm[:], t8[:, 0:1], -1.0), reads=["t8"], writes=["negm"])
        S.op("act", lambda e: e.activation(ev[:], t8[:, 0:4], AF.Exp, bias=negm[:], scale=1.0), reads=["t8", "negm"], writes=["ev"])
        S.op("dve", lambda e: e.reduce_sum(esum[:], ev[:], AX.X), reads=["ev"], writes=["esum"])
        S.op("dve", lambda e: e.reciprocal(esum[:], esum[:]), reads=["esum"], writes=["esum"])
        S.op("dve", lambda e: e.tensor_scalar_mul(k.gates_all[:, c, :], ev[:], esum[:, 0:1]), reads=["ev", "esum"], writes=["gates_all"])
        S.op("pe", lambda e: e.matmul(psL[:, 32:64], k.m_strict_bf[:], mab[:], start=True, stop=True),
             reads=["m_strict_bf", "mab"], writes=["psL"], inc=False)
        S.op("pe", lambda e: e.matmul(psL[:, 64:96], k.ones_bf[:], mab[:], start=True, stop=True),
             reads=["ones_bf", "mab"], writes=["psL"])
        S.op("dve", lambda e: e.tensor_tensor(pe_[:], psL[:, 32:64], carry[:], ALU.add), reads=["psL", "carry"], writes=["pe"])
        S.op("dve", lambda e: e.tensor_tensor(carry[:], psL[:, 64:96], carry[:], ALU.add), reads=["psL", "carry"], writes=["carry"])
        for kk in range(4):
            S.op("dve", lambda e: e.tensor_tensor(ohp[:, kk, :], oh[:, kk, :], pe_[:], ALU.mult), reads=["oh", "pe"], writes=["ohp"])
        S.op("dve", lambda e: e.reduce_sum(posk[:], ohp[:], AX.X), reads=["ohp"], writes=["posk"])
        for kk in range(4):
            S.op("dve", lambda e: e.tensor_tensor(ohp[:, kk, :], oh[:, kk, :], ecap[:], ALU.mult), reads=["oh", "ecap"], writes=["ohp"])
        S.op("dve", lambda e: e.reduce_sum(destf[:], ohp[:], AX.X), reads=["ohp"], writes=["destf"])
        S.op("dve", lambda e: e.tensor_scalar_min(posk[:], posk[:], float(CAP - 1)), reads=["posk"], writes=["posk"])
        S.op("dve", lambda e: e.tensor_tensor(destf[:], destf[:], posk[:], ALU.add), reads=["destf", "posk"], writes=["destf"])
        S.op("dve", lambda e: e.tensor_copy(k.dest_all[:, c, :], destf[:]), reads=["destf"], writes=["dest_all"])
        for kk in range(4):
            S.dma("pool", D["xdisp_d"][:, :], x1b[:], "b_disp", reads=["x1b", "dest_all", "xdisp"],
                  indirect=(bass.IndirectOffsetOnAxis(ap=k.dest_all[:, c, kk:kk + 1], axis=0), None))


def phase_c(k):
    nc, S, D = k.nc, k.S, k.D
    sb = lambda n, s, d=F32: k.sb(n, s, d, k.pst)
    ps = lambda n, s, d=F32: k.ps(n, s, d, k.pst)
    nexp = k.cfg.get("c_experts", NE)
    NJ = CAP // 128
    xtok = sb("c_xtok", [128, NJ, DM], BF16)
    xeT = [sb("c_xeT%d" % i, [128, 16, CAP], BF16) for i in range(2)]
    NW = 7
    wring = [sb("c_w%d" % i, [128, 16, 512], BF16) for i in range(NW)]
    gg = sb("c_gg", [128, 4, CAP]); gsb = sb("c_gsb", [128, CAP]); sg = sb("c_sg", [128, CAP]); usb = sb("c_usb", [128, CAP])
    actT = sb("c_actT", [128, 16, CAP], BF16)
    ysb = [sb("c_ysb%d" % i, [128, 512]) for i in range(4)]
    nys = [0]
    bdn = sb("c_bdn", [128, DM])
    bgu = sb("c_bgu", [128, NE, 32])
    S.dma("sp", bgu[:].rearrange("p e f -> p (e f)"), D["b_gu_r"], "c_bgu", writes=["bgu"])
    psT = [ps("c_psT%d" % i, [128, 512]) for i in range(2)]
    psG = [ps("c_psG%d" % i, [128, 512]) for i in range(3)]
    psD = [ps("c_psD%d" % i, [128, 512]) for i in range(3)]
    nwg = [0]; nwd = [0]; npg = [0]; npd = [0]

    def load_expert_inputs(e):
        S.dma("sp", xtok[:], D["xdisp_d"][e * CAP:(e + 1) * CAP, :].rearrange("(j p) d -> p j d", p=128), "c_xtok",
              reads=["xdisp"], writes=["xtok"])

    def transposes(e):
        xe = xeT[e % 2]
        xkey = ("xeT", e % 2)
        tn = 0
        for j in range(NJ):
            for r in range(2):
                pt = psT[tn % 2]
                ptb = pt[:].bitcast(BF16)
                for i in range(8):
                    dc = 8 * r + i
                    S.op("pe", lambda e_: e_.transpose(ptb[:, i * 128:(i + 1) * 128], xtok[:, j, dc * 128:(dc + 1) * 128], k.ident_bf[:]),
                         reads=["xtok", "ident_bf"], writes=[("c_psT", tn % 2)], inc=(i == 7))
                S.op("act", lambda e_: e_.copy(xe[:, 8 * r:8 * r + 8, j * 128:(j + 1) * 128],
                                               ptb[:].rearrange("p (i s) -> p i s", s=128)),
                     reads=[("c_psT", tn % 2)], writes=[xkey])
                tn += 1

    load_expert_inputs(0)
    transposes(0)
    for e in range(nexp):
        S.dma("sp", bdn[:], D["b_dn"][e].partition_broadcast(128), "c_bdn", writes=["bdn"])
        if e + 1 < nexp:
            load_expert_inputs(e + 1)
        xe = xeT[e % 2]
        xkey = ("xeT", e % 2)
        for t in range(4):
            for half in range(2):
                wi = nwg[0] % NW; nwg[0] += 1
                w = wring[wi]; wkey = ("w", wi)
                c0 = half * DM + t * 512
                S.dma("pool", w[:], D["w_gu"][e, :, c0:c0 + 512].rearrange("(c p) n -> p c n", p=128), "c_w%d" % wi,
                      writes=[wkey])
                for fbi in range(4):
                    fb = t * 4 + fbi
                    pg = psG[npg[0] % 3]; pgkey = ("psG", npg[0] % 3); npg[0] += 1
                    for dc in range(16):
                        S.op("pe", lambda e_: e_.matmul(pg[:, 0:CAP], w[:, dc, fbi * 128:(fbi + 1) * 128], xe[:, dc, :],
                                                        start=(dc == 0), stop=(dc == 15)),
                             reads=[wkey, xkey], writes=[pgkey], inc=(dc == 15))
                    bcol = bgu[:, e, half * 16 + fb:half * 16 + fb + 1]
                    if half == 0:
                        S.op("dve", lambda e_: e_.tensor_scalar(gsb[:], pg[:, 0:CAP], bcol, 7.0, ALU.add, ALU.min),
                             reads=[pgkey, "bgu"], writes=["gsb"])
                        S.op("act", lambda e_: e_.activation(sg[:], gsb[:], AF.Sigmoid, scale=1.702), reads=["gsb"], writes=["sg"])
                        S.op("dve", lambda e_: e_.tensor_tensor(gg[:, fbi, :], gsb[:], sg[:], ALU.mult), reads=["gsb", "sg"], writes=[("gg", fbi)])
                    else:
                        S.op("dve", lambda e_: e_.tensor_scalar(usb[:], pg[:, 0:CAP], bcol, 7.0, ALU.add, ALU.min),
                             reads=[pgkey, "bgu"], writes=["usb"])
                        S.op("dve", lambda e_: e_.tensor_scalar(usb[:], usb[:], -7.0, 1.0, ALU.max, ALU.add), reads=["usb"], writes=["usb"])
                        S.op("dve", lambda e_: e_.tensor_tensor(actT[:, fb, :], usb[:], gg[:, fbi, :], ALU.mult),
                             reads=["usb", ("gg", fbi)], writes=["actT"])
        if e + 1 < nexp:
            transposes(e + 1)
        for db in range(4):
            wi = nwg[0] % NW; nwg[0] += 1
            w = wring[wi]; wkey = ("w", wi)
            S.dma("pool", w[:], D["w_dn"][e, :, db * 512:(db + 1) * 512].rearrange("(c p) n -> p c n", p=128), "c_w%d" % wi,
                  writes=[wkey])
            for j in range(NJ):
                pd = psD[npd[0] % 3]; pdkey = ("psD", npd[0] % 3); npd[0] += 1
                for fc in range(16):
                    S.op("pe", lambda e_: e_.matmul(pd[:], actT[:, fc, j * 128:(j + 1) * 128], w[:, fc, :], start=(fc == 0), stop=(fc == 15)),
                         reads=["actT", wkey], writes=[pdkey], inc=(fc == 15))
                yi = nys[0] % 4; nys[0] += 1
                S.op("dve", lambda e_: e_.tensor_tensor(ysb[yi][:], pd[:], bdn[:, db * 512:(db + 1) * 512], ALU.add),
                     reads=[pdkey, "bdn"], writes=[("ysb", yi)])
                S.dma("sp", D["y_d"][e * CAP + j * 128:e * CAP + (j + 1) * 128, db * 512:(db + 1) * 512], ysb[yi][:], "c_yst%d" % yi,
                      reads=[("ysb", yi)])


def phase_d(k):
    nc, S, D = k.nc, k.S, k.D
    sb = lambda n, s, d=F32: k.sb(n, s, d, k.pst)
    nchunks = k.cfg.get("b_chunks", 16)
    gbc = sb("d_gbc", [128, DM]); bbc = sb("d_bbc", [128, DM])
    S.dma("sp", gbc[:], D["ln2"][0].partition_broadcast(128), "d_gbc", writes=["gb2"])
    S.dma("sp", bbc[:], D["ln2"][1].partition_broadcast(128), "d_bbc", writes=["gb2"])
    if not hasattr(k, "eps_ln"):
        k.eps_ln = sb("eps_ln", [128, 1])
    else:
        k.eps_ln = sb("eps_ln2", [128, 1])
    S.op("dve", lambda e: e.memset(k.eps_ln[:], 1e-5), writes=["eps_ln"])
    lntmp = (sb("ln2_st6", [128, 4, 6]), sb("ln2_mv", [128, 2]), sb("ln2_rstd", [128, 1]), sb("ln2_nmr", [128, 1]))
    x1 = [sb("d_x1_%d" % i, [128, DM]) for i in range(2)]
    yk = [[sb("d_y%d_%d" % (i, kk), [128, DM]) for kk in range(4)] for i in range(2)]
    acc = sb("d_acc", [128, DM]); ob = [sb("d_ob%d" % i, [128, DM]) for i in range(2)]
    def loads(c):
        tc_ = slice(c * 128, (c + 1) * 128)
        b = c % 2
        S.dma("sp", x1[b][:], D["x1f_d"][tc_, :], "d_x1_%d" % b, writes=[("dx1", b)])
        for kk in range(4):
            S.dma("pool", yk[b][kk][:], D["y_d"][:, :], "d_y%d_%d" % (b, kk), reads=["dest_all"], writes=[("dy", b, kk)],
                  indirect=(None, bass.IndirectOffsetOnAxis(ap=k.dest_all[:, c, kk:kk + 1], axis=0)))

    loads(0)
    for c in range(nchunks):
        tc_ = slice(c * 128, (c + 1) * 128)
        b = c % 2
        if c + 1 < nchunks:
            loads(c + 1)
        S.op("dve", lambda e: e.tensor_scalar_mul(acc[:], x1[b][:], ALPHA), reads=[("dx1", b)], writes=["acc"])
        for kk in range(4):
            S.op("dve", lambda e: e.scalar_tensor_tensor(acc[:], yk[b][kk][:], k.gates_all[:, c, kk:kk + 1], acc[:], ALU.mult, ALU.add),
                 reads=[("dy", b, kk), "gates_all", "acc"], writes=["acc"])
        layer_norm_rows(k, acc, "acc", gbc, bbc, "gb2", ob[b], ("ob", b), lntmp)
        S.dma("sp", D["out"][tc_, :], ob[b][:], "d_ob%d" % b, reads=[("ob", b)])


def prep_shared(inp):
    f = lambda a: np.ascontiguousarray(a, dtype=np.float32)
    w_in = inp["w_in"][0]

    def fm_tile(c0, n=128):
        t = np.zeros((2048, 128), np.float32)
        t[:, :n] = w_in[:, c0:c0 + n]
        return t.reshape(16, 128, 128).transpose(1, 0, 2).reshape(128, 2048)
    cols = [(128 * u, 128) for u in range(24)] + [(3072, 128), (3200, 128), (3328, 32)]
    cols += [(3360 + 128 * h, 128) for h in range(8)] + [(4384 + 128 * h, 128) for h in range(8)]
    w_fm = np.stack([fm_tile(c, n) for c, n in cols])
    w_v = np.stack([w_in[:, 5408 + 512 * g:5408 + 512 * (g + 1)].reshape(16, 128, 512).transpose(1, 0, 2).reshape(128, 8192)
                    for g in range(2)])
    mu = np.zeros(27 * 128, np.float32)
    mu[:3360] = inp["shift_mu"][0]
    chv = np.concatenate([inp[n][0].reshape(8, 128).T for n in ("w0", "a0", "k_k", "k_a", "gn_g", "gn_b", "r_k")], axis=1)
    sh = {
        "w_fm": f(w_fm), "w_v": f(w_v), "mu": f(mu.reshape(27, 128).T), "chv": f(chv),
        "w_up": f(inp["w_up"][0]), "a_up": f(inp["a_up"][0]), "g_up": f(inp["g_up"][0]),
        "lqk": f(np.concatenate([inp["lq1"][0], inp["lk1"][0], inp["lq2"][0], inp["lk2"][0]])[None, :]),
        "subln": f(inp["subln_g"][0][:, None]),
        "w_out": f(inp["w_out"][0]), "ln1": f(np.stack([inp["ln1_g"][0], inp["ln1_b"][0]])),
        "ln2": f(np.stack([inp["ln2_g"][0], inp["ln2_b"][0]])),
        "w_router": f(inp["w_router"][0]), "b_router": f(inp["b_router"][0][None, :]),
        "w_gu": f(inp["w_gu"][0]), "b_gu_r": f(inp["b_gu"][0].reshape(32, 32, 128).transpose(2, 0, 1).reshape(128, 1024)),
        "w_dn": f(inp["w_dn"][0]), "b_dn": f(inp["b_dn"][0]),
        "zeros": np.zeros((CAP, DM), dtype=ml_dtypes.bfloat16),
    }
    return sh


def prep_core(inp, b):
    xb = np.asarray(inp["x"][b], dtype=np.float32)
    return {"xT": np.ascontiguousarray(xb.T), "x": np.ascontiguousarray(xb)}


def kernel(**inputs):
    nc, _ = build()
    sh = prep_shared(inputs)
    in_maps = [dict(sh, **prep_core(inputs, b)) for b in range(8)]
    res = run_bass_kernel_spmd(nc, in_maps, core_ids=list(range(8)))
    return np.stack([np.asarray(r["out"], dtype=np.float32) for r in res.results])
```
